# Optimizing a Trainium2 kernel written in Bass

```python
import math
import jax
import jax.numpy as jnp
from jax import lax
import numpy as np

D_MODEL = 1024
BATCH = 16
SEQ = 4096
DEPTH = 2

CHUNK = 64
EPS = 1e-6
NEG_INF = -1e30
Q_BLOCK = 128
N_MIXERS = 4
GROUP_W = D_MODEL // N_MIXERS
D_MIX = N_MIXERS * GROUP_W
HEAD_DIM = 64
POOL_WINDOWS = (2, 4, 8, 16)
N_POOL_GROUPS = len(POOL_WINDOWS)
POOL_GROUP = GROUP_W // N_POOL_GROUPS
CA_HEADS = GROUP_W // HEAD_DIM
CA_LEFT_CHUNKS = 8
CA_MAX_REL = 256
CA_REL_SIZE = CHUNK + CA_MAX_REL
SA_HEADS = GROUP_W // HEAD_DIM
IDX_HEADS = 8
IDX_DIM = 64
TOPK_MAX = 256
MLA_HEADS = 4
MLA_NOPE = 64
MLA_ROPE = 32
MLA_V = GROUP_W // MLA_HEADS
Q_LORA = 256
KV_LORA = 128
ROPE_BASE = 10000.0
T5_BUCKETS = 32
T5_MAX_DIST = 128
D_FF = -(-8 * D_MODEL // (3 * 256)) * 256

IN_SPLITS = (
    ('pool_u', GROUP_W),
    ('ca_q', GROUP_W), ('ca_k', GROUP_W), ('ca_v', GROUP_W),
    ('sa_q', GROUP_W), ('sa_k', HEAD_DIM), ('sa_v', HEAD_DIM),
    ('idx_q', IDX_HEADS * IDX_DIM), ('idx_k', IDX_DIM), ('idx_w', IDX_HEADS),
    ('mla_cq', Q_LORA), ('mla_ckv', KV_LORA), ('mla_kr', MLA_ROPE),
)
IN_NAMES = tuple(n for n, _ in IN_SPLITS)
IN_OFFSETS = tuple(int(o) for o in np.cumsum([w for _, w in IN_SPLITS])[:-1])
IN_WIDTH = sum(w for _, w in IN_SPLITS)

kernel_name = 'hybrid_parallel_head_group_streaming_encoder'


def rmsnorm(x, g):
    xf = x.astype(jnp.float32)
    y = xf * lax.rsqrt(jnp.mean(xf * xf, axis=-1, keepdims=True) + EPS)
    return (y * g.astype(jnp.float32)).astype(x.dtype)


def group_rmsnorm(y, g):
    B, S, W = y.shape
    yg = y.reshape(B, S, N_MIXERS, W // N_MIXERS).astype(jnp.float32)
    yg = yg * lax.rsqrt(jnp.mean(yg * yg, axis=-1, keepdims=True) + EPS)
    return (yg.reshape(B, S, W) * g.astype(jnp.float32)).astype(y.dtype)


def rope(x, pos):
    half = x.shape[-1] // 2
    freqs = ROPE_BASE ** (-jnp.arange(half, dtype=jnp.float32) / half)
    ang = pos.astype(jnp.float32)[:, None] * freqs[None, :]
    ang = ang.reshape((1, x.shape[1]) + (1,) * (x.ndim - 3) + (half,))
    cos, sin = jnp.cos(ang), jnp.sin(ang)
    x1 = x[..., :half].astype(jnp.float32)
    x2 = x[..., half:].astype(jnp.float32)
    return jnp.concatenate([x1 * cos - x2 * sin, x1 * sin + x2 * cos], axis=-1).astype(x.dtype)


def t5_bucket(rel):
    nb = T5_BUCKETS // 2
    max_exact = nb // 2
    ret = jnp.where(rel > 0, nb, 0)
    n = jnp.abs(rel)
    nf = jnp.maximum(n, 1).astype(jnp.float32)
    large = max_exact + (jnp.log(nf / max_exact) / math.log(T5_MAX_DIST / max_exact)
                         * (nb - max_exact)).astype(jnp.int32)
    large = jnp.minimum(large, nb - 1)
    return ret + jnp.where(n < max_exact, n, large)


def pool_mixer(u, w_grp, scale):
    B, S, _ = u.shape
    ug = u.reshape(B, S, N_POOL_GROUPS, POOL_GROUP).astype(jnp.float32)
    cs = jnp.cumsum(ug, axis=1, dtype=jnp.float32)
    t = jnp.arange(S)
    means = []
    for gi, w in enumerate(POOL_WINDOWS):
        c = cs[:, :, gi]
        lag = jnp.pad(c, ((0, 0), (w, 0), (0, 0)))[:, :S]
        cnt = jnp.minimum(t + 1, w).astype(jnp.float32)[None, :, None]
        means.append((c - lag) / cnt)
    d = (jnp.stack(means, axis=2) - ug).astype(u.dtype)
    y = jnp.einsum('bsgc,gcd->bsgd', d, w_grp).reshape(B, S, GROUP_W)
    return y * scale


def chunk_band_attention(q, k, v, rel_table):
    B, S, H, Dh = q.shape
    NC = S // CHUNK
    NB = CA_LEFT_CHUNKS + 1
    qc = q.reshape(B, NC, CHUNK, H, Dh)

    def band(a):
        ac = a.reshape(B, NC, CHUNK, H, Dh)
        ap = jnp.pad(ac, ((0, 0), (CA_LEFT_CHUNKS, 0), (0, 0), (0, 0), (0, 0)))
        return jnp.concatenate([ap[:, j:j + NC] for j in range(NB)], axis=2)

    kb, vb = band(k), band(v)
    s = jnp.einsum('bnqhd,bnkhd->bnhqk', qc, kb).astype(jnp.float32) * (Dh ** -0.5)
    qi = jnp.arange(CHUNK)
    kj = jnp.arange(NB * CHUNK)
    dist = (CA_LEFT_CHUNKS * CHUNK + qi[:, None]) - kj[None, :]
    ridx = jnp.clip(dist, -(CHUNK - 1), CA_MAX_REL) + (CHUNK - 1)
    bias = rel_table[:, ridx].astype(jnp.float32)
    key_chunk = jnp.arange(NC)[:, None] + (kj // CHUNK)[None, :] - CA_LEFT_CHUNKS
    valid = key_chunk >= 0
    s = jnp.where(valid[None, :, None, None, :], s + bias[None, None], NEG_INF)
    p = jax.nn.softmax(s, axis=-1).astype(v.dtype)
    o = jnp.einsum('bnhqk,bnkhd->bnqhd', p, vb)
    return o.reshape(B, S, H * Dh)


def dsa_attention(q, k, v, iq, ik, iw, t5_table):
    B, S, H, Dh = q.shape
    topk = min(TOPK_MAX, S // 4)
    n_blocks = S // Q_BLOCK
    kpos = jnp.arange(S, dtype=jnp.int32)
    iw = iw.astype(jnp.float32) * ((IDX_HEADS ** -0.5) * (IDX_DIM ** -0.5))
    gather = jax.vmap(lambda a, i: a[i])

    def block(bi):
        t0 = bi * Q_BLOCK
        qb = lax.dynamic_slice_in_dim(q, t0, Q_BLOCK, axis=1)
        iqb = lax.dynamic_slice_in_dim(iq, t0, Q_BLOCK, axis=1)
        iwb = lax.dynamic_slice_in_dim(iw, t0, Q_BLOCK, axis=1)
        qpos = t0 + jnp.arange(Q_BLOCK, dtype=jnp.int32)
        logits = jnp.einsum('bthd,bsd->bths', iqb, ik).astype(jnp.float32)
        score = jnp.einsum('bth,bths->bts', iwb, jax.nn.relu(logits))
        adm = (kpos[None, :] // CHUNK) <= (qpos[:, None] // CHUNK)
        score = jnp.where(adm[None], score, -jnp.inf)
        _, sel = lax.top_k(score, topk)
        kg = gather(k, sel)
        vg = gather(v, sel)
        s = jnp.einsum('bthd,btkd->bhtk', qb, kg).astype(jnp.float32) * (Dh ** -0.5)
        bias = t5_table[t5_bucket(sel - qpos[None, :, None])].astype(jnp.float32)
        s = s + jnp.moveaxis(bias, -1, 1)
        valid = (sel // CHUNK) <= (qpos[None, :, None] // CHUNK)
        s = jnp.where(valid[:, None], s, NEG_INF)
        p = jax.nn.softmax(s, axis=-1).astype(vg.dtype)
        return jnp.einsum('bhtk,btkd->bthd', p, vg)

    out = lax.map(block, jnp.arange(n_blocks))
    return jnp.moveaxis(out, 0, 1).reshape(B, S, H * Dh)


def mla_attention(cq, ckv, kr, g_cq, g_ckv, w_uq, w_ukv, pos):
    B, S, _ = cq.shape
    q = jnp.einsum('bsr,rhd->bshd', rmsnorm(cq, g_cq), w_uq)
    q_nope = q[..., :MLA_NOPE]
    q_rope = rope(q[..., MLA_NOPE:], pos)
    kv = jnp.einsum('bsr,rhd->bshd', rmsnorm(ckv, g_ckv), w_ukv)
    k_nope = kv[..., :MLA_NOPE]
    v = kv[..., MLA_NOPE:]
    k_rope = rope(kr, pos)
    scale = (MLA_NOPE + MLA_ROPE) ** -0.5
    kchunk = jnp.arange(S) // CHUNK

    def block(bi):
        t0 = bi * Q_BLOCK
        qn = lax.dynamic_slice_in_dim(q_nope, t0, Q_BLOCK, axis=1)
        qr = lax.dynamic_slice_in_dim(q_rope, t0, Q_BLOCK, axis=1)
        s = (jnp.einsum('bthd,bshd->bhts', qn, k_nope)
             + jnp.einsum('bthd,bsd->bhts', qr, k_rope)).astype(jnp.float32) * scale
        qchunk = (t0 + jnp.arange(Q_BLOCK)) // CHUNK
        mask = kchunk[None, :] <= qchunk[:, None]
        s = jnp.where(mask, s, NEG_INF)
        p = jax.nn.softmax(s, axis=-1).astype(v.dtype)
        return jnp.einsum('bhts,bshd->bthd', p, v)

    out = lax.map(block, jnp.arange(S // Q_BLOCK))
    return jnp.moveaxis(out, 0, 1).reshape(B, S, MLA_HEADS * MLA_V)


def setup_inputs(seed: int = 0) -> dict:
    key = jax.random.key(seed)
    ks = jax.random.split(key, 24)
    f32 = jnp.float32

    def nrm(k, shape, s):
        return jax.random.normal(k, shape, f32) * s

    def gain(k, shape):
        return 1.0 + 0.05 * jax.random.normal(k, shape, f32)

    L = DEPTH
    return {
        'x': nrm(ks[0], (BATCH, SEQ, D_MODEL), 1.0),
        'c': nrm(ks[1], (BATCH, D_MODEL), 1.0),
        't5_table': nrm(ks[2], (T5_BUCKETS, SA_HEADS), 0.5),
        'w_mod': nrm(ks[3], (L, D_MODEL, 6 * D_MODEL), 0.5 * D_MODEL ** -0.5),
        'b_mod': nrm(ks[4], (L, 6 * D_MODEL), 0.02),
        'g_mix': gain(ks[5], (L, D_MODEL)),
        'w_in': nrm(ks[6], (L, D_MODEL, IN_WIDTH), D_MODEL ** -0.5),
        'pool_w': nrm(ks[7], (L, N_POOL_GROUPS, POOL_GROUP, POOL_GROUP), POOL_GROUP ** -0.5),
        'pool_scale': 1.0 + 0.1 * jax.random.normal(ks[8], (L, GROUP_W), f32),
        'ca_rel': nrm(ks[9], (L, CA_HEADS, CA_REL_SIZE), 0.5),
        'mla_g_cq': gain(ks[10], (L, Q_LORA)),
        'mla_g_ckv': gain(ks[11], (L, KV_LORA)),
        'mla_w_uq': nrm(ks[12], (L, Q_LORA, MLA_HEADS, MLA_NOPE + MLA_ROPE), Q_LORA ** -0.5),
        'mla_w_ukv': nrm(ks[13], (L, KV_LORA, MLA_HEADS, MLA_NOPE + MLA_V), KV_LORA ** -0.5),
        'g_group': gain(ks[14], (L, D_MIX)),
        'w_out': nrm(ks[15], (L, D_MIX, D_MODEL), D_MIX ** -0.5),
        'g_ffn': gain(ks[16], (L, D_MODEL)),
        'ffn_w1': nrm(ks[17], (L, D_MODEL, D_FF), D_MODEL ** -0.5),
        'ffn_w3': nrm(ks[18], (L, D_MODEL, D_FF), D_MODEL ** -0.5),
        'ffn_w2': nrm(ks[19], (L, D_FF, D_MODEL), D_FF ** -0.5),
        'g_final': gain(ks[20], (D_MODEL,)),
    }


def reference(x, c, t5_table, w_mod, b_mod, g_mix, w_in, pool_w, pool_scale, ca_rel,
              mla_g_cq, mla_g_ckv, mla_w_uq, mla_w_ukv, g_group, w_out,
              g_ffn, ffn_w1, ffn_w3, ffn_w2, g_final):
    B, S, D = x.shape
    pos = jnp.arange(S, dtype=jnp.int32)
    c_act = jax.nn.silu(c)
    for l in range(DEPTH):
        mod = c_act @ w_mod[l] + b_mod[l]
        sh1, sc1, gt1, sh2, sc2, gt2 = jnp.split(mod[:, None, :], 6, axis=-1)
        h = rmsnorm(x, g_mix[l]) * (1.0 + sc1) + sh1
        z = h @ w_in[l]
        parts = dict(zip(IN_NAMES, jnp.split(z, IN_OFFSETS, axis=-1)))
        y_a = pool_mixer(parts['pool_u'], pool_w[l], pool_scale[l])
        y_b = chunk_band_attention(
            parts['ca_q'].reshape(B, S, CA_HEADS, HEAD_DIM),
            parts['ca_k'].reshape(B, S, CA_HEADS, HEAD_DIM),
            parts['ca_v'].reshape(B, S, CA_HEADS, HEAD_DIM),
            ca_rel[l])
        y_c = dsa_attention(
            parts['sa_q'].reshape(B, S, SA_HEADS, HEAD_DIM),
            parts['sa_k'], parts['sa_v'],
            parts['idx_q'].reshape(B, S, IDX_HEADS, IDX_DIM),
            parts['idx_k'], parts['idx_w'], t5_table)
        y_d = mla_attention(parts['mla_cq'], parts['mla_ckv'], parts['mla_kr'],
                            mla_g_cq[l], mla_g_ckv[l], mla_w_uq[l], mla_w_ukv[l], pos)
        mix = group_rmsnorm(jnp.concatenate([y_a, y_b, y_c, y_d], axis=-1), g_group[l])
        x = x + gt1 * (mix @ w_out[l])
        h = rmsnorm(x, g_ffn[l]) * (1.0 + sc2) + sh2
        f = (jax.nn.silu(h @ ffn_w1[l]) * (h @ ffn_w3[l])) @ ffn_w2[l]
        x = x + gt2 * f
    return rmsnorm(x, g_final)
```

```python
import math
from contextlib import ExitStack

import numpy as np
import ml_dtypes
import concourse.bass as bass
import concourse.mybir as mybir
from concourse.bass_types import AP
from concourse.bass_utils import run_bass_kernel_spmd

AF = mybir.ActivationFunctionType
ALU = mybir.AluOpType
AX = mybir.AxisListType
F32 = mybir.dt.float32
BF16 = mybir.dt.bfloat16

D = 1024
DFF = 2816
FH = DFF // 2
NFC = FH // 128
EPS = 1e-6
TMW = 968
FMW = 2048
NBIS = 16
TOPK = 256
NEG = -1.0e30
EPOCH = 16000
N_CORES = 8


class Buf:
    def __init__(self, h, name):
        self.h = h
        self.name = name
        self.w = None
        self.r = {}

    def __getitem__(self, idx):
        return self.h[idx]


class Sched:
    ENG = ('pe', 'act', 'dve', 'pool', 'sp')

    def __init__(self, nc, es):
        self.nc = nc
        self.es = es
        self.engs = {'pe': nc.tensor, 'act': nc.scalar, 'dve': nc.vector, 'pool': nc.gpsimd, 'sp': nc.sync}
        self.sems = {}
        self.cnt = {k: 0 for k in self.ENG}
        self.seen = {k: {} for k in self.ENG}
        self.dq = {}
        for q, n in (('sp', 10), ('pool', 6), ('act', 4)):
            self.dq[q] = dict(sems=[es.enter_context(nc.semaphore(f"dma_{q}{i}")) for i in range(n)],
                              cnt=[0] * n, i=0)
        self.ninstr = 0

    def _sem(self, key, n):
        ep = n // EPOCH
        k = (key, ep)
        if k not in self.sems:
            self.sems[k] = self.es.enter_context(self.nc.semaphore(f"s_{key}_{ep}"))
        return self.sems[k], n % EPOCH + 1

    def _semval(self, ev):
        key, n = ev
        if isinstance(key, tuple):
            return self.dq[key[1]]['sems'][key[2]], 16 * (n + 1)
        return self._sem(key, n)

    def _need(self, eng, evs):
        out = {}
        for key, n in evs:
            if self.seen[eng].get(key, -1) >= n:
                continue
            if out.get(key, -1) < n:
                out[key] = n
        return list(out.items())

    def _emit_waits(self, eng, evs, ins_fn=None):
        need = self._need(eng, evs)
        for key, n in need:
            self.seen[eng][key] = n
        if ins_fn is None:
            for ev in need:
                sem, v = self._semval(ev)
                self.engs[eng].wait_ge(sem, v)
                self.ninstr += 1
            return None
        for ev in need[:-1]:
            sem, v = self._semval(ev)
            self.engs[eng].wait_ge(sem, v)
            self.ninstr += 1
        ins = ins_fn()
        if need:
            sem, v = self._semval(need[-1])
            ins._wait_ge(sem, v)
        return ins

    def _deps(self, eng, reads, writes):
        evs = []
        for b in reads:
            if b.w is not None:
                evs.append(b.w)
        for b in writes:
            if b.w is not None and b.w[0] != eng:
                evs.append(b.w)
            for k, n in b.r.items():
                if k != eng:
                    evs.append((k, n))
        return evs

    def op(self, eng, fn, reads=(), writes=(), multi=False):
        evs = self._deps(eng, reads, writes)
        if multi or eng == 'pe':
            self._emit_waits(eng, evs)
            ins = fn()
        else:
            ins = self._emit_waits(eng, evs, fn)
        n = self.cnt[eng]
        self.cnt[eng] += 1
        sem, _ = self._sem(eng, n)
        ins.then_inc(sem, 1)
        self.ninstr += 1
        for b in reads:
            b.r[eng] = n
        for b in writes:
            b.w = (eng, n)
            b.r = {}
        return ins

    def dma(self, q, out_ap, in_ap, reads=(), writes=(), **kw):
        dq = self.dq[q]
        i = dq['i'] % len(dq['sems'])
        dq['i'] += 1
        key = ('dma', q, i)
        evs = self._deps(q, reads, writes)
        if dq['cnt'][i] > 0:
            evs.append((key, dq['cnt'][i] - 1))
        self._emit_waits(q, evs)
        ins = self.engs[q].dma_start(out=out_ap, in_=in_ap, **kw)
        ins.then_inc(dq['sems'][i], 16)
        n = dq['cnt'][i]
        dq['cnt'][i] += 1
        self.ninstr += 1
        for b in reads:
            b.r[key] = n
        for b in writes:
            b.w = (key, n)
            b.r = {}

    def barrier(self):
        evs = []
        for k in self.ENG:
            if k != 'sp' and self.cnt[k] > 0:
                evs.append((k, self.cnt[k] - 1))
        for q, dq in self.dq.items():
            for i, c in enumerate(dq['cnt']):
                if c > 0:
                    evs.append((('dma', q, i), c - 1))
        self._emit_waits('sp', evs)
        ins = self.nc.sync.nop()
        n = self.cnt['sp']
        self.cnt['sp'] += 1
        sem, _ = self._sem('sp', n)
        ins.then_inc(sem, 1)
        self.ninstr += 1
        for k in self.ENG:
            if k != 'sp':
                self._emit_waits(k, [('sp', n)])
                for k2 in self.ENG:
                    if self.cnt[k2] > 0:
                        self.seen[k][k2] = max(self.seen[k].get(k2, -1), self.cnt[k2] - 1 if k2 != 'sp' else n)
                for q, dq in self.dq.items():
                    for i, c in enumerate(dq['cnt']):
                        if c > 0:
                            self.seen[k][('dma', q, i)] = c - 1


def bc_mid(ap, n):
    a = [list(x) for x in ap.ap]
    return AP(tensor=ap.tensor, offset=ap.offset, ap=[a[0], [0, n]] + a[1:])


def dram_ap(t, offset, pairs):
    return AP(tensor=t.tensor, offset=offset, ap=[list(p) for p in pairs])


def _t5_bucket_np(rel):
    nb = 16
    max_exact = 8
    ret = np.where(rel > 0, nb, 0)
    n = np.abs(rel)
    nf = np.maximum(n, 1).astype(np.float32)
    large = max_exact + (np.log(nf / np.float32(max_exact)) / np.float32(math.log(128 / max_exact))
                         * np.float32(nb - max_exact)).astype(np.int32)
    large = np.minimum(large, nb - 1)
    return ret + np.where(n < max_exact, n, large)


def host_consts(S):
    bf = ml_dtypes.bfloat16
    pa = np.zeros((128, 12, 128), np.float32)
    s = np.arange(128)[:, None]
    t = np.arange(128)[None, :]
    for gi, w in enumerate((2, 4, 8, 16)):
        inwin = ((s <= t) & (s > t - w)).astype(np.float32)
        pa[:, gi, :] = inwin / w - (s == t)
        cnt = np.minimum(t + 1, w).astype(np.float32)
        pa[:, 8 + gi, :] = inwin / cnt - (s == t)
        inprev = ((s - 128) > t - w).astype(np.float32)
        pa[:, 4 + gi, :] = inprev / w
    bm = np.zeros((128, 5, 128), np.float32)
    for d in range(5):
        diff = 2 * d + (t // 64) - (s // 64)
        bm[:, d, :] = ((diff >= 0) & (diff <= 8)).astype(np.float32)
    m = np.arange(384)
    bk = _t5_bucket_np((127 - m).astype(np.int32))
    oh = np.zeros((32, 384), np.float32)
    oh[bk, m] = 1.0
    half = 16
    freqs = (10000.0 ** (-np.arange(half, dtype=np.float32) / half)).astype(np.float32)
    ang = np.arange(S, dtype=np.float32)[None, :] * freqs[:, None]
    cs = np.zeros((2, 32, S), np.float32)
    cs[0, :16] = np.cos(ang)
    cs[0, 16:] = np.cos(ang)
    cs[1, :16] = -np.sin(ang)
    cs[1, 16:] = np.sin(ang)
    p2 = np.tile((0.5 ** np.arange(1, NBIS + 1, dtype=np.float32))[None, :], (128, 1)).astype(np.float32)
    return dict(c_poolA=pa.astype(bf), c_bandm=bm.astype(bf), c_t5oh=oh, c_cs=cs.astype(bf), c_pow2=p2)


def _in_cols():
    o = {}
    off = 0
    for n, w in (('pool_u', 256), ('ca_q', 256), ('ca_k', 256), ('ca_v', 256), ('sa_q', 256), ('sa_k', 64),
                 ('sa_v', 64), ('idx_q', 512), ('idx_k', 64), ('idx_w', 8), ('mla_cq', 256), ('mla_ckv', 128),
                 ('mla_kr', 32)):
        o[n] = np.arange(off, off + w)
        off += w
    return o


def layout_weights(inp, l):
    o = _in_cols()
    w_in = np.asarray(inp['w_in'][l])
    tm = np.concatenate([o['pool_u'], o['ca_v'], o['sa_v'], o['idx_w'], o['mla_cq'], o['mla_ckv']])
    w_tm = np.ascontiguousarray(w_in[:, tm])
    w_fm = np.zeros((D, FMW), np.float32)
    fm = np.concatenate([o['ca_q'], o['ca_k'], o['sa_q'], o['idx_q'], o['sa_k'], o['idx_k'], o['mla_cq'], o['mla_ckv']])
    w_fm[:, :fm.size] = w_in[:, fm]
    kr = o['mla_kr']
    w_fm[:, 1792 + 64:1792 + 96] = w_in[:, kr]
    w_fm[:, 1920 + 64:1920 + 96] = w_in[:, np.concatenate([kr[16:], kr[:16]])]
    wuq = np.asarray(inp['mla_w_uq'][l])
    wqa = np.ascontiguousarray(wuq.reshape(256, 384))
    wqb = np.zeros((256, 4, 96), np.float32)
    wqb[:, :, 64:80] = wuq[:, :, 80:96]
    wqb[:, :, 80:96] = wuq[:, :, 64:80]
    wukv = np.asarray(inp['mla_w_ukv'][l])
    wk = np.ascontiguousarray(wukv[:, :, :64].reshape(128, 256))
    wv = np.ascontiguousarray(wukv[:, :, 64:].reshape(128, 256))
    f = lambda a: np.ascontiguousarray(np.asarray(a), dtype=np.float32)
    return dict(
        w_mod=f(inp['w_mod'][l]), b_mod=f(inp['b_mod'][l]).reshape(1, -1), g_mix=f(inp['g_mix'][l]).reshape(1, -1),
        w_tm=w_tm, w_fm=w_fm, pool_w=f(inp['pool_w'][l]), pool_scale=f(inp['pool_scale'][l]).reshape(1, -1),
        ca_rel=f(inp['ca_rel'][l]), g_cq=f(inp['mla_g_cq'][l]).reshape(1, -1), g_ckv=f(inp['mla_g_ckv'][l]).reshape(1, -1),
        wqa=wqa, wqb=wqb.reshape(256, 384), wk=wk, wv=wv, g_group=f(inp['g_group'][l]).reshape(1, -1),
        w_out=f(inp['w_out'][l]), g_ffn=f(inp['g_ffn'][l]).reshape(1, -1), w1=f(inp['ffn_w1'][l]),
        w3=f(inp['ffn_w3'][l]), w2=f(inp['ffn_w2'][l]))


WSHAPES = dict(w_mod=[D, 6 * D], b_mod=[1, 6 * D], g_mix=[1, D], w_tm=[D, TMW], w_fm=[D, FMW], pool_w=[4, 64, 64],
               pool_scale=[1, 256], ca_rel=[4, 320], g_cq=[1, 256], g_ckv=[1, 128], wqa=[256, 384], wqb=[256, 384],
               wk=[128, 256], wv=[128, 256], g_group=[1, D], w_out=[D, D], g_ffn=[1, D], w1=[D, DFF], w3=[D, DFF],
               w2=[DFF, D])


def build(S, nseq, nlayer, steps, debug=False, phases=None):
    NT = S // 128
    NG = S // 512
    nc = bass.Bass("TRN2", target_bir_lowering=False)
    es = ExitStack()
    with es:
        def dram(name, shape, dt, kind="Internal"):
            return nc.dram_tensor(name, shape, dt, kind=kind).ap()

        x_in = dram("x", [nseq, S, D], F32, "ExternalInput")
        c_in = dram("c", [nseq, D], F32, "ExternalInput")
        t5_in = dram("t5", [32, 4], F32, "ExternalInput")
        gfin_in = dram("g_final", [1, D], F32, "ExternalInput")
        W = []
        for l in range(nlayer):
            W.append({k: dram(f"{k}_{l}", shp, F32, "ExternalInput") for k, shp in WSHAPES.items()})
        c_poolA = dram("c_poolA", [128, 12, 128], BF16, "ExternalInput")
        c_bandm = dram("c_bandm", [128, 5, 128], BF16, "ExternalInput")
        c_t5oh = dram("c_t5oh", [32, 384], F32, "ExternalInput")
        c_cs = dram("c_cs", [2, 32, S], BF16, "ExternalInput")
        c_pow2 = dram("c_pow2", [128, NBIS], F32, "ExternalInput")
        xo = dram("xo", [nseq, S, D], F32, "ExternalOutput")
        out = dram("out", [nseq, S, D], F32, "ExternalOutput")
        dk = "ExternalOutput" if debug else "Internal"
        ztm = dram("ztm", [S, 576], BF16, dk)
        zsm = dram("zsm", [S, 8], F32, dk)
        zrq = dram("zrq", [1, S], F32, dk)
        zrk = dram("zrk", [1, S], F32, dk)
        zfm = dram("zfm", [FMW, S], BF16, dk)
        ybuf = dram("ybuf", [S, D], F32, dk)
        h2T = dram("h2T", [D, S], BF16, dk)
        ext = dram("ext", [4, 768], F32, "Internal")
        t5v = dram("t5v", [4, 384], F32, "Internal")

        Sc = Sched(nc, es)
        op = Sc.op
        V, A, P, T = nc.vector, nc.scalar, nc.gpsimd, nc.tensor

        uid = [0]

        def sb(st, name, shape, dt):
            uid[0] += 1
            name = f"{name}_{uid[0]}"
            return Buf(st.enter_context(nc.sbuf_tensor(name, shape, dt)), name)

        def ps(st, name, dt=F32):
            uid[0] += 1
            name = f"{name}_{uid[0]}"
            shape = [128, 512] if dt == F32 else [128, 1024]
            return Buf(st.enter_context(nc.psum_tensor(name, shape, dt)), name)

        def want(p):
            return phases is None or p in phases

        ident = sb(es, "ident", [128, 128], BF16)
        identf = sb(es, "identf", [128, 128], F32)
        onesf = sb(es, "onesf", [128, 128], F32)
        epsT = sb(es, "epsT", [128, 1], F32)
        MOD = sb(es, "MOD", [128, 6, D], F32)
        op('pool', lambda: P.memset(identf[:], 1.0), writes=[identf])
        op('pool', lambda: P.affine_select(out=identf[:], in_=identf[:], pattern=[[-1, 128]], compare_op=ALU.is_equal,
                                           fill=0.0, base=0, channel_multiplier=1), reads=[identf], writes=[identf])
        op('dve', lambda: V.tensor_copy(out=ident[:], in_=identf[:]), reads=[identf], writes=[ident])
        Jf = sb(es, "Jf", [128, 128], F32)
        op('pool', lambda: P.memset(Jf[:], 1.0), writes=[Jf])
        op('pool', lambda: P.affine_select(out=Jf[:], in_=Jf[:], pattern=[[1, 128]], compare_op=ALU.is_equal,
                                           fill=0.0, base=-127, channel_multiplier=1), reads=[Jf], writes=[Jf])
        op('dve', lambda: V.memset(onesf[:], 1.0), writes=[onesf])
        op('dve', lambda: V.memset(epsT[:], EPS), writes=[epsT])

        rr = [0]

        def cast(out_ap, in_ap, reads, writes):
            k = rr[0] % 3
            rr[0] += 1
            if k == 0:
                op('pool', lambda: P.tensor_copy(out=out_ap, in_=in_ap), reads=reads, writes=writes)
            elif k == 1:
                op('act', lambda: A.copy(out=out_ap, in_=in_ap), reads=reads, writes=writes)
            else:
                op('dve', lambda: V.tensor_copy(out=out_ap, in_=in_ap), reads=reads, writes=writes)

        def rstd_from(ms, sq, rs, n=1):
            op('act', lambda: A.activation(out=sq[:, 0:n], in_=ms[:, 0:n], func=AF.Sqrt, bias=epsT[:, 0:1], scale=1.0),
               reads=[ms, epsT], writes=[sq])
            op('dve', lambda: V.reciprocal(out=rs[:, 0:n], in_=sq[:, 0:n]), reads=[sq], writes=[rs])

        for (seq, l, last) in steps:
            Wl = W[l]
            xin = x_in[seq] if l == 0 else xo[seq]
            xout = xo[seq]

            if want('mod'):
                with ExitStack() as ph:
                    cT = sb(ph, "cT", [128, 8], F32)
                    cact = sb(ph, "cact", [128, 8], F32)
                    cbc = sb(ph, "cbc", [128, 8, 128], F32)
                    wst = [sb(ph, f"wst{i}", [128, 8, 512], F32) for i in range(2)]
                    bst = [sb(ph, f"bst{i}", [128, 512], F32) for i in range(2)]
                    gbc = [sb(ph, f"gbc{i}", [128, D], F32) for i in range(2)]
                    pm = [ps(ph, f"pm{i}") for i in range(2)]
                    Sc.dma('sp', cT[:], dram_ap(c_in, seq * D, [[1, 128], [128, 8]]), writes=[cT],
                           allow_slow_non_contiguous=True)
                    Sc.dma('sp', gbc[0][:], Wl['g_mix'][0].partition_broadcast(128), writes=[gbc[0]])
                    Sc.dma('sp', gbc[1][:], Wl['g_ffn'][0].partition_broadcast(128), writes=[gbc[1]])
                    op('act', lambda: A.activation(out=cact[:], in_=cT[:], func=AF.Silu), reads=[cT], writes=[cact])
                    for kc in range(8):
                        op('dve', lambda: V.tensor_scalar(out=cbc[:, kc, :], in0=onesf[:], scalar1=cact[:, kc:kc + 1],
                                                          scalar2=None, op0=ALU.mult), reads=[onesf, cact], writes=[cbc])
                    MODf = MOD[:].rearrange("p a d -> p (a d)")
                    for n in range(12):
                        b = n % 2
                        Sc.dma('sp', wst[b][:], Wl['w_mod'][:, n * 512:(n + 1) * 512].rearrange("(k p) n -> p k n", p=128),
                               writes=[wst[b]])
                        Sc.dma('sp', bst[b][:], Wl['b_mod'][0, n * 512:(n + 1) * 512].partition_broadcast(128),
                               writes=[bst[b]])
                        for kc in range(8):
                            op('pe', lambda: T.matmul(pm[b][:], lhsT=cbc[:, kc, :], rhs=wst[b][:, kc, :], start=(kc == 0),
                                                      stop=(kc == 7)), reads=[cbc, wst[b]], writes=[pm[b]])
                        op('dve', lambda: V.tensor_tensor(out=MODf[:, n * 512:(n + 1) * 512], in0=pm[b][:], in1=bst[b][:],
                                                          op=ALU.add), reads=[pm[b], bst[b]], writes=[MOD])
                    for (j, g) in ((1, 0), (4, 1)):
                        op('dve', lambda: V.scalar_tensor_tensor(out=MOD[:, j, :], in0=MOD[:, j, :], scalar=1.0,
                                                                 in1=gbc[g][:], op0=ALU.add, op1=ALU.mult),
                           reads=[MOD, gbc[g]], writes=[MOD])
                Sc.barrier()

            if want('inproj'):
                with ExitStack() as ph:
                    WTM = sb(ph, "WTM", [128, 8, TMW], BF16)
                    WFM = sb(ph, "WFM", [128, 8, FMW], BF16)
                    stg = [sb(ph, f"stg{i}", [128, FMW], F32) for i in range(2)]
                    xb = [sb(ph, f"xb{i}", [128, D], F32) for i in range(2)]
                    t1 = sb(ph, "t1", [128, D], F32)
                    junk = sb(ph, "junk", [128, D], F32)
                    hb = sb(ph, "hb", [128, D], BF16)
                    hT4 = [sb(ph, f"hT4{i}", [128, 8, 512], BF16) for i in range(2)]
                    st = sb(ph, "st", [128, 8], F32)
                    stm = [sb(ph, f"stm{i}", [128, 576], BF16) for i in range(2)]
                    ssm = [sb(ph, f"ssm{i}", [128, 16], F32) for i in range(2)]
                    sfm = [sb(ph, f"sfm{i}", [128, 16, 512], BF16) for i in range(2)]
                    pT = ps(ph, "pT", BF16)
                    ptm = [ps(ph, f"ptm{i}") for i in range(2)]
                    pfm = [ps(ph, f"pfm{i}") for i in range(3)]
                    for kc in range(8):
                        b = kc % 2
                        Sc.dma('sp', stg[b][:, 0:TMW], Wl['w_tm'][kc * 128:(kc + 1) * 128, :], writes=[stg[b]])
                        cast(WTM[:, kc, :], stg[b][:, 0:TMW], [stg[b]], [WTM])
                    for kc in range(8):
                        b = kc % 2
                        Sc.dma('sp', stg[b][:], Wl['w_fm'][kc * 128:(kc + 1) * 128, :], writes=[stg[b]])
                        cast(WFM[:, kc, :], stg[b][:], [stg[b]], [WFM])
                    for i in range(NT):
                        g, r = i // 4, i % 4
                        x_t = xb[i % 2]
                        hT = hT4[g % 2]
                        Sc.dma('sp', x_t[:], xin[i * 128:(i + 1) * 128, :], writes=[x_t])
                        op('act', lambda: A.activation(out=junk[:], in_=x_t[:], func=AF.Square, scale=1.0 / 32.0,
                                                       accum_out=st[:, 0:1]), reads=[x_t], writes=[junk, st], multi=True)
                        op('act', lambda: A.activation(out=st[:, 1:2], in_=st[:, 0:1], func=AF.Sqrt, bias=epsT[:, 0:1],
                                                       scale=1.0), reads=[st, epsT], writes=[st])
                        op('dve', lambda: V.reciprocal(out=st[:, 2:3], in_=st[:, 1:2]), reads=[st], writes=[st])
                        op('dve', lambda: V.scalar_tensor_tensor(out=t1[:], in0=x_t[:], scalar=st[:, 2:3], in1=MOD[:, 1, :],
                                                                 op0=ALU.mult, op1=ALU.mult), reads=[x_t, st, MOD], writes=[t1])
                        op('pool', lambda: P.tensor_tensor(out=hb[:], in0=t1[:], in1=MOD[:, 0, :], op=ALU.add),
                           reads=[t1, MOD], writes=[hb])
                        for kc in range(8):
                            op('pe', lambda: T.transpose(out=pT[:, kc * 128:(kc + 1) * 128], in_=hb[:, kc * 128:(kc + 1) * 128],
                                                         identity=ident[:]), reads=[hb, ident], writes=[pT])
                        op('act', lambda: A.copy(out=hT[:, :, r * 128:(r + 1) * 128],
                                                 in_=pT[:].rearrange("p (k t) -> p k t", k=8)), reads=[pT], writes=[hT])
                        for kc in range(8):
                            op('pe', lambda: T.matmul(ptm[0][:], lhsT=hT[:, kc, r * 128:(r + 1) * 128], rhs=WTM[:, kc, 0:512],
                                                      start=(kc == 0), stop=(kc == 7)), reads=[hT, WTM], writes=[ptm[0]])
                        for kc in range(8):
                            op('pe', lambda: T.matmul(ptm[1][:, 0:456], lhsT=hT[:, kc, r * 128:(r + 1) * 128],
                                                      rhs=WTM[:, kc, 512:968], start=(kc == 0), stop=(kc == 7)),
                               reads=[hT, WTM], writes=[ptm[1]])
                        sm_, ss_ = stm[i % 2], ssm[i % 2]
                        op('act', lambda: A.copy(out=sm_[:, 0:512], in_=ptm[0][:]), reads=[ptm[0]], writes=[sm_])
                        op('dve', lambda: V.tensor_copy(out=sm_[:, 512:576], in_=ptm[1][:, 0:64]), reads=[ptm[1]], writes=[sm_])
                        op('dve', lambda: V.tensor_copy(out=ss_[:, 0:8], in_=ptm[1][:, 64:72]), reads=[ptm[1]], writes=[ss_])
                        op('act', lambda: A.activation(out=junk[:, 0:256], in_=ptm[1][:, 72:328], func=AF.Square,
                                                       scale=1.0 / 16.0, accum_out=ss_[:, 8:9]),
                           reads=[ptm[1]], writes=[junk, ss_], multi=True)
                        op('act', lambda: A.activation(out=junk[:, 0:128], in_=ptm[1][:, 328:456], func=AF.Square,
                                                       scale=1.0 / math.sqrt(128.0), accum_out=ss_[:, 9:10]),
                           reads=[ptm[1]], writes=[junk, ss_], multi=True)
                        op('act', lambda: A.activation(out=ss_[:, 10:12], in_=ss_[:, 8:10], func=AF.Sqrt, bias=epsT[:, 0:1],
                                                       scale=1.0), reads=[ss_, epsT], writes=[ss_])
                        op('dve', lambda: V.reciprocal(out=ss_[:, 12:14], in_=ss_[:, 10:12]), reads=[ss_], writes=[ss_])
                        Sc.dma('pool', ztm[i * 128:(i + 1) * 128, :], sm_[:], reads=[sm_])
                        Sc.dma('pool', zsm[i * 128:(i + 1) * 128, :], ss_[:, 0:8], reads=[ss_])
                        Sc.dma('pool', dram_ap(zrq, i * 128, [[1, 128], [1, 1]]), ss_[:, 12:13], reads=[ss_])
                        Sc.dma('pool', dram_ap(zrk, i * 128, [[1, 128], [1, 1]]), ss_[:, 13:14], reads=[ss_])
                        if r == 3:
                            sf = sfm[g % 2]
                            for ch in range(16):
                                pf = pfm[ch % 3]
                                for kc in range(8):
                                    op('pe', lambda: T.matmul(pf[:], lhsT=WFM[:, kc, ch * 128:(ch + 1) * 128], rhs=hT[:, kc, :],
                                                              start=(kc == 0), stop=(kc == 7)), reads=[WFM, hT], writes=[pf])
                                if ch % 2 == 0:
                                    op('act', lambda: A.copy(out=sf[:, ch, :], in_=pf[:]), reads=[pf], writes=[sf])
                                else:
                                    op('dve', lambda: V.tensor_copy(out=sf[:, ch, :], in_=pf[:]), reads=[pf], writes=[sf])
                            Sc.dma('pool', zfm[:, g * 512:(g + 1) * 512].rearrange("(c p) t -> p c t", p=128), sf[:], reads=[sf])
                Sc.barrier()

            if want('pool'):
                with ExitStack() as ph:
                    U = sb(ph, "U", [128, NT, 256], BF16)
                    PA = sb(ph, "PA", [128, 12, 128], BF16)
                    pwf = sb(ph, "pwf", [64, 4, 64], F32)
                    scb = sb(ph, "scb", [64, 256], F32)
                    PW = sb(ph, "PW", [64, 4, 64], BF16)
                    dT = [sb(ph, f"dT{i}", [64, 4, 128], BF16) for i in range(2)]
                    yst = [sb(ph, f"yst{i}", [128, 256], F32) for i in range(2)]
                    pd = [ps(ph, f"pd{i}") for i in range(2)]
                    py = [ps(ph, f"py{i}") for i in range(2)]
                    Sc.dma('sp', U[:], ztm[:, 0:256].rearrange("(n p) c -> p n c", p=128), writes=[U])
                    Sc.dma('sp', PA[:], c_poolA, writes=[PA])
                    Sc.dma('sp', pwf[:], Wl['pool_w'].rearrange("g c d -> c g d"), writes=[pwf])
                    Sc.dma('sp', scb[:], Wl['pool_scale'][0].partition_broadcast(64), writes=[scb])
                    op('dve', lambda: V.tensor_tensor(out=PW[:], in0=pwf[:], in1=scb[:].rearrange("p (g d) -> p g d", g=4),
                                                      op=ALU.mult), reads=[pwf, scb], writes=[PW])
                    for i in range(NT):
                        b = i % 2
                        for g in range(4):
                            a0 = 8 + g if i == 0 else g
                            op('pe', lambda: T.matmul(pd[b][0:64, g * 128:(g + 1) * 128], lhsT=U[:, i, g * 64:(g + 1) * 64],
                                                      rhs=PA[:, a0, :], start=True, stop=(i == 0)), reads=[U, PA], writes=[pd[b]])
                            if i > 0:
                                op('pe', lambda: T.matmul(pd[b][0:64, g * 128:(g + 1) * 128], lhsT=U[:, i - 1, g * 64:(g + 1) * 64],
                                                          rhs=PA[:, 4 + g, :], start=False, stop=True), reads=[U, PA], writes=[pd[b]])
                        op('act', lambda: A.copy(out=dT[b][:], in_=pd[b][0:64, :].rearrange("p (g t) -> p g t", g=4)),
                           reads=[pd[b]], writes=[dT[b]])
                        for g in range(4):
                            op('pe', lambda: T.matmul(py[b][:, g * 64:(g + 1) * 64], lhsT=dT[b][:, g, :], rhs=PW[:, g, :],
                                                      start=True, stop=True), reads=[dT[b], PW], writes=[py[b]])
                        op('dve', lambda: V.tensor_copy(out=yst[b][:], in_=py[b][:, 0:256]), reads=[py[b]], writes=[yst[b]])
                        Sc.dma('pool', ybuf[i * 128:(i + 1) * 128, 0:256], yst[b][:], reads=[yst[b]])
                Sc.barrier()

            if want('band'):
                with ExitStack() as ph:
                    KT = sb(ph, "KT", [64, 4, S], BF16)
                    QT = sb(ph, "QT", [64, 4, S], BF16)
                    VA = sb(ph, "VA", [128, NT, 4, 65], BF16)
                    raw = sb(ph, "raw", [128, 20, 128], F32)
                    raw2 = sb(ph, "raw2", [128, 20, 128], F32)
                    EB = sb(ph, "EB", [128, 20, 128], BF16)
                    bmk = sb(ph, "bmk", [128, 5, 128], BF16)
                    E = [sb(ph, f"E{i}", [128, 20, 128], BF16) for i in range(2)]
                    Pm = [sb(ph, f"Pm{i}", [128, 20, 128], BF16) for i in range(2)]
                    rden = sb(ph, "rden", [128, 4], F32)
                    yst = [sb(ph, f"ystb{i}", [128, 4, 64], F32) for i in range(2)]
                    SCp = [ps(ph, f"SCp{i}") for i in range(5)]
                    Op = [ps(ph, f"Op{i}") for i in range(2)]
                    Sc.dma('sp', QT[:], zfm[0:256, :].rearrange("(h d) t -> d h t", h=4), writes=[QT])
                    Sc.dma('sp', KT[:], zfm[256:512, :].rearrange("(h d) t -> d h t", h=4), writes=[KT])
                    for h in range(4):
                        Sc.dma('sp', VA[:, :, h, 0:64], ztm[:, 256 + h * 64:256 + (h + 1) * 64].rearrange("(n p) d -> p n d", p=128),
                               writes=[VA])
                    op('pool', lambda: P.memset(VA[:, :, :, 64:65], 1.0), writes=[VA])
                    Sc.dma('sp', bmk[:], c_bandm, writes=[bmk])
                    crs = sb(ph, "crs", [4, 320], F32)
                    exs = sb(ph, "exs", [4, 768], F32)
                    Sc.dma('sp', crs[:], Wl['ca_rel'], writes=[crs])
                    op('dve', lambda: V.tensor_copy(out=exs[:, 64:384], in_=crs[:]), reads=[crs], writes=[exs])
                    op('dve', lambda: V.tensor_copy(out=exs[:, 0:64], in_=crs[:, 0:1].to_broadcast([4, 64])), reads=[crs], writes=[exs])
                    op('dve', lambda: V.tensor_copy(out=exs[:, 384:768], in_=crs[:, 319:320].to_broadcast([4, 384])),
                       reads=[crs], writes=[exs])
                    Sc.dma('sp', ext, exs[:], reads=[exs])
                    Sc.barrier()
                    for h in range(4):
                        Sc.dma('sp', raw[:, h * 5:(h + 1) * 5, :], dram_ap(ext, h * 768, [[1, 128], [1, 640]]), writes=[raw])
                    for h in range(4):
                        op('pe', lambda: T.matmul(SCp[0][:], lhsT=Jf[:], rhs=raw[:, h * 5:h * 5 + 4, :].rearrange("p a t -> p (a t)"),
                                                  start=True, stop=True), reads=[Jf, raw], writes=[SCp[0]])
                        op('pe', lambda: T.matmul(SCp[1][:, 0:128], lhsT=Jf[:], rhs=raw[:, h * 5 + 4, :], start=True, stop=True),
                           reads=[Jf, raw], writes=[SCp[1]])
                        op('act', lambda: A.activation(out=raw2[:, h * 5:h * 5 + 4, :], in_=SCp[0][:].rearrange("p (a t) -> p a t", a=4),
                                                       func=AF.Exp), reads=[SCp[0]], writes=[raw2])
                        op('act', lambda: A.activation(out=raw2[:, h * 5 + 4, :], in_=SCp[1][:, 0:128], func=AF.Exp),
                           reads=[SCp[1]], writes=[raw2])
                        op('dve', lambda: V.tensor_tensor(out=EB[:, h * 5:(h + 1) * 5, :], in0=raw2[:, h * 5:(h + 1) * 5, :],
                                                          in1=bmk[:], op=ALU.mult), reads=[raw2, bmk], writes=[EB])
                    for i in range(NT):
                        b = i % 2
                        nd = min(i, 4) + 1
                        for h in range(4):
                            for d in range(nd):
                                j = i - d
                                blk = h * 5 + d
                                pb = SCp[blk // 4]
                                op('pe', lambda: T.matmul(pb[:, (blk % 4) * 128:(blk % 4 + 1) * 128], lhsT=KT[:, h, j * 128:(j + 1) * 128],
                                                          rhs=QT[:, h, i * 128:(i + 1) * 128], start=True, stop=True),
                                   reads=[KT, QT], writes=[pb])
                        for k in range(5):
                            op('act', lambda: A.activation(out=E[b][:, 4 * k:4 * k + 4, :],
                                                           in_=SCp[k][:].rearrange("p (a t) -> p a t", a=4), func=AF.Exp,
                                                           scale=0.125), reads=[SCp[k]], writes=[E[b]])
                        for hh in range(2):
                            op('dve', lambda: V.tensor_tensor(out=Pm[b][:, hh * 10:(hh + 1) * 10, :], in0=E[b][:, hh * 10:(hh + 1) * 10, :],
                                                              in1=EB[:, hh * 10:(hh + 1) * 10, :], op=ALU.mult),
                               reads=[E[b], EB], writes=[Pm[b]])
                        for h in range(4):
                            for d in range(nd):
                                j = i - d
                                op('pe', lambda: T.matmul(Op[b][:, h * 65:(h + 1) * 65], lhsT=Pm[b][:, h * 5 + d, :],
                                                          rhs=VA[:, j, h, :], start=(d == 0), stop=(d == nd - 1)),
                                   reads=[Pm[b], VA], writes=[Op[b]])
                        ov = Op[b][:, 0:260].rearrange("p (h d) -> p h d", h=4)
                        op('dve', lambda: V.reciprocal(out=rden[:], in_=ov[:, :, 64]), reads=[Op[b]], writes=[rden])
                        op('dve', lambda: V.tensor_tensor(out=yst[b][:], in0=ov[:, :, 0:64],
                                                          in1=rden[:].unsqueeze(2).to_broadcast([128, 4, 64]), op=ALU.mult),
                           reads=[Op[b], rden], writes=[yst[b]])
                        Sc.dma('pool', ybuf[i * 128:(i + 1) * 128, 256:512], yst[b][:].rearrange("p h d -> p (h d)"), reads=[yst[b]])
                Sc.barrier()

            if want('dsa'):
                with ExitStack() as ph:
                    IKT = sb(ph, "IKT", [64, S], BF16)
                    SKT = sb(ph, "SKT", [64, S], BF16)
                    VC = sb(ph, "VC", [128, NT, 65], BF16)
                    IQT = [sb(ph, f"IQT{i}", [64, 8, 128], BF16) for i in range(2)]
                    SQT = [sb(ph, f"SQT{i}", [64, 4, 128], BF16) for i in range(2)]
                    IW = [sb(ph, f"IW{i}", [128, 8], F32) for i in range(2)]
                    Dg = [sb(ph, f"Dg{i}", [128, 8, 128], BF16) for i in range(2)]
                    R = [sb(ph, f"R{i}", [128, 512], BF16) for i in range(3)]
                    SCs = sb(ph, "SCs", [128, S], F32)
                    cjunk = sb(ph, "cjunk", [128, S], BF16)
                    MK = sb(ph, "MK", [128, S], BF16)
                    MT = sb(ph, "MT", [128, NT, 128], BF16)
                    NM = sb(ph, "NM", [128, 8, 128], BF16)
                    EBT = sb(ph, "EBT", [128, 8, 128], BF16)
                    rawt = sb(ph, "rawt", [128, 8, 128], F32)
                    t5s = sb(ph, "t5s", [32, 4], F32)
                    ohs = sb(ph, "ohs", [32, 384], F32)
                    t5r = sb(ph, "t5r", [4, 384], F32)
                    t5bc = sb(ph, "t5bc", [128, 128], F32)
                    nt5 = sb(ph, "nt5", [128, 4], F32)
                    pw2 = sb(ph, "pw2", [128, NBIS], F32)
                    wts = sb(ph, "wts", [128, NBIS], F32)
                    bs = sb(ph, "bs", [128, 8], F32)
                    Eb = [sb(ph, f"Eb{i}", [128, 512], BF16) for i in range(2)]
                    Pb = [sb(ph, f"Pb{i}", [128, 4, 128], BF16) for i in range(2)]
                    rden = sb(ph, "rdenc", [128, 4], F32)
                    yst = [sb(ph, f"ystc{i}", [128, 4, 64], F32) for i in range(2)]
                    Lp = [ps(ph, f"Lp{i}") for i in range(2)]
                    SPp = ps(ph, "SPp")
                    MTp = ps(ph, "MTp", BF16)
                    STp = [ps(ph, f"STp{i}") for i in range(2)]
                    Op = [ps(ph, f"Opc{i}") for i in range(2)]
                    Sc.dma('sp', SKT[:], zfm[1280:1344, :], writes=[SKT])
                    Sc.dma('sp', IKT[:], zfm[1344:1408, :], writes=[IKT])
                    Sc.dma('sp', VC[:, :, 0:64], ztm[:, 512:576].rearrange("(n p) d -> p n d", p=128), writes=[VC])
                    op('pool', lambda: P.memset(VC[:, :, 64:65], 1.0), writes=[VC])
                    Sc.dma('sp', pw2[:], c_pow2, writes=[pw2])
                    Sc.dma('sp', t5s[:], t5_in, writes=[t5s])
                    Sc.dma('sp', ohs[:], c_t5oh, writes=[ohs])
                    Sc.dma('sp', t5bc[:], t5_in.rearrange("a b -> (a b)").partition_broadcast(128), writes=[t5bc])
                    op('pe', lambda: T.matmul(SPp[0:4, 0:384], lhsT=t5s[:], rhs=ohs[:], start=True, stop=True),
                       reads=[t5s, ohs], writes=[SPp])
                    op('dve', lambda: V.tensor_copy(out=t5r[:], in_=SPp[0:4, 0:384]), reads=[SPp], writes=[t5r])
                    Sc.dma('sp', t5v, t5r[:], reads=[t5r])
                    op('dve', lambda: V.tensor_scalar(out=nt5[:], in0=t5bc[:, 60:64], scalar1=-1.0, scalar2=None, op0=ALU.mult),
                       reads=[t5bc], writes=[nt5])
                    Sc.barrier()
                    for dd in range(2):
                        for h in range(4):
                            Sc.dma('sp', rawt[:, dd * 4 + h, :], dram_ap(t5v, h * 384 + 128 * dd, [[1, 128], [1, 128]]),
                                   writes=[rawt])
                    for dd in range(2):
                        op('pe', lambda: T.matmul(Lp[dd][:], lhsT=Jf[:], rhs=rawt[:, dd * 4:dd * 4 + 4, :].rearrange("p a t -> p (a t)"),
                                                  start=True, stop=True), reads=[Jf, rawt], writes=[Lp[dd]])
                        for h in range(4):
                            op('act', lambda: A.activation(out=EBT[:, dd * 4 + h, :], in_=Lp[dd][:, h * 128:(h + 1) * 128], func=AF.Exp,
                                                           bias=nt5[:, h:h + 1], scale=1.0), reads=[Lp[dd], nt5], writes=[EBT])
                    for i in range(NT):
                        b = i % 2
                        N = 128 * (i + 1)
                        Sc.dma('sp', IQT[b][:], zfm[768:1280, i * 128:(i + 1) * 128].rearrange("(h d) t -> d h t", h=8), writes=[IQT[b]])
                        Sc.dma('sp', SQT[b][:], zfm[512:768, i * 128:(i + 1) * 128].rearrange("(h d) t -> d h t", h=4), writes=[SQT[b]])
                        Sc.dma('sp', IW[b][:], zsm[i * 128:(i + 1) * 128, :], writes=[IW[b]])
                        for h in range(8):
                            op('pool', lambda: P.tensor_scalar(out=Dg[b][:, h, :], in0=ident[:], scalar1=IW[b][:, h:h + 1],
                                                               scalar2=None, op0=ALU.mult), reads=[ident, IW[b]], writes=[Dg[b]])
                        nblk = (N + 511) // 512
                        cnt = 0
                        for kb in range(nblk):
                            wb = min(512, N - kb * 512)

                            def acc(h, wb=wb, kb=kb):
                                rb = R[(cnt_base + h) % 3]
                                op('pe', lambda: T.matmul(SPp[:, 0:wb], lhsT=Dg[b][:, h, :], rhs=rb[:, 0:wb], start=(h == 0),
                                                          stop=(h == 7)), reads=[Dg[b], rb], writes=[SPp])
                            cnt_base = cnt
                            for h in range(8):
                                lp = Lp[(cnt_base + h) % 2]
                                rb = R[(cnt_base + h) % 3]
                                op('pe', lambda: T.matmul(lp[:, 0:wb], lhsT=IQT[b][:, h, :], rhs=IKT[:, kb * 512:kb * 512 + wb],
                                                          start=True, stop=True), reads=[IQT[b], IKT], writes=[lp])
                                op('act', lambda: A.activation(out=rb[:, 0:wb], in_=lp[:, 0:wb], func=AF.Relu),
                                   reads=[lp], writes=[rb])
                                if h >= 1:
                                    acc(h - 1)
                            acc(7)
                            cnt += 8
                            op('act', lambda: A.copy(out=SCs[:, kb * 512:kb * 512 + wb], in_=SPp[:, 0:wb]), reads=[SPp], writes=[SCs])
                        op('pool', lambda: P.memset(SCs[0:64, N - 64:N], NEG), writes=[SCs])
                        if i >= 2:
                            op('dve', lambda: V.tensor_reduce(out=bs[:, 0:1], in_=SCs[:, 0:N], axis=AX.X, op=ALU.max),
                               reads=[SCs], writes=[bs])
                            op('dve', lambda: V.tensor_reduce(out=bs[:, 3:4], in_=SCs[:, 0:N - 64], axis=AX.X, op=ALU.min),
                               reads=[SCs], writes=[bs])
                            op('dve', lambda: V.tensor_tensor(out=bs[:, 2:3], in0=bs[:, 0:1], in1=bs[:, 3:4], op=ALU.subtract),
                               reads=[bs], writes=[bs])
                            op('dve', lambda: V.tensor_scalar(out=wts[:], in0=pw2[:], scalar1=bs[:, 2:3], scalar2=None,
                                                              op0=ALU.mult), reads=[pw2, bs], writes=[wts])
                            for it in range(NBIS):
                                op('dve', lambda: V.tensor_tensor(out=bs[:, 4:5], in0=bs[:, 3:4], in1=wts[:, it:it + 1], op=ALU.add),
                                   reads=[bs, wts], writes=[bs])
                                op('dve', lambda: V.tensor_scalar(out=cjunk[:, 0:N], in0=SCs[:, 0:N], scalar1=bs[:, 4:5], scalar2=0.0,
                                                                  op0=ALU.is_ge, op1=ALU.add, accum_out=bs[:, 5:6]),
                                   reads=[SCs, bs], writes=[cjunk, bs], multi=True)
                                op('dve', lambda: V.tensor_scalar(out=bs[:, 6:7], in0=bs[:, 5:6], scalar1=TOPK - 0.5,
                                                                  scalar2=wts[:, it:it + 1], op0=ALU.is_gt, op1=ALU.mult),
                                   reads=[bs, wts], writes=[bs])
                                op('dve', lambda: V.tensor_tensor(out=bs[:, 3:4], in0=bs[:, 3:4], in1=bs[:, 6:7], op=ALU.add),
                                   reads=[bs], writes=[bs])
                        else:
                            op('dve', lambda: V.memset(bs[:, 3:4], -1.0e29), writes=[bs])
                        op('dve', lambda: V.tensor_scalar(out=MK[:, 0:N], in0=SCs[:, 0:N], scalar1=bs[:, 3:4], scalar2=None,
                                                          op0=ALU.is_ge), reads=[SCs, bs], writes=[MK])
                        for j0 in range(0, i + 1, 8):
                            nb = min(8, i + 1 - j0)
                            for jj in range(nb):
                                j = j0 + jj
                                op('pe', lambda: T.transpose(out=MTp[:, jj * 128:(jj + 1) * 128], in_=MK[:, j * 128:(j + 1) * 128],
                                                             identity=ident[:]), reads=[MK, ident], writes=[MTp])
                            op('act', lambda: A.copy(out=MT[:, j0:j0 + nb, :],
                                                     in_=MTp[:, 0:nb * 128].rearrange("p (a t) -> p a t", a=nb)),
                               reads=[MTp], writes=[MT])
                        op('dve', lambda: V.tensor_tensor(out=NM[:, 0:4, :], in0=EBT[:, 0:4, :], in1=bc_mid(MT[:, i, :], 4),
                                                          op=ALU.mult), reads=[EBT, MT], writes=[NM])
                        if i >= 1:
                            op('dve', lambda: V.tensor_tensor(out=NM[:, 4:8, :], in0=EBT[:, 4:8, :], in1=bc_mid(MT[:, i - 1, :], 4),
                                                              op=ALU.mult), reads=[EBT, MT], writes=[NM])
                        ob = Op[b]
                        for j in range(i + 1):
                            sp_, eb_, pb_ = STp[j % 2], Eb[j % 2], Pb[j % 2]
                            op('pe', lambda: T.matmul(sp_[:], lhsT=SKT[:, j * 128:(j + 1) * 128],
                                                      rhs=SQT[b][:].rearrange("p h t -> p (h t)"), start=True, stop=True),
                               reads=[SKT, SQT[b]], writes=[sp_])
                            op('act', lambda: A.activation(out=eb_[:], in_=sp_[:], func=AF.Exp, scale=0.125), reads=[sp_], writes=[eb_])
                            if j == i:
                                m_ap, m_rd = NM[:, 0:4, :], NM
                            elif j == i - 1:
                                m_ap, m_rd = NM[:, 4:8, :], NM
                            else:
                                m_ap, m_rd = bc_mid(MT[:, j, :], 4), MT
                            op('dve', lambda: V.tensor_tensor(out=pb_[:], in0=eb_[:].rearrange("p (h t) -> p h t", h=4), in1=m_ap,
                                                              op=ALU.mult), reads=[eb_, m_rd], writes=[pb_])
                            for h in range(4):
                                op('pe', lambda: T.matmul(ob[:, h * 65:(h + 1) * 65], lhsT=pb_[:, h, :], rhs=VC[:, j, :],
                                                          start=(j == 0 and h == 0), stop=(j == i), skip_group_check=True),
                                   reads=[pb_, VC], writes=[ob])
                        ov = ob[:, 0:260].rearrange("p (h d) -> p h d", h=4)
                        op('dve', lambda: V.reciprocal(out=rden[:], in_=ov[:, :, 64]), reads=[ob], writes=[rden])
                        op('dve', lambda: V.tensor_tensor(out=yst[b][:], in0=ov[:, :, 0:64],
                                                          in1=rden[:].unsqueeze(2).to_broadcast([128, 4, 64]), op=ALU.mult),
                           reads=[ob, rden], writes=[yst[b]])
                        Sc.dma('pool', ybuf[i * 128:(i + 1) * 128, 512:768], yst[b][:].rearrange("p h d -> p (h d)"), reads=[yst[b]])
                Sc.barrier()

            if want('mla'):
                with ExitStack() as ph:
                    CQTb = [sb(ph, f"CQT{i}", [128, 2, 512], BF16) for i in range(2)]
                    CKTb = [sb(ph, f"CKT{i}", [128, 512], BF16) for i in range(2)]
                    KRRb = [sb(ph, f"KRR{i}", [96, 2, 512], BF16) for i in range(2)]
                    CSb = [sb(ph, f"CS{i}", [96, 2, 512], BF16) for i in range(2)]
                    QT = sb(ph, "QTd", [96, 4, S], BF16)
                    KT = sb(ph, "KTd", [96, 4, S], BF16)
                    VD = sb(ph, "VD", [128, NT, 4, 65], BF16)
                    wst = sb(ph, "wstd", [128, 2, 384], F32)
                    gq = sb(ph, "gq", [128, 2], F32)
                    gk = sb(ph, "gk", [128, 1], F32)
                    WQA = sb(ph, "WQA", [128, 2, 384], BF16)
                    WQB = sb(ph, "WQB", [128, 2, 384], BF16)
                    WK = sb(ph, "WK", [128, 256], BF16)
                    WV = sb(ph, "WV", [128, 256], BF16)
                    RKc = sb(ph, "RKc", [128, NT], F32)
                    RQb = [sb(ph, f"RQb{i}", [96, 512], F32) for i in range(2)]
                    RKb = [sb(ph, f"RKb{i}", [64, 512], F32) for i in range(2)]
                    cr = sb(ph, "cr", [96, 512], F32)
                    sr = sb(ph, "sr", [96, 512], F32)
                    ta = sb(ph, "ta", [96, 512], F32)
                    tb = sb(ph, "tb", [96, 512], F32)
                    tk1 = sb(ph, "tk1", [96, 512], F32)
                    tk2 = sb(ph, "tk2", [96, 512], F32)
                    Eb = [sb(ph, f"Ed{i}", [128, 512], BF16) for i in range(3)]
                    rden = sb(ph, "rdend", [128, 4], F32)
                    yst = [sb(ph, f"ystd{i}", [128, 4, 64], F32) for i in range(2)]
                    PAp = [ps(ph, f"PAp{i}") for i in range(2)]
                    PBp = [ps(ph, f"PBp{i}") for i in range(2)]
                    STp = [ps(ph, f"STd{i}") for i in range(2)]
                    Op = [ps(ph, f"Opd{i}") for i in range(2)]
                    Sc.dma('sp', RKc[:], dram_ap(zrk, 0, [[1, 128], [128, NT]]), writes=[RKc], allow_slow_non_contiguous=True)
                    Sc.dma('sp', gq[:], dram_ap(Wl['g_cq'], 0, [[1, 128], [128, 2]]), writes=[gq], allow_slow_non_contiguous=True)
                    Sc.dma('sp', gk[:], dram_ap(Wl['g_ckv'], 0, [[1, 128], [1, 1]]), writes=[gk])
                    for (src, dst) in ((Wl['wqa'], WQA), (Wl['wqb'], WQB)):
                        Sc.dma('sp', wst[:], src.rearrange("(c p) n -> p c n", p=128), writes=[wst])
                        for c2 in range(2):
                            op('dve', lambda: V.tensor_scalar(out=dst[:, c2, :], in0=wst[:, c2, :], scalar1=gq[:, c2:c2 + 1],
                                                              scalar2=None, op0=ALU.mult), reads=[wst, gq], writes=[dst])
                    for (src, dst) in ((Wl['wk'], WK), (Wl['wv'], WV)):
                        Sc.dma('sp', wst[:, 0, 0:256], src, writes=[wst])
                        op('dve', lambda: V.tensor_scalar(out=dst[:], in0=wst[:, 0, 0:256], scalar1=gk[:, 0:1], scalar2=None,
                                                          op0=ALU.mult), reads=[wst, gk], writes=[dst])
                    op('pool', lambda: P.memset(VD[:, :, :, 64:65], 1.0), writes=[VD])
                    for m in range(NG):
                        b = m % 2
                        gblk = slice(m * 512, (m + 1) * 512)
                        blk = slice(0, 512)
                        CQT, CKT, KRR, CS = CQTb[b], CKTb[b], KRRb[b], CSb[b]
                        Sc.dma('sp', CQT[:], zfm[1408:1664, gblk].rearrange("(c p) t -> p c t", p=128), writes=[CQT])
                        Sc.dma('sp', CKT[:], zfm[1664:1792, gblk], writes=[CKT])
                        Sc.dma('sp', KRR[64:96, 0, :], zfm[1792 + 64:1792 + 96, gblk], writes=[KRR])
                        Sc.dma('sp', KRR[64:96, 1, :], zfm[1920 + 64:1920 + 96, gblk], writes=[KRR])
                        Sc.dma('sp', CS[64:96, 0, :], c_cs[0, :, gblk], writes=[CS])
                        Sc.dma('sp', CS[64:96, 1, :], c_cs[1, :, gblk], writes=[CS])
                        Sc.dma('sp', RQb[b][:], zrq[0, gblk].partition_broadcast(96), writes=[RQb[b]])
                        Sc.dma('sp', RKb[b][:], zrk[0, gblk].partition_broadcast(64), writes=[RKb[b]])
                        op('dve', lambda: V.tensor_tensor(out=cr[64:96, :], in0=CS[64:96, 0, blk], in1=RQb[b][64:96, :], op=ALU.mult),
                           reads=[CS, RQb[b]], writes=[cr])
                        op('dve', lambda: V.tensor_tensor(out=sr[64:96, :], in0=CS[64:96, 1, blk], in1=RQb[b][64:96, :], op=ALU.mult),
                           reads=[CS, RQb[b]], writes=[sr])
                        for h in range(4):
                            pa, pb2 = PAp[h % 2], PBp[h % 2]
                            for c2 in range(2):
                                op('pe', lambda: T.matmul(pa[0:96, :], lhsT=WQA[:, c2, h * 96:(h + 1) * 96], rhs=CQT[:, c2, blk],
                                                          start=(c2 == 0), stop=(c2 == 1)), reads=[WQA, CQT], writes=[pa])
                            for c2 in range(2):
                                op('pe', lambda: T.matmul(pb2[0:96, :], lhsT=WQB[:, c2, h * 96:(h + 1) * 96], rhs=CQT[:, c2, blk],
                                                          start=(c2 == 0), stop=(c2 == 1)), reads=[WQB, CQT], writes=[pb2])
                            op('dve', lambda: V.tensor_tensor(out=QT[0:64, h, gblk], in0=pa[0:64, :], in1=RQb[b][0:64, :], op=ALU.mult),
                               reads=[pa, RQb[b]], writes=[QT])
                            op('dve', lambda: V.tensor_tensor(out=ta[64:96, :], in0=pa[64:96, :], in1=cr[64:96, :], op=ALU.mult),
                               reads=[pa, cr], writes=[ta])
                            op('dve', lambda: V.tensor_tensor(out=tb[64:96, :], in0=pb2[64:96, :], in1=sr[64:96, :], op=ALU.mult),
                               reads=[pb2, sr], writes=[tb])
                            op('dve', lambda: V.tensor_tensor(out=QT[64:96, h, gblk], in0=ta[64:96, :], in1=tb[64:96, :], op=ALU.add),
                               reads=[ta, tb], writes=[QT])
                        for h in range(4):
                            pa = PAp[h % 2]
                            op('pe', lambda: T.matmul(pa[0:64, :], lhsT=WK[:, h * 64:(h + 1) * 64], rhs=CKT[:, blk], start=True,
                                                      stop=True), reads=[WK, CKT], writes=[pa])
                            op('dve', lambda: V.tensor_tensor(out=KT[0:64, h, gblk], in0=pa[0:64, :], in1=RKb[b][0:64, :], op=ALU.mult),
                               reads=[pa, RKb[b]], writes=[KT])
                        op('dve', lambda: V.tensor_tensor(out=tk1[64:96, :], in0=KRR[64:96, 0, blk], in1=CS[64:96, 0, blk], op=ALU.mult),
                           reads=[KRR, CS], writes=[tk1])
                        op('dve', lambda: V.tensor_tensor(out=tk2[64:96, :], in0=KRR[64:96, 1, blk], in1=CS[64:96, 1, blk], op=ALU.mult),
                           reads=[KRR, CS], writes=[tk2])
                        for h in range(4):
                            op('dve', lambda: V.tensor_tensor(out=KT[64:96, h, gblk], in0=tk1[64:96, :], in1=tk2[64:96, :], op=ALU.add),
                               reads=[tk1, tk2], writes=[KT])
                        for r in range(4):
                            tl = m * 4 + r
                            pv = PBp[r % 2]
                            op('pe', lambda: T.matmul(pv[:, 0:256], lhsT=CKT[:, r * 128:(r + 1) * 128], rhs=WV[:], start=True,
                                                      stop=True), reads=[CKT, WV], writes=[pv])
                            op('dve', lambda: V.tensor_scalar(out=VD[:, tl, :, 0:64], in0=pv[:, 0:256].rearrange("p (h d) -> p h d", h=4),
                                                              scalar1=RKc[:, tl:tl + 1], scalar2=None, op0=ALU.mult),
                               reads=[pv, RKc], writes=[VD])
                    Sc.barrier()
                    sc_mla = 1.0 / math.sqrt(96.0)
                    ecnt = 0
                    import os as _os
                    for h in range(0 if _os.environ.get("MLA_STOP") == "prep" else 4):
                        for m in range(NG):
                            ob = Op[(h * NG + m) % 2]
                            nj = 4 * m + 4
                            first = True
                            for j in range(nj):
                                rlo = max(0, j - 4 * m)
                                tlo = rlo * 128
                                n = 512 - tlo
                                sp_ = STp[ecnt % 2]
                                eb_ = Eb[ecnt % 3]
                                ecnt += 1
                                op('pe', lambda: T.matmul(sp_[:, 0:n], lhsT=KT[:, h, j * 128:(j + 1) * 128],
                                                          rhs=QT[:, h, m * 512 + tlo:(m + 1) * 512], start=True, stop=True),
                                   reads=[KT, QT], writes=[sp_])
                                op('act', lambda: A.activation(out=eb_[:, 0:n], in_=sp_[:, 0:n], func=AF.Exp, scale=sc_mla),
                                   reads=[sp_], writes=[eb_])
                                if j >= 4 * m:
                                    op('pool', lambda: P.memset(eb_[64:128, 0:64], 0.0), writes=[eb_])
                                for r in range(rlo, 4):
                                    op('pe', lambda: T.matmul(ob[:, r * 65:(r + 1) * 65], lhsT=eb_[:, (r - rlo) * 128:(r - rlo + 1) * 128],
                                                              rhs=VD[:, j, h, :], start=first, stop=(j == 4 * m + r),
                                                              skip_group_check=True), reads=[eb_, VD], writes=[ob])
                                    first = False
                            ov = ob[:, 0:260].rearrange("p (r d) -> p r d", r=4)
                            yb = yst[(h * NG + m) % 2]
                            op('dve', lambda: V.reciprocal(out=rden[:], in_=ov[:, :, 64]), reads=[ob], writes=[rden])
                            op('dve', lambda: V.tensor_tensor(out=yb[:], in0=ov[:, :, 0:64],
                                                              in1=rden[:].unsqueeze(2).to_broadcast([128, 4, 64]), op=ALU.mult),
                               reads=[ob, rden], writes=[yb])
                            Sc.dma('pool', ybuf[m * 512:(m + 1) * 512, 768 + h * 64:768 + (h + 1) * 64].rearrange("(r p) d -> p r d", p=128),
                                   yb[:], reads=[yb])
                Sc.barrier()

            if want('oproj'):
                with ExitStack() as ph:
                    WO = sb(ph, "WO", [128, 8, D], BF16)
                    gg = sb(ph, "gg", [128, 8], F32)
                    stg = [sb(ph, f"stgo{i}", [128, D], F32) for i in range(2)]
                    yt = [sb(ph, f"yt{i}", [128, D], F32) for i in range(2)]
                    xt = [sb(ph, f"xt{i}", [128, D], F32) for i in range(2)]
                    junk = sb(ph, "junko", [128, D], F32)
                    tmp = sb(ph, "tmpo", [128, D], F32)
                    yb = sb(ph, "yb", [128, D], BF16)
                    mT = sb(ph, "mT", [128, 8, 128], BF16)
                    hb = sb(ph, "hbo", [128, D], BF16)
                    h2s = [sb(ph, f"h2s{i}", [128, 8, 128], BF16) for i in range(2)]
                    st = sb(ph, "sto", [128, 16], F32)
                    pT = [ps(ph, f"pTo{i}", BF16) for i in range(2)]
                    PO = [ps(ph, f"PO{i}") for i in range(4)]
                    Sc.dma('sp', gg[:], dram_ap(Wl['g_group'], 0, [[1, 128], [128, 8]]), writes=[gg], allow_slow_non_contiguous=True)
                    for kc in range(8):
                        b = kc % 2
                        Sc.dma('sp', stg[b][:], Wl['w_out'][kc * 128:(kc + 1) * 128, :], writes=[stg[b]])
                        if kc % 2 == 0:
                            op('dve', lambda: V.tensor_scalar(out=WO[:, kc, :], in0=stg[b][:], scalar1=gg[:, kc:kc + 1], scalar2=None,
                                                              op0=ALU.mult), reads=[stg[b], gg], writes=[WO])
                        else:
                            op('pool', lambda: P.tensor_scalar(out=WO[:, kc, :], in0=stg[b][:], scalar1=gg[:, kc:kc + 1], scalar2=None,
                                                               op0=ALU.mult), reads=[stg[b], gg], writes=[WO])
                    for i in range(NT):
                        b = i % 2
                        y_t, x_t = yt[b], xt[b]
                        Sc.dma('sp', y_t[:], ybuf[i * 128:(i + 1) * 128, :], writes=[y_t])
                        Sc.dma('sp', x_t[:], xin[i * 128:(i + 1) * 128, :], writes=[x_t])
                        for g in range(4):
                            op('act', lambda: A.activation(out=junk[:, 0:256], in_=y_t[:, g * 256:(g + 1) * 256], func=AF.Square,
                                                           scale=1.0 / 16.0, accum_out=st[:, g:g + 1]), reads=[y_t], writes=[junk, st],
                               multi=True)
                        op('act', lambda: A.activation(out=st[:, 4:8], in_=st[:, 0:4], func=AF.Sqrt, bias=epsT[:, 0:1], scale=1.0),
                           reads=[st, epsT], writes=[st])
                        op('dve', lambda: V.reciprocal(out=st[:, 8:12], in_=st[:, 4:8]), reads=[st], writes=[st])
                        for g in range(4):
                            if g % 2 == 0:
                                op('dve', lambda: V.tensor_scalar(out=yb[:, g * 256:(g + 1) * 256], in0=y_t[:, g * 256:(g + 1) * 256],
                                                                  scalar1=st[:, 8 + g:9 + g], scalar2=None, op0=ALU.mult),
                                   reads=[y_t, st], writes=[yb])
                            else:
                                op('pool', lambda: P.tensor_scalar(out=yb[:, g * 256:(g + 1) * 256], in0=y_t[:, g * 256:(g + 1) * 256],
                                                                   scalar1=st[:, 8 + g:9 + g], scalar2=None, op0=ALU.mult),
                                   reads=[y_t, st], writes=[yb])
                        for kc in range(8):
                            op('pe', lambda: T.transpose(out=pT[0][:, kc * 128:(kc + 1) * 128], in_=yb[:, kc * 128:(kc + 1) * 128],
                                                         identity=ident[:]), reads=[yb, ident], writes=[pT[0]])
                        op('act', lambda: A.copy(out=mT[:], in_=pT[0][:].rearrange("p (k t) -> p k t", k=8)), reads=[pT[0]], writes=[mT])
                        for nh in range(2):
                            po = PO[(i % 2) * 2 + nh]
                            for kc in range(8):
                                op('pe', lambda: T.matmul(po[:], lhsT=mT[:, kc, :], rhs=WO[:, kc, nh * 512:(nh + 1) * 512],
                                                          start=(kc == 0), stop=(kc == 7)), reads=[mT, WO], writes=[po])
                            hs = slice(nh * 512, (nh + 1) * 512)
                            op('dve', lambda: V.tensor_tensor(out=tmp[:, hs], in0=po[:], in1=MOD[:, 2, hs], op=ALU.mult),
                               reads=[po, MOD], writes=[tmp])
                            op('pool', lambda: P.tensor_tensor(out=x_t[:, hs], in0=x_t[:, hs], in1=tmp[:, hs], op=ALU.add),
                               reads=[x_t, tmp], writes=[x_t])
                        Sc.dma('pool', xout[i * 128:(i + 1) * 128, :], x_t[:], reads=[x_t])
                        op('act', lambda: A.activation(out=junk[:], in_=x_t[:], func=AF.Square, scale=1.0 / 32.0,
                                                       accum_out=st[:, 12:13]), reads=[x_t], writes=[junk, st], multi=True)
                        op('act', lambda: A.activation(out=st[:, 13:14], in_=st[:, 12:13], func=AF.Sqrt, bias=epsT[:, 0:1], scale=1.0),
                           reads=[st, epsT], writes=[st])
                        op('dve', lambda: V.reciprocal(out=st[:, 14:15], in_=st[:, 13:14]), reads=[st], writes=[st])
                        op('dve', lambda: V.scalar_tensor_tensor(out=tmp[:], in0=x_t[:], scalar=st[:, 14:15], in1=MOD[:, 4, :],
                                                                 op0=ALU.mult, op1=ALU.mult), reads=[x_t, st, MOD], writes=[tmp])
                        op('pool', lambda: P.tensor_tensor(out=hb[:], in0=tmp[:], in1=MOD[:, 3, :], op=ALU.add),
                           reads=[tmp, MOD], writes=[hb])
                        for kc in range(8):
                            op('pe', lambda: T.transpose(out=pT[1][:, kc * 128:(kc + 1) * 128], in_=hb[:, kc * 128:(kc + 1) * 128],
                                                         identity=ident[:]), reads=[hb, ident], writes=[pT[1]])
                        op('act', lambda: A.copy(out=h2s[b][:], in_=pT[1][:].rearrange("p (k t) -> p k t", k=8)),
                           reads=[pT[1]], writes=[h2s[b]])
                        Sc.dma('pool', h2T[:, i * 128:(i + 1) * 128].rearrange("(c p) t -> p c t", p=128), h2s[b][:], reads=[h2s[b]])
                Sc.barrier()

            if want('ffn'):
                for fp in range(2):
                    with ExitStack() as ph:
                        W1 = sb(ph, "W1", [128, 8, FH], BF16)
                        W3 = sb(ph, "W3", [128, 8, FH], BF16)
                        W2 = sb(ph, "W2", [128, NFC, D], BF16)
                        stg = [sb(ph, f"stgf{i}", [128, FH], F32) for i in range(2)]
                        H2 = [sb(ph, f"H2{i}", [128, 8, 512], BF16) for i in range(2)]
                        AT = sb(ph, "AT", [128, NFC, 512], BF16)
                        sl = [sb(ph, f"sl{i}", [128, 512], F32) for i in range(2)]
                        xt = [sb(ph, f"xtf{i}", [128, D], F32) for i in range(2)]
                        tmp = sb(ph, "tmpf", [128, D], F32)
                        ost = [sb(ph, f"ost{i}", [128, D], F32) for i in range(2)]
                        junk = sb(ph, "junkf", [128, D], F32)
                        gfb = sb(ph, "gfb", [128, D], F32)
                        st = sb(ph, "stf", [128, 4], F32)
                        G1p = [ps(ph, f"G1p{i}") for i in range(2)]
                        G3p = [ps(ph, f"G3p{i}") for i in range(2)]
                        PD = [ps(ph, f"PD{i}") for i in range(4)]
                        f0 = fp * FH
                        for kc in range(8):
                            b = kc % 2
                            Sc.dma('sp', stg[b][:], Wl['w1'][kc * 128:(kc + 1) * 128, f0:f0 + FH], writes=[stg[b]])
                            cast(W1[:, kc, :], stg[b][:], [stg[b]], [W1])
                        for kc in range(8):
                            b = kc % 2
                            Sc.dma('sp', stg[b][:], Wl['w3'][kc * 128:(kc + 1) * 128, f0:f0 + FH], writes=[stg[b]])
                            cast(W3[:, kc, :], stg[b][:], [stg[b]], [W3])
                        for fc in range(NFC):
                            b = fc % 2
                            Sc.dma('sp', stg[b][:, 0:D], Wl['w2'][f0 + fc * 128:f0 + (fc + 1) * 128, :], writes=[stg[b]])
                            cast(W2[:, fc, :], stg[b][:, 0:D], [stg[b]], [W2])
                        if last and fp == 1:
                            Sc.dma('sp', gfb[:], gfin_in[0].partition_broadcast(128), writes=[gfb])
                        for g in range(NG):
                            hh = H2[g % 2]
                            Sc.dma('sp', hh[:], h2T[:, g * 512:(g + 1) * 512].rearrange("(c p) t -> p c t", p=128), writes=[hh])
                            for fc in range(NFC):
                                g1, g3, s_ = G1p[fc % 2], G3p[fc % 2], sl[fc % 2]
                                for kc in range(8):
                                    op('pe', lambda: T.matmul(g1[:], lhsT=W1[:, kc, fc * 128:(fc + 1) * 128], rhs=hh[:, kc, :],
                                                              start=(kc == 0), stop=(kc == 7)), reads=[W1, hh], writes=[g1])
                                for kc in range(8):
                                    op('pe', lambda: T.matmul(g3[:], lhsT=W3[:, kc, fc * 128:(fc + 1) * 128], rhs=hh[:, kc, :],
                                                              start=(kc == 0), stop=(kc == 7)), reads=[W3, hh], writes=[g3])
                                op('act', lambda: A.activation(out=s_[:], in_=g1[:], func=AF.Silu), reads=[g1], writes=[s_])
                                op('dve', lambda: V.tensor_tensor(out=AT[:, fc, :], in0=s_[:], in1=g3[:], op=ALU.mult),
                                   reads=[s_, g3], writes=[AT])
                            for r in range(4):
                                i = g * 4 + r
                                x_t = xt[i % 2]
                                Sc.dma('sp', x_t[:], xout[i * 128:(i + 1) * 128, :], writes=[x_t])
                                for nh in range(2):
                                    pd = PD[(i % 2) * 2 + nh]
                                    hs = slice(nh * 512, (nh + 1) * 512)
                                    for fc in range(NFC):
                                        op('pe', lambda: T.matmul(pd[:], lhsT=AT[:, fc, r * 128:(r + 1) * 128], rhs=W2[:, fc, hs],
                                                                  start=(fc == 0), stop=(fc == NFC - 1)), reads=[AT, W2], writes=[pd])
                                    op('dve', lambda: V.tensor_tensor(out=tmp[:, hs], in0=pd[:], in1=MOD[:, 5, hs], op=ALU.mult),
                                       reads=[pd, MOD], writes=[tmp])
                                    op('pool', lambda: P.tensor_tensor(out=x_t[:, hs], in0=x_t[:, hs], in1=tmp[:, hs], op=ALU.add),
                                       reads=[x_t, tmp], writes=[x_t])
                                Sc.dma('pool', xout[i * 128:(i + 1) * 128, :], x_t[:], reads=[x_t])
                                if last and fp == 1:
                                    o_t = ost[i % 2]
                                    op('act', lambda: A.activation(out=junk[:], in_=x_t[:], func=AF.Square, scale=1.0 / 32.0,
                                                                   accum_out=st[:, 0:1]), reads=[x_t], writes=[junk, st], multi=True)
                                    op('act', lambda: A.activation(out=st[:, 1:2], in_=st[:, 0:1], func=AF.Sqrt, bias=epsT[:, 0:1],
                                                                   scale=1.0), reads=[st, epsT], writes=[st])
                                    op('dve', lambda: V.reciprocal(out=st[:, 2:3], in_=st[:, 1:2]), reads=[st], writes=[st])
                                    op('dve', lambda: V.scalar_tensor_tensor(out=o_t[:], in0=x_t[:], scalar=st[:, 2:3], in1=gfb[:],
                                                                             op0=ALU.mult, op1=ALU.mult), reads=[x_t, st, gfb], writes=[o_t])
                                    Sc.dma('pool', out[seq, i * 128:(i + 1) * 128, :], o_t[:], reads=[o_t])
                    Sc.barrier()
        Sc.barrier()
        print("program instructions:", Sc.ninstr, Sc.cnt)
    return nc


_PROG_CACHE = {}


def _get_prog(S, nseq, nlayer, steps):
    key = (S, nseq, nlayer, tuple(steps))
    if key not in _PROG_CACHE:
        _PROG_CACHE[key] = build(S, nseq, nlayer, list(steps))
    return _PROG_CACHE[key]


def kernel(**inputs):
    x = np.asarray(inputs['x'], dtype=np.float32)
    c = np.asarray(inputs['c'], dtype=np.float32)
    B, S, _ = x.shape
    L = np.asarray(inputs['w_mod']).shape[0]
    per = B // N_CORES
    consts = host_consts(S)
    t5 = np.ascontiguousarray(np.asarray(inputs['t5_table'], dtype=np.float32))
    gfin = np.ascontiguousarray(np.asarray(inputs['g_final'], dtype=np.float32)).reshape(1, -1)
    lw = [layout_weights(inputs, l) for l in range(L)]
    steps = tuple((sl, l, l == L - 1) for l in range(L) for sl in range(per))
    nc = _get_prog(S, per, L, steps)
    in_maps = []
    for core in range(N_CORES):
        m = dict(x=np.ascontiguousarray(x[core * per:(core + 1) * per]),
                 c=np.ascontiguousarray(c[core * per:(core + 1) * per]), t5=t5, g_final=gfin)
        for l in range(L):
            m.update({f"{k}_{l}": v for k, v in lw[l].items()})
        m.update(consts)
        in_maps.append(m)
    res = run_bass_kernel_spmd(nc, in_maps, core_ids=list(range(N_CORES)))
    full = np.empty((B, S, D), np.float32)
    for core in range(N_CORES):
        full[core * per:(core + 1) * per] = res.results[core]["out"]
    return full
```

```python
import math
from contextlib import ExitStack

import numpy as np
import ml_dtypes
import concourse.bass as bass
import concourse.mybir as mybir
from concourse.bass_types import AP
from concourse.bass_utils import run_bass_kernel_spmd

AF = mybir.ActivationFunctionType
ALU = mybir.AluOpType
AX = mybir.AxisListType
F32 = mybir.dt.float32
BF16 = mybir.dt.bfloat16

D = 1024
DFF = 2816
FH = DFF // 2
NFC = FH // 128
EPS = 1e-6
TMW = 968
FMW = 2048
NBIS = 12
TOPK = 256
NEG = -1.0e30
EPOCH = 16000
N_CORES = 8


class Buf:
    def __init__(self, h, name):
        self.h = h
        self.name = name
        self.w = None
        self.r = {}

    def __getitem__(self, idx):
        return self.h[idx]


class Sched:
    ENG = ('pe', 'act', 'dve', 'pool', 'sp')

    def __init__(self, nc, es):
        self.nc = nc
        self.es = es
        self.engs = {'pe': nc.tensor, 'act': nc.scalar, 'dve': nc.vector, 'pool': nc.gpsimd, 'sp': nc.sync}
        self.sems = {}
        self.cnt = {k: 0 for k in self.ENG}
        self.seen = {k: {} for k in self.ENG}
        self.dq = {}
        for q, n in (('sp', 10), ('pool', 6), ('act', 4)):
            self.dq[q] = dict(sems=[es.enter_context(nc.semaphore(f"dma_{q}{i}")) for i in range(n)],
                              cnt=[0] * n, i=0)
        self.ninstr = 0

    def _sem(self, key, n):
        ep = n // EPOCH
        k = (key, ep)
        if k not in self.sems:
            self.sems[k] = self.es.enter_context(self.nc.semaphore(f"s_{key}_{ep}"))
        return self.sems[k], n % EPOCH + 1

    def _semval(self, ev):
        key, n = ev
        if isinstance(key, tuple):
            return self.dq[key[1]]['sems'][key[2]], 16 * (n + 1)
        return self._sem(key, n)

    def _need(self, eng, evs):
        out = {}
        for key, n in evs:
            if self.seen[eng].get(key, -1) >= n:
                continue
            if out.get(key, -1) < n:
                out[key] = n
        return list(out.items())

    def _emit_waits(self, eng, evs, ins_fn=None):
        need = self._need(eng, evs)
        for key, n in need:
            self.seen[eng][key] = n
        if ins_fn is None:
            for ev in need:
                sem, v = self._semval(ev)
                self.engs[eng].wait_ge(sem, v)
                self.ninstr += 1
            return None
        for ev in need[:-1]:
            sem, v = self._semval(ev)
            self.engs[eng].wait_ge(sem, v)
            self.ninstr += 1
        ins = ins_fn()
        if need:
            sem, v = self._semval(need[-1])
            ins._wait_ge(sem, v)
        return ins

    def _deps(self, eng, reads, writes):
        evs = []
        for b in reads:
            if b.w is not None:
                evs.append(b.w)
        for b in writes:
            if b.w is not None and b.w[0] != eng:
                evs.append(b.w)
            for k, n in b.r.items():
                if k != eng:
                    evs.append((k, n))
        return evs

    def op(self, eng, fn, reads=(), writes=(), multi=False):
        evs = self._deps(eng, reads, writes)
        if multi or eng == 'pe':
            self._emit_waits(eng, evs)
            ins = fn()
        else:
            ins = self._emit_waits(eng, evs, fn)
        n = self.cnt[eng]
        self.cnt[eng] += 1
        sem, _ = self._sem(eng, n)
        ins.then_inc(sem, 1)
        self.ninstr += 1
        for b in reads:
            b.r[eng] = n
        for b in writes:
            b.w = (eng, n)
            b.r = {}
        return ins

    def dma(self, q, out_ap, in_ap, reads=(), writes=(), **kw):
        dq = self.dq[q]
        i = dq['i'] % len(dq['sems'])
        dq['i'] += 1
        key = ('dma', q, i)
        evs = self._deps(q, reads, writes)
        if dq['cnt'][i] > 0:
            evs.append((key, dq['cnt'][i] - 1))
        self._emit_waits(q, evs)
        ins = self.engs[q].dma_start(out=out_ap, in_=in_ap, **kw)
        ins.then_inc(dq['sems'][i], 16)
        n = dq['cnt'][i]
        dq['cnt'][i] += 1
        self.ninstr += 1
        for b in reads:
            b.r[key] = n
        for b in writes:
            b.w = (key, n)
            b.r = {}

    def barrier(self):
        evs = []
        for k in self.ENG:
            if k != 'sp' and self.cnt[k] > 0:
                evs.append((k, self.cnt[k] - 1))
        for q, dq in self.dq.items():
            for i, c in enumerate(dq['cnt']):
                if c > 0:
                    evs.append((('dma', q, i), c - 1))
        self._emit_waits('sp', evs)
        ins = self.nc.sync.nop()
        n = self.cnt['sp']
        self.cnt['sp'] += 1
        sem, _ = self._sem('sp', n)
        ins.then_inc(sem, 1)
        self.ninstr += 1
        for k in self.ENG:
            if k != 'sp':
                self._emit_waits(k, [('sp', n)])
                for k2 in self.ENG:
                    if self.cnt[k2] > 0:
                        self.seen[k][k2] = max(self.seen[k].get(k2, -1), self.cnt[k2] - 1 if k2 != 'sp' else n)
                for q, dq in self.dq.items():
                    for i, c in enumerate(dq['cnt']):
                        if c > 0:
                            self.seen[k][('dma', q, i)] = c - 1


def bc_mid(ap, n):
    a = [list(x) for x in ap.ap]
    return AP(tensor=ap.tensor, offset=ap.offset, ap=[a[0], [0, n]] + a[1:])


def dram_ap(t, offset, pairs):
    return AP(tensor=t.tensor, offset=offset, ap=[list(p) for p in pairs])


def _t5_bucket_np(rel):
    nb = 16
    max_exact = 8
    ret = np.where(rel > 0, nb, 0)
    n = np.abs(rel)
    nf = np.maximum(n, 1).astype(np.float32)
    large = max_exact + (np.log(nf / np.float32(max_exact)) / np.float32(math.log(128 / max_exact))
                         * np.float32(nb - max_exact)).astype(np.int32)
    large = np.minimum(large, nb - 1)
    return ret + np.where(n < max_exact, n, large)


def host_consts(S):
    bf = ml_dtypes.bfloat16
    pa = np.zeros((128, 12, 128), np.float32)
    s = np.arange(128)[:, None]
    t = np.arange(128)[None, :]
    for gi, w in enumerate((2, 4, 8, 16)):
        inwin = ((s <= t) & (s > t - w)).astype(np.float32)
        pa[:, gi, :] = inwin / w - (s == t)
        cnt = np.minimum(t + 1, w).astype(np.float32)
        pa[:, 8 + gi, :] = inwin / cnt - (s == t)
        inprev = ((s - 128) > t - w).astype(np.float32)
        pa[:, 4 + gi, :] = inprev / w
    bm = np.zeros((128, 5, 128), np.float32)
    for d in range(5):
        diff = 2 * d + (t // 64) - (s // 64)
        bm[:, d, :] = ((diff >= 0) & (diff <= 8)).astype(np.float32)
    m = np.arange(384)
    bk = _t5_bucket_np((127 - m).astype(np.int32))
    oh = np.zeros((32, 384), np.float32)
    oh[bk, m] = 1.0
    half = 16
    freqs = (10000.0 ** (-np.arange(half, dtype=np.float32) / half)).astype(np.float32)
    ang = np.arange(S, dtype=np.float32)[None, :] * freqs[:, None]
    cs = np.zeros((2, 32, S), np.float32)
    cs[0, :16] = np.cos(ang)
    cs[0, 16:] = np.cos(ang)
    cs[1, :16] = -np.sin(ang)
    cs[1, 16:] = np.sin(ang)
    p2 = np.tile((0.5 ** np.arange(1, NBIS + 1, dtype=np.float32))[None, :], (128, 1)).astype(np.float32)
    return dict(c_poolA=pa.astype(bf), c_bandm=bm.astype(bf), c_t5oh=oh, c_cs=cs.astype(bf), c_pow2=p2)


def _in_cols():
    o = {}
    off = 0
    for n, w in (('pool_u', 256), ('ca_q', 256), ('ca_k', 256), ('ca_v', 256), ('sa_q', 256), ('sa_k', 64),
                 ('sa_v', 64), ('idx_q', 512), ('idx_k', 64), ('idx_w', 8), ('mla_cq', 256), ('mla_ckv', 128),
                 ('mla_kr', 32)):
        o[n] = np.arange(off, off + w)
        off += w
    return o


def layout_weights(inp, l):
    o = _in_cols()
    w_in = np.asarray(inp['w_in'][l])
    tm = np.concatenate([o['pool_u'], o['ca_v'], o['sa_v'], o['idx_w'], o['mla_cq'], o['mla_ckv']])
    w_tm = np.ascontiguousarray(w_in[:, tm])
    w_fm = np.zeros((D, FMW), np.float32)
    fm = np.concatenate([o['ca_q'], o['ca_k'], o['sa_q'], o['idx_q'], o['sa_k'], o['idx_k'], o['mla_cq'], o['mla_ckv']])
    w_fm[:, :fm.size] = w_in[:, fm]
    kr = o['mla_kr']
    w_fm[:, 1792 + 64:1792 + 96] = w_in[:, kr]
    w_fm[:, 1920 + 64:1920 + 96] = w_in[:, np.concatenate([kr[16:], kr[:16]])]
    wuq = np.asarray(inp['mla_w_uq'][l])
    wqa = np.ascontiguousarray(wuq.reshape(256, 384))
    wqb = np.zeros((256, 4, 96), np.float32)
    wqb[:, :, 64:80] = wuq[:, :, 80:96]
    wqb[:, :, 80:96] = wuq[:, :, 64:80]
    wukv = np.asarray(inp['mla_w_ukv'][l])
    wk = np.ascontiguousarray(wukv[:, :, :64].reshape(128, 256))
    wv = np.ascontiguousarray(wukv[:, :, 64:].reshape(128, 256))
    f = lambda a: np.ascontiguousarray(np.asarray(a), dtype=np.float32)
    return dict(
        w_mod=f(inp['w_mod'][l]), b_mod=f(inp['b_mod'][l]).reshape(1, -1), g_mix=f(inp['g_mix'][l]).reshape(1, -1),
        w_tm=w_tm, w_fm=w_fm, pool_w=f(inp['pool_w'][l]), pool_scale=f(inp['pool_scale'][l]).reshape(1, -1),
        ca_rel=f(inp['ca_rel'][l]), g_cq=f(inp['mla_g_cq'][l]).reshape(1, -1), g_ckv=f(inp['mla_g_ckv'][l]).reshape(1, -1),
        wqa=wqa, wqb=wqb.reshape(256, 384), wk=wk, wv=wv, g_group=f(inp['g_group'][l]).reshape(1, -1),
        w_out=f(inp['w_out'][l]), g_ffn=f(inp['g_ffn'][l]).reshape(1, -1), w1=f(inp['ffn_w1'][l]),
        w3=f(inp['ffn_w3'][l]), w2=f(inp['ffn_w2'][l]))


WSHAPES = dict(w_mod=[D, 6 * D], b_mod=[1, 6 * D], g_mix=[1, D], w_tm=[D, TMW], w_fm=[D, FMW], pool_w=[4, 64, 64],
               pool_scale=[1, 256], ca_rel=[4, 320], g_cq=[1, 256], g_ckv=[1, 128], wqa=[256, 384], wqb=[256, 384],
               wk=[128, 256], wv=[128, 256], g_group=[1, D], w_out=[D, D], g_ffn=[1, D], w1=[D, DFF], w3=[D, DFF],
               w2=[DFF, D])


def build(S, nseq, nlayer, steps, debug=False, phases=None):
    NT = S // 128
    NG = S // 512
    nc = bass.Bass("TRN2", target_bir_lowering=False)
    es = ExitStack()
    with es:
        def dram(name, shape, dt, kind="Internal"):
            return nc.dram_tensor(name, shape, dt, kind=kind).ap()

        x_in = dram("x", [nseq, S, D], F32, "ExternalInput")
        c_in = dram("c", [nseq, D], F32, "ExternalInput")
        t5_in = dram("t5", [32, 4], F32, "ExternalInput")
        gfin_in = dram("g_final", [1, D], F32, "ExternalInput")
        W = []
        for l in range(nlayer):
            W.append({k: dram(f"{k}_{l}", shp, F32, "ExternalInput") for k, shp in WSHAPES.items()})
        c_poolA = dram("c_poolA", [128, 12, 128], BF16, "ExternalInput")
        c_bandm = dram("c_bandm", [128, 5, 128], BF16, "ExternalInput")
        c_t5oh = dram("c_t5oh", [32, 384], F32, "ExternalInput")
        c_cs = dram("c_cs", [2, 32, S], BF16, "ExternalInput")
        c_pow2 = dram("c_pow2", [128, NBIS], F32, "ExternalInput")
        xo = dram("xo", [nseq, S, D], F32, "ExternalOutput")
        out = dram("out", [nseq, S, D], F32, "ExternalOutput")
        dk = "ExternalOutput" if debug else "Internal"
        ztm = dram("ztm", [S, 576], BF16, dk)
        zsm = dram("zsm", [S, 8], F32, dk)
        zrq = dram("zrq", [1, S], F32, dk)
        zrk = dram("zrk", [1, S], F32, dk)
        zfm = dram("zfm", [FMW, S], BF16, dk)
        ybuf = dram("ybuf", [S, D], F32, dk)
        h2T = dram("h2T", [D, S], BF16, dk)
        ext = dram("ext", [4, 768], F32, "Internal")
        t5v = dram("t5v", [4, 384], F32, "Internal")

        Sc = Sched(nc, es)
        op = Sc.op
        V, A, P, T = nc.vector, nc.scalar, nc.gpsimd, nc.tensor

        uid = [0]

        def sb(st, name, shape, dt):
            uid[0] += 1
            name = f"{name}_{uid[0]}"
            return Buf(st.enter_context(nc.sbuf_tensor(name, shape, dt)), name)

        def ps(st, name, dt=F32):
            uid[0] += 1
            name = f"{name}_{uid[0]}"
            shape = [128, 512] if dt == F32 else [128, 1024]
            return Buf(st.enter_context(nc.psum_tensor(name, shape, dt)), name)

        def want(p):
            return phases is None or p in phases

        ident = sb(es, "ident", [128, 128], BF16)
        identf = sb(es, "identf", [128, 128], F32)
        onesf = sb(es, "onesf", [128, 128], F32)
        epsT = sb(es, "epsT", [128, 1], F32)
        MOD = sb(es, "MOD", [128, 6, D], F32)
        op('pool', lambda: P.memset(identf[:], 1.0), writes=[identf])
        op('pool', lambda: P.affine_select(out=identf[:], in_=identf[:], pattern=[[-1, 128]], compare_op=ALU.is_equal,
                                           fill=0.0, base=0, channel_multiplier=1), reads=[identf], writes=[identf])
        op('dve', lambda: V.tensor_copy(out=ident[:], in_=identf[:]), reads=[identf], writes=[ident])
        Jf = sb(es, "Jf", [128, 128], F32)
        op('pool', lambda: P.memset(Jf[:], 1.0), writes=[Jf])
        op('pool', lambda: P.affine_select(out=Jf[:], in_=Jf[:], pattern=[[1, 128]], compare_op=ALU.is_equal,
                                           fill=0.0, base=-127, channel_multiplier=1), reads=[Jf], writes=[Jf])
        op('dve', lambda: V.memset(onesf[:], 1.0), writes=[onesf])
        op('dve', lambda: V.memset(epsT[:], EPS), writes=[epsT])

        rr = [0]

        def cast(out_ap, in_ap, reads, writes):
            k = rr[0] % 3
            rr[0] += 1
            if k == 0:
                op('pool', lambda: P.tensor_copy(out=out_ap, in_=in_ap), reads=reads, writes=writes)
            elif k == 1:
                op('act', lambda: A.copy(out=out_ap, in_=in_ap), reads=reads, writes=writes)
            else:
                op('dve', lambda: V.tensor_copy(out=out_ap, in_=in_ap), reads=reads, writes=writes)

        def rstd_from(ms, sq, rs, n=1):
            op('act', lambda: A.activation(out=sq[:, 0:n], in_=ms[:, 0:n], func=AF.Sqrt, bias=epsT[:, 0:1], scale=1.0),
               reads=[ms, epsT], writes=[sq])
            op('dve', lambda: V.reciprocal(out=rs[:, 0:n], in_=sq[:, 0:n]), reads=[sq], writes=[rs])

        for (seq, l, last) in steps:
            Wl = W[l]
            xin = x_in[seq] if l == 0 else xo[seq]
            xout = xo[seq]

            if want('mod'):
                with ExitStack() as ph:
                    cT = sb(ph, "cT", [128, 8], F32)
                    cact = sb(ph, "cact", [128, 8], F32)
                    cbc = sb(ph, "cbc", [128, 8, 128], F32)
                    wst = [sb(ph, f"wst{i}", [128, 8, 512], F32) for i in range(2)]
                    bst = [sb(ph, f"bst{i}", [128, 512], F32) for i in range(2)]
                    gbc = [sb(ph, f"gbc{i}", [128, D], F32) for i in range(2)]
                    pm = [ps(ph, f"pm{i}") for i in range(2)]
                    Sc.dma('sp', cT[:], dram_ap(c_in, seq * D, [[1, 128], [128, 8]]), writes=[cT],
                           allow_slow_non_contiguous=True)
                    Sc.dma('sp', gbc[0][:], Wl['g_mix'][0].partition_broadcast(128), writes=[gbc[0]])
                    Sc.dma('sp', gbc[1][:], Wl['g_ffn'][0].partition_broadcast(128), writes=[gbc[1]])
                    op('act', lambda: A.activation(out=cact[:], in_=cT[:], func=AF.Silu), reads=[cT], writes=[cact])
                    for kc in range(8):
                        op('dve', lambda: V.tensor_scalar(out=cbc[:, kc, :], in0=onesf[:], scalar1=cact[:, kc:kc + 1],
                                                          scalar2=None, op0=ALU.mult), reads=[onesf, cact], writes=[cbc])
                    MODf = MOD[:].rearrange("p a d -> p (a d)")
                    for n in range(12):
                        b = n % 2
                        Sc.dma('sp', wst[b][:], Wl['w_mod'][:, n * 512:(n + 1) * 512].rearrange("(k p) n -> p k n", p=128),
                               writes=[wst[b]])
                        Sc.dma('sp', bst[b][:], Wl['b_mod'][0, n * 512:(n + 1) * 512].partition_broadcast(128),
                               writes=[bst[b]])
                        for kc in range(8):
                            op('pe', lambda: T.matmul(pm[b][:], lhsT=cbc[:, kc, :], rhs=wst[b][:, kc, :], start=(kc == 0),
                                                      stop=(kc == 7)), reads=[cbc, wst[b]], writes=[pm[b]])
                        op('dve', lambda: V.tensor_tensor(out=MODf[:, n * 512:(n + 1) * 512], in0=pm[b][:], in1=bst[b][:],
                                                          op=ALU.add), reads=[pm[b], bst[b]], writes=[MOD])
                    for (j, g) in ((1, 0), (4, 1)):
                        op('dve', lambda: V.scalar_tensor_tensor(out=MOD[:, j, :], in0=MOD[:, j, :], scalar=1.0,
                                                                 in1=gbc[g][:], op0=ALU.add, op1=ALU.mult),
                           reads=[MOD, gbc[g]], writes=[MOD])
                Sc.barrier()

            if want('inproj'):
                with ExitStack() as ph:
                    WTM = sb(ph, "WTM", [128, 8, TMW], BF16)
                    WFM = sb(ph, "WFM", [128, 8, FMW], BF16)
                    stg = [sb(ph, f"stg{i}", [128, FMW], F32) for i in range(2)]
                    xb = [sb(ph, f"xb{i}", [128, D], F32) for i in range(2)]
                    t1 = sb(ph, "t1", [128, D], F32)
                    junk = sb(ph, "junk", [128, D], F32)
                    hbs = [sb(ph, f"hb{i}", [128, D], BF16) for i in range(2)]
                    hT4 = [sb(ph, f"hT4{i}", [128, 8, 512], BF16) for i in range(2)]
                    sts = [sb(ph, f"st{i}", [128, 8], F32) for i in range(2)]
                    stm = [sb(ph, f"stm{i}", [128, 576], BF16) for i in range(2)]
                    ssm = [sb(ph, f"ssm{i}", [128, 16], F32) for i in range(2)]
                    sfm = [sb(ph, f"sfm{i}", [128, 16, 512], BF16) for i in range(2)]
                    pT = ps(ph, "pT", BF16)
                    ptm = [ps(ph, f"ptm{i}") for i in range(2)]
                    pfm = [ps(ph, f"pfm{i}") for i in range(3)]
                    for kc in range(8):
                        b = kc % 2
                        Sc.dma('sp', stg[b][:, 0:TMW], Wl['w_tm'][kc * 128:(kc + 1) * 128, :], writes=[stg[b]])
                        cast(WTM[:, kc, :], stg[b][:, 0:TMW], [stg[b]], [WTM])
                    for kc in range(8):
                        b = kc % 2
                        Sc.dma('sp', stg[b][:], Wl['w_fm'][kc * 128:(kc + 1) * 128, :], writes=[stg[b]])
                        cast(WFM[:, kc, :], stg[b][:], [stg[b]], [WFM])
                    def pa_s1(i):
                        x_t = xb[i % 2]
                        hb = hbs[i % 2]
                        st = sts[i % 2]
                        Sc.dma('sp', x_t[:], xin[i * 128:(i + 1) * 128, :], writes=[x_t])
                        op('act', lambda: A.activation(out=junk[:], in_=x_t[:], func=AF.Square, scale=1.0 / 32.0,
                                                       accum_out=st[:, 0:1]), reads=[x_t], writes=[junk, st], multi=True)
                        op('act', lambda: A.activation(out=st[:, 1:2], in_=st[:, 0:1], func=AF.Sqrt, bias=epsT[:, 0:1],
                                                       scale=1.0), reads=[st, epsT], writes=[st])
                        op('dve', lambda: V.reciprocal(out=st[:, 2:3], in_=st[:, 1:2]), reads=[st], writes=[st])
                        op('dve', lambda: V.scalar_tensor_tensor(out=t1[:], in0=x_t[:], scalar=st[:, 2:3], in1=MOD[:, 1, :],
                                                                 op0=ALU.mult, op1=ALU.mult), reads=[x_t, st, MOD], writes=[t1])
                        op('dve', lambda: V.tensor_tensor(out=hb[:], in0=t1[:], in1=MOD[:, 0, :], op=ALU.add),
                           reads=[t1, MOD], writes=[hb])

                    def pa_s2(i):
                        g, r = i // 4, i % 4
                        hT = hT4[g % 2]
                        hb = hbs[i % 2]
                        for kc in range(8):
                            op('pe', lambda: T.transpose(out=pT[:, kc * 128:(kc + 1) * 128], in_=hb[:, kc * 128:(kc + 1) * 128],
                                                         identity=ident[:]), reads=[hb, ident], writes=[pT])
                        op('act', lambda: A.copy(out=hT[:, :, r * 128:(r + 1) * 128],
                                                 in_=pT[:].rearrange("p (k t) -> p k t", k=8)), reads=[pT], writes=[hT])
                        for kc in range(8):
                            op('pe', lambda: T.matmul(ptm[0][:], lhsT=hT[:, kc, r * 128:(r + 1) * 128], rhs=WTM[:, kc, 0:512],
                                                      start=(kc == 0), stop=(kc == 7)), reads=[hT, WTM], writes=[ptm[0]])
                        for kc in range(8):
                            op('pe', lambda: T.matmul(ptm[1][:, 0:456], lhsT=hT[:, kc, r * 128:(r + 1) * 128],
                                                      rhs=WTM[:, kc, 512:968], start=(kc == 0), stop=(kc == 7)),
                               reads=[hT, WTM], writes=[ptm[1]])
                        sm_, ss_ = stm[i % 2], ssm[i % 2]
                        op('act', lambda: A.copy(out=sm_[:, 0:512], in_=ptm[0][:]), reads=[ptm[0]], writes=[sm_])
                        op('dve', lambda: V.tensor_copy(out=sm_[:, 512:576], in_=ptm[1][:, 0:64]), reads=[ptm[1]], writes=[sm_])
                        op('dve', lambda: V.tensor_copy(out=ss_[:, 0:8], in_=ptm[1][:, 64:72]), reads=[ptm[1]], writes=[ss_])
                        op('act', lambda: A.activation(out=junk[:, 0:256], in_=ptm[1][:, 72:328], func=AF.Square,
                                                       scale=1.0 / 16.0, accum_out=ss_[:, 8:9]),
                           reads=[ptm[1]], writes=[junk, ss_], multi=True)
                        op('act', lambda: A.activation(out=junk[:, 0:128], in_=ptm[1][:, 328:456], func=AF.Square,
                                                       scale=1.0 / math.sqrt(128.0), accum_out=ss_[:, 9:10]),
                           reads=[ptm[1]], writes=[junk, ss_], multi=True)
                        op('act', lambda: A.activation(out=ss_[:, 10:12], in_=ss_[:, 8:10], func=AF.Sqrt, bias=epsT[:, 0:1],
                                                       scale=1.0), reads=[ss_, epsT], writes=[ss_])
                        op('dve', lambda: V.reciprocal(out=ss_[:, 12:14], in_=ss_[:, 10:12]), reads=[ss_], writes=[ss_])
                        Sc.dma('pool', ztm[i * 128:(i + 1) * 128, :], sm_[:], reads=[sm_])
                        Sc.dma('pool', zsm[i * 128:(i + 1) * 128, :], ss_[:, 0:8], reads=[ss_])
                        Sc.dma('pool', dram_ap(zrq, i * 128, [[1, 128], [1, 1]]), ss_[:, 12:13], reads=[ss_])
                        Sc.dma('pool', dram_ap(zrk, i * 128, [[1, 128], [1, 1]]), ss_[:, 13:14], reads=[ss_])
                        if r == 3:
                            sf = sfm[g % 2]
                            for ch in range(16):
                                pf = pfm[ch % 3]
                                for kc in range(8):
                                    op('pe', lambda: T.matmul(pf[:], lhsT=WFM[:, kc, ch * 128:(ch + 1) * 128], rhs=hT[:, kc, :],
                                                              start=(kc == 0), stop=(kc == 7)), reads=[WFM, hT], writes=[pf])
                                if ch % 2 == 0:
                                    op('act', lambda: A.copy(out=sf[:, ch, :], in_=pf[:]), reads=[pf], writes=[sf])
                                else:
                                    op('dve', lambda: V.tensor_copy(out=sf[:, ch, :], in_=pf[:]), reads=[pf], writes=[sf])
                            Sc.dma('pool', zfm[:, g * 512:(g + 1) * 512].rearrange("(c p) t -> p c t", p=128), sf[:], reads=[sf])
                    pa_s1(0)
                    for i in range(NT):
                        if i + 1 < NT:
                            pa_s1(i + 1)
                        pa_s2(i)
                Sc.barrier()

            if want('pool'):
                with ExitStack() as ph:
                    U = sb(ph, "U", [128, NT, 256], BF16)
                    PA = sb(ph, "PA", [128, 12, 128], BF16)
                    pwf = sb(ph, "pwf", [64, 4, 64], F32)
                    scb = sb(ph, "scb", [64, 256], F32)
                    PW = sb(ph, "PW", [64, 4, 64], BF16)
                    dT = [sb(ph, f"dT{i}", [64, 4, 128], BF16) for i in range(2)]
                    yst = [sb(ph, f"yst{i}", [128, 256], F32) for i in range(2)]
                    pd = [ps(ph, f"pd{i}") for i in range(2)]
                    py = [ps(ph, f"py{i}") for i in range(2)]
                    Sc.dma('sp', U[:], ztm[:, 0:256].rearrange("(n p) c -> p n c", p=128), writes=[U])
                    Sc.dma('sp', PA[:], c_poolA, writes=[PA])
                    Sc.dma('sp', pwf[:], Wl['pool_w'].rearrange("g c d -> c g d"), writes=[pwf])
                    Sc.dma('sp', scb[:], Wl['pool_scale'][0].partition_broadcast(64), writes=[scb])
                    op('dve', lambda: V.tensor_tensor(out=PW[:], in0=pwf[:], in1=scb[:].rearrange("p (g d) -> p g d", g=4),
                                                      op=ALU.mult), reads=[pwf, scb], writes=[PW])
                    for i in range(NT):
                        b = i % 2
                        for g in range(4):
                            a0 = 8 + g if i == 0 else g
                            op('pe', lambda: T.matmul(pd[b][0:64, g * 128:(g + 1) * 128], lhsT=U[:, i, g * 64:(g + 1) * 64],
                                                      rhs=PA[:, a0, :], start=True, stop=(i == 0)), reads=[U, PA], writes=[pd[b]])
                            if i > 0:
                                op('pe', lambda: T.matmul(pd[b][0:64, g * 128:(g + 1) * 128], lhsT=U[:, i - 1, g * 64:(g + 1) * 64],
                                                          rhs=PA[:, 4 + g, :], start=False, stop=True), reads=[U, PA], writes=[pd[b]])
                        op('act', lambda: A.copy(out=dT[b][:], in_=pd[b][0:64, :].rearrange("p (g t) -> p g t", g=4)),
                           reads=[pd[b]], writes=[dT[b]])
                        for g in range(4):
                            op('pe', lambda: T.matmul(py[b][:, g * 64:(g + 1) * 64], lhsT=dT[b][:, g, :], rhs=PW[:, g, :],
                                                      start=True, stop=True), reads=[dT[b], PW], writes=[py[b]])
                        op('dve', lambda: V.tensor_copy(out=yst[b][:], in_=py[b][:, 0:256]), reads=[py[b]], writes=[yst[b]])
                        Sc.dma('pool', ybuf[i * 128:(i + 1) * 128, 0:256], yst[b][:], reads=[yst[b]])
                Sc.barrier()

            if want('band'):
                with ExitStack() as ph:
                    KT = sb(ph, "KT", [64, 4, S], BF16)
                    QT = sb(ph, "QT", [64, 4, S], BF16)
                    VA = sb(ph, "VA", [128, NT, 4, 65], BF16)
                    raw = sb(ph, "raw", [128, 20, 128], F32)
                    raw2 = sb(ph, "raw2", [128, 20, 128], F32)
                    EB = sb(ph, "EB", [128, 20, 128], BF16)
                    bmk = sb(ph, "bmk", [128, 5, 128], BF16)
                    E = [sb(ph, f"E{i}", [128, 20, 128], BF16) for i in range(2)]
                    Pm = [sb(ph, f"Pm{i}", [128, 20, 128], BF16) for i in range(2)]
                    rden = sb(ph, "rden", [128, 4], F32)
                    yst = [sb(ph, f"ystb{i}", [128, 4, 64], F32) for i in range(2)]
                    SCp = [ps(ph, f"SCp{i}") for i in range(5)]
                    Op = [ps(ph, f"Op{i}") for i in range(2)]
                    Sc.dma('sp', QT[:], zfm[0:256, :].rearrange("(h d) t -> d h t", h=4), writes=[QT])
                    Sc.dma('sp', KT[:], zfm[256:512, :].rearrange("(h d) t -> d h t", h=4), writes=[KT])
                    for h in range(4):
                        Sc.dma('sp', VA[:, :, h, 0:64], ztm[:, 256 + h * 64:256 + (h + 1) * 64].rearrange("(n p) d -> p n d", p=128),
                               writes=[VA])
                    op('pool', lambda: P.memset(VA[:, :, :, 64:65], 1.0), writes=[VA])
                    Sc.dma('sp', bmk[:], c_bandm, writes=[bmk])
                    crs = sb(ph, "crs", [4, 320], F32)
                    exs = sb(ph, "exs", [4, 768], F32)
                    Sc.dma('sp', crs[:], Wl['ca_rel'], writes=[crs])
                    op('dve', lambda: V.tensor_copy(out=exs[:, 64:384], in_=crs[:]), reads=[crs], writes=[exs])
                    op('dve', lambda: V.tensor_copy(out=exs[:, 0:64], in_=crs[:, 0:1].to_broadcast([4, 64])), reads=[crs], writes=[exs])
                    op('dve', lambda: V.tensor_copy(out=exs[:, 384:768], in_=crs[:, 319:320].to_broadcast([4, 384])),
                       reads=[crs], writes=[exs])
                    Sc.dma('sp', ext, exs[:], reads=[exs])
                    Sc.barrier()
                    for h in range(4):
                        Sc.dma('sp', raw[:, h * 5:(h + 1) * 5, :], dram_ap(ext, h * 768, [[1, 128], [1, 640]]), writes=[raw])
                    for h in range(4):
                        op('pe', lambda: T.matmul(SCp[0][:], lhsT=Jf[:], rhs=raw[:, h * 5:h * 5 + 4, :].rearrange("p a t -> p (a t)"),
                                                  start=True, stop=True), reads=[Jf, raw], writes=[SCp[0]])
                        op('pe', lambda: T.matmul(SCp[1][:, 0:128], lhsT=Jf[:], rhs=raw[:, h * 5 + 4, :], start=True, stop=True),
                           reads=[Jf, raw], writes=[SCp[1]])
                        op('act', lambda: A.activation(out=raw2[:, h * 5:h * 5 + 4, :], in_=SCp[0][:].rearrange("p (a t) -> p a t", a=4),
                                                       func=AF.Exp), reads=[SCp[0]], writes=[raw2])
                        op('act', lambda: A.activation(out=raw2[:, h * 5 + 4, :], in_=SCp[1][:, 0:128], func=AF.Exp),
                           reads=[SCp[1]], writes=[raw2])
                        op('dve', lambda: V.tensor_tensor(out=EB[:, h * 5:(h + 1) * 5, :], in0=raw2[:, h * 5:(h + 1) * 5, :],
                                                          in1=bmk[:], op=ALU.mult), reads=[raw2, bmk], writes=[EB])
                    for i in range(NT):
                        b = i % 2
                        nd = min(i, 4) + 1
                        for h in range(4):
                            for d in range(nd):
                                j = i - d
                                blk = h * 5 + d
                                pb = SCp[blk // 4]
                                op('pe', lambda: T.matmul(pb[:, (blk % 4) * 128:(blk % 4 + 1) * 128], lhsT=KT[:, h, j * 128:(j + 1) * 128],
                                                          rhs=QT[:, h, i * 128:(i + 1) * 128], start=True, stop=True),
                                   reads=[KT, QT], writes=[pb])
                        for k in range(5):
                            op('act', lambda: A.activation(out=E[b][:, 4 * k:4 * k + 4, :],
                                                           in_=SCp[k][:].rearrange("p (a t) -> p a t", a=4), func=AF.Exp,
                                                           scale=0.125), reads=[SCp[k]], writes=[E[b]])
                        for hh in range(2):
                            op('dve', lambda: V.tensor_tensor(out=Pm[b][:, hh * 10:(hh + 1) * 10, :], in0=E[b][:, hh * 10:(hh + 1) * 10, :],
                                                              in1=EB[:, hh * 10:(hh + 1) * 10, :], op=ALU.mult),
                               reads=[E[b], EB], writes=[Pm[b]])
                        for h in range(4):
                            for d in range(nd):
                                j = i - d
                                op('pe', lambda: T.matmul(Op[b][:, h * 65:(h + 1) * 65], lhsT=Pm[b][:, h * 5 + d, :],
                                                          rhs=VA[:, j, h, :], start=(d == 0), stop=(d == nd - 1)),
                                   reads=[Pm[b], VA], writes=[Op[b]])
                        ov = Op[b][:, 0:260].rearrange("p (h d) -> p h d", h=4)
                        op('dve', lambda: V.reciprocal(out=rden[:], in_=ov[:, :, 64]), reads=[Op[b]], writes=[rden])
                        op('dve', lambda: V.tensor_tensor(out=yst[b][:], in0=ov[:, :, 0:64],
                                                          in1=rden[:].unsqueeze(2).to_broadcast([128, 4, 64]), op=ALU.mult),
                           reads=[Op[b], rden], writes=[yst[b]])
                        Sc.dma('pool', ybuf[i * 128:(i + 1) * 128, 256:512], yst[b][:].rearrange("p h d -> p (h d)"), reads=[yst[b]])
                Sc.barrier()

            if want('dsa'):
                with ExitStack() as ph:
                    BIG = 30000.0
                    IKT = sb(ph, "IKT", [64, S], BF16)
                    SKT = sb(ph, "SKT", [64, S], BF16)
                    VC = sb(ph, "VC", [128, NT, 65], BF16)
                    IQT = [sb(ph, f"IQT{i}", [64, 8, 128], BF16) for i in range(2)]
                    SQT = [sb(ph, f"SQT{i}", [64, 4, 128], BF16) for i in range(3)]
                    IW = [sb(ph, f"IW{i}", [128, 8], F32) for i in range(2)]
                    Dg = [sb(ph, f"Dg{i}", [128, 8, 128], BF16) for i in range(2)]
                    R = [sb(ph, f"R{i}", [128, 512], BF16) for i in range(3)]
                    SCs = [sb(ph, f"SCs{i}", [128, S], F32) for i in range(2)]
                    cjunk = sb(ph, "cjunk", [128, S], BF16)
                    MK = [sb(ph, f"MK{i}", [128, S], BF16) for i in range(2)]
                    PEN = sb(ph, "PEN", [128, NT, 128], BF16)
                    T5B = sb(ph, "T5B", [128, 8, 128], BF16)
                    rawt = sb(ph, "rawt", [128, 8, 128], F32)
                    t5s = sb(ph, "t5s", [32, 4], F32)
                    ohs = sb(ph, "ohs", [32, 384], F32)
                    t5r = sb(ph, "t5r", [4, 384], F32)
                    t5bc = sb(ph, "t5bc", [128, 128], F32)
                    nt5 = sb(ph, "nt5", [128, 4], F32)
                    negbig = sb(ph, "negbig", [128, 1], F32)
                    pw2 = sb(ph, "pw2", [128, NBIS], F32)
                    wts = sb(ph, "wts", [128, NBIS], F32)
                    bs = sb(ph, "bs", [128, 8], F32)
                    Eb = [sb(ph, f"Eb{i}", [128, 512], BF16) for i in range(3)]
                    rden = sb(ph, "rdenc", [128, 4], F32)
                    yst = [sb(ph, f"ystc{i}", [128, 4, 64], F32) for i in range(2)]
                    Lp = [ps(ph, f"Lp{i}") for i in range(2)]
                    SPp = ps(ph, "SPp")
                    MTp = ps(ph, "MTp", BF16)
                    STp = [ps(ph, f"STp{i}") for i in range(2)]
                    Op = [ps(ph, f"Opc{i}") for i in range(2)]
                    Sc.dma('sp', SKT[:], zfm[1280:1344, :], writes=[SKT])
                    Sc.dma('sp', IKT[:], zfm[1344:1408, :], writes=[IKT])
                    Sc.dma('sp', VC[:, :, 0:64], ztm[:, 512:576].rearrange("(n p) d -> p n d", p=128), writes=[VC])
                    op('pool', lambda: P.memset(VC[:, :, 64:65], 1.0), writes=[VC])
                    op('pool', lambda: P.memset(negbig[:], -BIG), writes=[negbig])
                    Sc.dma('sp', pw2[:], c_pow2, writes=[pw2])
                    Sc.dma('sp', t5s[:], t5_in, writes=[t5s])
                    Sc.dma('sp', ohs[:], c_t5oh, writes=[ohs])
                    Sc.dma('sp', t5bc[:], t5_in.rearrange("a b -> (a b)").partition_broadcast(128), writes=[t5bc])
                    op('pe', lambda: T.matmul(SPp[0:4, 0:384], lhsT=t5s[:], rhs=ohs[:], start=True, stop=True),
                       reads=[t5s, ohs], writes=[SPp])
                    op('dve', lambda: V.tensor_copy(out=t5r[:], in_=SPp[0:4, 0:384]), reads=[SPp], writes=[t5r])
                    Sc.dma('sp', t5v, t5r[:], reads=[t5r])
                    op('dve', lambda: V.tensor_scalar(out=nt5[:], in0=t5bc[:, 60:64], scalar1=-8.0, scalar2=None, op0=ALU.mult),
                       reads=[t5bc], writes=[nt5])
                    Sc.barrier()
                    for dd in range(2):
                        for h in range(4):
                            Sc.dma('sp', rawt[:, dd * 4 + h, :], dram_ap(t5v, h * 384 + 128 * dd, [[1, 128], [1, 128]]),
                                   writes=[rawt])
                    for dd in range(2):
                        op('pe', lambda: T.matmul(Lp[dd][:], lhsT=Jf[:], rhs=rawt[:, dd * 4:dd * 4 + 4, :].rearrange("p a t -> p (a t)"),
                                                  start=True, stop=True), reads=[Jf, rawt], writes=[Lp[dd]])
                        for h in range(4):
                            op('act', lambda: A.activation(out=T5B[:, dd * 4 + h, :], in_=Lp[dd][:, h * 128:(h + 1) * 128],
                                                           func=AF.Identity, bias=nt5[:, h:h + 1], scale=8.0),
                               reads=[Lp[dd], nt5], writes=[T5B])
                    rcnt = [0]

                    def stageA(i):
                        b = i % 2
                        N = 128 * (i + 1)
                        sq = SQT[i % 3]
                        Sc.dma('sp', IQT[b][:], zfm[768:1280, i * 128:(i + 1) * 128].rearrange("(h d) t -> d h t", h=8), writes=[IQT[b]])
                        Sc.dma('sp', sq[:], zfm[512:768, i * 128:(i + 1) * 128].rearrange("(h d) t -> d h t", h=4), writes=[sq])
                        Sc.dma('sp', IW[b][:], zsm[i * 128:(i + 1) * 128, :], writes=[IW[b]])
                        for h in range(8):
                            op('pool', lambda: P.tensor_scalar(out=Dg[b][:, h, :], in0=ident[:], scalar1=IW[b][:, h:h + 1],
                                                               scalar2=None, op0=ALU.mult), reads=[ident, IW[b]], writes=[Dg[b]])
                        nblk = (N + 511) // 512
                        for kb in range(nblk):
                            wb = min(512, N - kb * 512)
                            base = rcnt[0]

                            def acc(h):
                                rb = R[(base + h) % 3]
                                op('pe', lambda: T.matmul(SPp[:, 0:wb], lhsT=Dg[b][:, h, :], rhs=rb[:, 0:wb], start=(h == 0),
                                                          stop=(h == 7)), reads=[Dg[b], rb], writes=[SPp])
                            for h in range(8):
                                lp = Lp[(base + h) % 2]
                                rb = R[(base + h) % 3]
                                op('pe', lambda: T.matmul(lp[:, 0:wb], lhsT=IQT[b][:, h, :], rhs=IKT[:, kb * 512:kb * 512 + wb],
                                                          start=True, stop=True), reads=[IQT[b], IKT], writes=[lp])
                                op('act', lambda: A.activation(out=rb[:, 0:wb], in_=lp[:, 0:wb], func=AF.Relu),
                                   reads=[lp], writes=[rb])
                                if h >= 1:
                                    acc(h - 1)
                            acc(7)
                            rcnt[0] += 8
                            op('act', lambda: A.copy(out=SCs[b][:, kb * 512:kb * 512 + wb], in_=SPp[:, 0:wb]), reads=[SPp], writes=[SCs[b]])
                        op('pool', lambda: P.memset(SCs[b][0:64, N - 64:N], NEG), writes=[SCs[b]])

                    def stageB(i):
                        b = i % 2
                        N = 128 * (i + 1)
                        sc = SCs[b]
                        if i >= 2:
                            op('dve', lambda: V.tensor_reduce(out=bs[:, 0:1], in_=sc[:, 0:N], axis=AX.X, op=ALU.max),
                               reads=[sc], writes=[bs])
                            op('dve', lambda: V.tensor_reduce(out=bs[:, 3:4], in_=sc[:, 0:N - 64], axis=AX.X, op=ALU.min),
                               reads=[sc], writes=[bs])
                            op('dve', lambda: V.tensor_tensor(out=bs[:, 2:3], in0=bs[:, 0:1], in1=bs[:, 3:4], op=ALU.subtract),
                               reads=[bs], writes=[bs])
                            op('dve', lambda: V.tensor_scalar(out=wts[:], in0=pw2[:], scalar1=bs[:, 2:3], scalar2=None,
                                                              op0=ALU.mult), reads=[pw2, bs], writes=[wts])
                            for it in range(NBIS):
                                op('dve', lambda: V.tensor_tensor(out=bs[:, 4:5], in0=bs[:, 3:4], in1=wts[:, it:it + 1], op=ALU.add),
                                   reads=[bs, wts], writes=[bs])
                                op('dve', lambda: V.tensor_scalar(out=cjunk[:, 0:N], in0=sc[:, 0:N], scalar1=bs[:, 4:5], scalar2=0.0,
                                                                  op0=ALU.is_ge, op1=ALU.add, accum_out=bs[:, 5:6]),
                                   reads=[sc, bs], writes=[cjunk, bs], multi=True)
                                op('dve', lambda: V.tensor_scalar(out=bs[:, 6:7], in0=bs[:, 5:6], scalar1=TOPK - 0.5,
                                                                  scalar2=wts[:, it:it + 1], op0=ALU.is_gt, op1=ALU.mult),
                                   reads=[bs, wts], writes=[bs])
                                op('dve', lambda: V.tensor_tensor(out=bs[:, 3:4], in0=bs[:, 3:4], in1=bs[:, 6:7], op=ALU.add),
                                   reads=[bs], writes=[bs])
                        else:
                            op('dve', lambda: V.memset(bs[:, 3:4], -1.0e29), writes=[bs])
                        op('dve', lambda: V.tensor_scalar(out=MK[b][:, 0:N], in0=sc[:, 0:N], scalar1=bs[:, 3:4], scalar2=None,
                                                          op0=ALU.is_ge), reads=[sc, bs], writes=[MK[b]])

                    ccnt = [0]

                    def stageC(i):
                        b = i % 2
                        mk = MK[b]
                        sq = SQT[i % 3]
                        for j0 in range(0, i + 1, 8):
                            nb = min(8, i + 1 - j0)
                            for jj in range(nb):
                                j = j0 + jj
                                op('pe', lambda: T.transpose(out=MTp[:, jj * 128:(jj + 1) * 128], in_=mk[:, j * 128:(j + 1) * 128],
                                                             identity=ident[:]), reads=[mk, ident], writes=[MTp])
                            op('act', lambda: A.activation(out=PEN[:, j0:j0 + nb, :],
                                                           in_=MTp[:, 0:nb * 128].rearrange("p (a t) -> p a t", a=nb),
                                                           func=AF.Identity, bias=negbig[:, 0:1], scale=BIG),
                               reads=[MTp, negbig], writes=[PEN])
                        ob = Op[b]
                        for j in range(i + 1):
                            k = ccnt[0]
                            ccnt[0] += 1
                            sp_, eb_ = STp[k % 2], Eb[k % 3]
                            op('pe', lambda: T.matmul(sp_[:], lhsT=SKT[:, j * 128:(j + 1) * 128],
                                                      rhs=sq[:].rearrange("p h t -> p (h t)"), start=True, stop=False),
                               reads=[SKT, sq], writes=[sp_])
                            near = i - j
                            op('pe', lambda: T.matmul(sp_[:], lhsT=ident[:], rhs=bc_mid(PEN[:, j, :], 4), start=False,
                                                      stop=(near > 1)), reads=[ident, PEN], writes=[sp_])
                            if near <= 1:
                                op('pe', lambda: T.matmul(sp_[:], lhsT=ident[:],
                                                          rhs=T5B[:, near * 4:near * 4 + 4, :].rearrange("p a t -> p (a t)"),
                                                          start=False, stop=True), reads=[ident, T5B], writes=[sp_])
                            op('act', lambda: A.activation(out=eb_[:], in_=sp_[:], func=AF.Exp, scale=0.125), reads=[sp_], writes=[eb_])
                            for h in range(4):
                                op('pe', lambda: T.matmul(ob[:, h * 65:(h + 1) * 65], lhsT=eb_[:, h * 128:(h + 1) * 128], rhs=VC[:, j, :],
                                                          start=(j == 0 and h == 0), stop=(j == i), skip_group_check=True),
                                   reads=[eb_, VC], writes=[ob])
                        ov = ob[:, 0:260].rearrange("p (h d) -> p h d", h=4)
                        op('dve', lambda: V.reciprocal(out=rden[:], in_=ov[:, :, 64]), reads=[ob], writes=[rden])
                        op('dve', lambda: V.tensor_tensor(out=yst[b][:], in0=ov[:, :, 0:64],
                                                          in1=rden[:].unsqueeze(2).to_broadcast([128, 4, 64]), op=ALU.mult),
                           reads=[ob, rden], writes=[yst[b]])
                        Sc.dma('pool', ybuf[i * 128:(i + 1) * 128, 512:768], yst[b][:].rearrange("p h d -> p (h d)"), reads=[yst[b]])

                    stageA(0)
                    if NT > 1:
                        stageA(1)
                    stageB(0)
                    for i in range(NT):
                        if i + 2 < NT:
                            stageA(i + 2)
                        if i + 1 < NT:
                            stageB(i + 1)
                        stageC(i)
                Sc.barrier()

            if want('mla'):
                with ExitStack() as ph:
                    CQTb = [sb(ph, f"CQT{i}", [128, 2, 512], BF16) for i in range(2)]
                    CKTb = [sb(ph, f"CKT{i}", [128, 512], BF16) for i in range(2)]
                    KRRb = [sb(ph, f"KRR{i}", [96, 2, 512], BF16) for i in range(2)]
                    CSb = [sb(ph, f"CS{i}", [96, 2, 512], BF16) for i in range(2)]
                    QT = sb(ph, "QTd", [96, 4, S], BF16)
                    KT = sb(ph, "KTd", [96, 4, S], BF16)
                    VD = sb(ph, "VD", [128, NT, 4, 65], BF16)
                    wst = sb(ph, "wstd", [128, 2, 384], F32)
                    gq = sb(ph, "gq", [128, 2], F32)
                    gk = sb(ph, "gk", [128, 1], F32)
                    WQA = sb(ph, "WQA", [128, 2, 384], BF16)
                    WQB = sb(ph, "WQB", [128, 2, 384], BF16)
                    WK = sb(ph, "WK", [128, 256], BF16)
                    WV = sb(ph, "WV", [128, 256], BF16)
                    RKc = sb(ph, "RKc", [128, NT], F32)
                    RQb = [sb(ph, f"RQb{i}", [96, 512], F32) for i in range(2)]
                    RKb = [sb(ph, f"RKb{i}", [64, 512], F32) for i in range(2)]
                    cr = sb(ph, "cr", [96, 512], F32)
                    sr = sb(ph, "sr", [96, 512], F32)
                    ta = sb(ph, "ta", [96, 512], F32)
                    tb = sb(ph, "tb", [96, 512], F32)
                    tk1 = sb(ph, "tk1", [96, 512], F32)
                    tk2 = sb(ph, "tk2", [96, 512], F32)
                    Eb = [sb(ph, f"Ed{i}", [128, 512], BF16) for i in range(3)]
                    rden = sb(ph, "rdend", [128, 4], F32)
                    yst = [sb(ph, f"ystd{i}", [128, 4, 64], F32) for i in range(2)]
                    PAp = [ps(ph, f"PAp{i}") for i in range(2)]
                    PBp = [ps(ph, f"PBp{i}") for i in range(2)]
                    STp = [ps(ph, f"STd{i}") for i in range(2)]
                    Op = [ps(ph, f"Opd{i}") for i in range(2)]
                    Sc.dma('sp', RKc[:], dram_ap(zrk, 0, [[1, 128], [128, NT]]), writes=[RKc], allow_slow_non_contiguous=True)
                    Sc.dma('sp', gq[:], dram_ap(Wl['g_cq'], 0, [[1, 128], [128, 2]]), writes=[gq], allow_slow_non_contiguous=True)
                    Sc.dma('sp', gk[:], dram_ap(Wl['g_ckv'], 0, [[1, 128], [1, 1]]), writes=[gk])
                    for (src, dst) in ((Wl['wqa'], WQA), (Wl['wqb'], WQB)):
                        Sc.dma('sp', wst[:], src.rearrange("(c p) n -> p c n", p=128), writes=[wst])
                        for c2 in range(2):
                            op('dve', lambda: V.tensor_scalar(out=dst[:, c2, :], in0=wst[:, c2, :], scalar1=gq[:, c2:c2 + 1],
                                                              scalar2=None, op0=ALU.mult), reads=[wst, gq], writes=[dst])
                    for (src, dst) in ((Wl['wk'], WK), (Wl['wv'], WV)):
                        Sc.dma('sp', wst[:, 0, 0:256], src, writes=[wst])
                        op('dve', lambda: V.tensor_scalar(out=dst[:], in0=wst[:, 0, 0:256], scalar1=gk[:, 0:1], scalar2=None,
                                                          op0=ALU.mult), reads=[wst, gk], writes=[dst])
                    op('pool', lambda: P.memset(VD[:, :, :, 64:65], 1.0), writes=[VD])
                    for m in range(NG):
                        b = m % 2
                        gblk = slice(m * 512, (m + 1) * 512)
                        blk = slice(0, 512)
                        CQT, CKT, KRR, CS = CQTb[b], CKTb[b], KRRb[b], CSb[b]
                        Sc.dma('sp', CQT[:], zfm[1408:1664, gblk].rearrange("(c p) t -> p c t", p=128), writes=[CQT])
                        Sc.dma('sp', CKT[:], zfm[1664:1792, gblk], writes=[CKT])
                        Sc.dma('sp', KRR[64:96, 0, :], zfm[1792 + 64:1792 + 96, gblk], writes=[KRR])
                        Sc.dma('sp', KRR[64:96, 1, :], zfm[1920 + 64:1920 + 96, gblk], writes=[KRR])
                        Sc.dma('sp', CS[64:96, 0, :], c_cs[0, :, gblk], writes=[CS])
                        Sc.dma('sp', CS[64:96, 1, :], c_cs[1, :, gblk], writes=[CS])
                        Sc.dma('sp', RQb[b][:], zrq[0, gblk].partition_broadcast(96), writes=[RQb[b]])
                        Sc.dma('sp', RKb[b][:], zrk[0, gblk].partition_broadcast(64), writes=[RKb[b]])
                        op('dve', lambda: V.tensor_tensor(out=cr[64:96, :], in0=CS[64:96, 0, blk], in1=RQb[b][64:96, :], op=ALU.mult),
                           reads=[CS, RQb[b]], writes=[cr])
                        op('dve', lambda: V.tensor_tensor(out=sr[64:96, :], in0=CS[64:96, 1, blk], in1=RQb[b][64:96, :], op=ALU.mult),
                           reads=[CS, RQb[b]], writes=[sr])
                        for h in range(4):
                            pa, pb2 = PAp[h % 2], PBp[h % 2]
                            for c2 in range(2):
                                op('pe', lambda: T.matmul(pa[0:96, :], lhsT=WQA[:, c2, h * 96:(h + 1) * 96], rhs=CQT[:, c2, blk],
                                                          start=(c2 == 0), stop=(c2 == 1)), reads=[WQA, CQT], writes=[pa])
                            for c2 in range(2):
                                op('pe', lambda: T.matmul(pb2[0:96, :], lhsT=WQB[:, c2, h * 96:(h + 1) * 96], rhs=CQT[:, c2, blk],
                                                          start=(c2 == 0), stop=(c2 == 1)), reads=[WQB, CQT], writes=[pb2])
                            op('dve', lambda: V.tensor_tensor(out=QT[0:64, h, gblk], in0=pa[0:64, :], in1=RQb[b][0:64, :], op=ALU.mult),
                               reads=[pa, RQb[b]], writes=[QT])
                            op('dve', lambda: V.tensor_tensor(out=ta[64:96, :], in0=pa[64:96, :], in1=cr[64:96, :], op=ALU.mult),
                               reads=[pa, cr], writes=[ta])
                            op('dve', lambda: V.tensor_tensor(out=tb[64:96, :], in0=pb2[64:96, :], in1=sr[64:96, :], op=ALU.mult),
                               reads=[pb2, sr], writes=[tb])
                            op('dve', lambda: V.tensor_tensor(out=QT[64:96, h, gblk], in0=ta[64:96, :], in1=tb[64:96, :], op=ALU.add),
                               reads=[ta, tb], writes=[QT])
                        for h in range(4):
                            pa = PAp[h % 2]
                            op('pe', lambda: T.matmul(pa[0:64, :], lhsT=WK[:, h * 64:(h + 1) * 64], rhs=CKT[:, blk], start=True,
                                                      stop=True), reads=[WK, CKT], writes=[pa])
                            op('dve', lambda: V.tensor_tensor(out=KT[0:64, h, gblk], in0=pa[0:64, :], in1=RKb[b][0:64, :], op=ALU.mult),
                               reads=[pa, RKb[b]], writes=[KT])
                        op('dve', lambda: V.tensor_tensor(out=tk1[64:96, :], in0=KRR[64:96, 0, blk], in1=CS[64:96, 0, blk], op=ALU.mult),
                           reads=[KRR, CS], writes=[tk1])
                        op('dve', lambda: V.tensor_tensor(out=tk2[64:96, :], in0=KRR[64:96, 1, blk], in1=CS[64:96, 1, blk], op=ALU.mult),
                           reads=[KRR, CS], writes=[tk2])
                        for h in range(4):
                            op('dve', lambda: V.tensor_tensor(out=KT[64:96, h, gblk], in0=tk1[64:96, :], in1=tk2[64:96, :], op=ALU.add),
                               reads=[tk1, tk2], writes=[KT])
                        for r in range(4):
                            tl = m * 4 + r
                            pv = PBp[r % 2]
                            op('pe', lambda: T.matmul(pv[:, 0:256], lhsT=CKT[:, r * 128:(r + 1) * 128], rhs=WV[:], start=True,
                                                      stop=True), reads=[CKT, WV], writes=[pv])
                            op('dve', lambda: V.tensor_scalar(out=VD[:, tl, :, 0:64], in0=pv[:, 0:256].rearrange("p (h d) -> p h d", h=4),
                                                              scalar1=RKc[:, tl:tl + 1], scalar2=None, op0=ALU.mult),
                               reads=[pv, RKc], writes=[VD])
                    Sc.barrier()
                    sc_mla = 1.0 / math.sqrt(96.0)
                    ecnt = 0
                    import os as _os
                    for h in range(0 if _os.environ.get("MLA_STOP") == "prep" else 4):
                        for m in range(NG):
                            ob = Op[(h * NG + m) % 2]
                            nj = 4 * m + 4
                            first = True
                            for j in range(nj):
                                rlo = max(0, j - 4 * m)
                                tlo = rlo * 128
                                n = 512 - tlo
                                sp_ = STp[ecnt % 2]
                                eb_ = Eb[ecnt % 3]
                                ecnt += 1
                                op('pe', lambda: T.matmul(sp_[:, 0:n], lhsT=KT[:, h, j * 128:(j + 1) * 128],
                                                          rhs=QT[:, h, m * 512 + tlo:(m + 1) * 512], start=True, stop=True),
                                   reads=[KT, QT], writes=[sp_])
                                op('act', lambda: A.activation(out=eb_[:, 0:n], in_=sp_[:, 0:n], func=AF.Exp, scale=sc_mla),
                                   reads=[sp_], writes=[eb_])
                                if j >= 4 * m:
                                    op('pool', lambda: P.memset(eb_[64:128, 0:64], 0.0), writes=[eb_])
                                for r in range(rlo, 4):
                                    op('pe', lambda: T.matmul(ob[:, r * 65:(r + 1) * 65], lhsT=eb_[:, (r - rlo) * 128:(r - rlo + 1) * 128],
                                                              rhs=VD[:, j, h, :], start=first, stop=(j == 4 * m + r),
                                                              skip_group_check=True), reads=[eb_, VD], writes=[ob])
                                    first = False
                            ov = ob[:, 0:260].rearrange("p (r d) -> p r d", r=4)
                            yb = yst[(h * NG + m) % 2]
                            op('dve', lambda: V.reciprocal(out=rden[:], in_=ov[:, :, 64]), reads=[ob], writes=[rden])
                            op('dve', lambda: V.tensor_tensor(out=yb[:], in0=ov[:, :, 0:64],
                                                              in1=rden[:].unsqueeze(2).to_broadcast([128, 4, 64]), op=ALU.mult),
                               reads=[ob, rden], writes=[yb])
                            Sc.dma('pool', ybuf[m * 512:(m + 1) * 512, 768 + h * 64:768 + (h + 1) * 64].rearrange("(r p) d -> p r d", p=128),
                                   yb[:], reads=[yb])
                Sc.barrier()

            if want('oproj'):
                with ExitStack() as ph:
                    WO = sb(ph, "WO", [128, 8, D], BF16)
                    gg = sb(ph, "gg", [128, 8], F32)
                    stg = [sb(ph, f"stgo{i}", [128, D], F32) for i in range(2)]
                    yt = [sb(ph, f"yt{i}", [128, D], F32) for i in range(2)]
                    xt = [sb(ph, f"xt{i}", [128, D], F32) for i in range(3)]
                    junk = sb(ph, "junko", [128, D], F32)
                    tmp = sb(ph, "tmpo", [128, D], F32)
                    ybs = [sb(ph, f"yb{i}", [128, D], BF16) for i in range(2)]
                    mT = sb(ph, "mT", [128, 8, 128], BF16)
                    hbs = [sb(ph, f"hbo{i}", [128, D], BF16) for i in range(2)]
                    h2s = [sb(ph, f"h2s{i}", [128, 8, 128], BF16) for i in range(2)]
                    sts = [sb(ph, f"sto{i}", [128, 16], F32) for i in range(2)]
                    pT = [ps(ph, f"pTo{i}", BF16) for i in range(2)]
                    PO = [ps(ph, f"PO{i}") for i in range(4)]
                    Sc.dma('sp', gg[:], dram_ap(Wl['g_group'], 0, [[1, 128], [128, 8]]), writes=[gg], allow_slow_non_contiguous=True)
                    for kc in range(8):
                        b = kc % 2
                        Sc.dma('sp', stg[b][:], Wl['w_out'][kc * 128:(kc + 1) * 128, :], writes=[stg[b]])
                        if kc % 2 == 0:
                            op('dve', lambda: V.tensor_scalar(out=WO[:, kc, :], in0=stg[b][:], scalar1=gg[:, kc:kc + 1], scalar2=None,
                                                              op0=ALU.mult), reads=[stg[b], gg], writes=[WO])
                        else:
                            op('pool', lambda: P.tensor_scalar(out=WO[:, kc, :], in0=stg[b][:], scalar1=gg[:, kc:kc + 1], scalar2=None,
                                                               op0=ALU.mult), reads=[stg[b], gg], writes=[WO])

                    def po_s0(i):
                        b = i % 2
                        y_t, x_t, st, yb = yt[b], xt[i % 3], sts[b], ybs[b]
                        Sc.dma('sp', y_t[:], ybuf[i * 128:(i + 1) * 128, :], writes=[y_t])
                        Sc.dma('sp', x_t[:], xin[i * 128:(i + 1) * 128, :], writes=[x_t])
                        for g in range(4):
                            op('act', lambda: A.activation(out=junk[:, 0:256], in_=y_t[:, g * 256:(g + 1) * 256], func=AF.Square,
                                                           scale=1.0 / 16.0, accum_out=st[:, g:g + 1]), reads=[y_t], writes=[junk, st],
                               multi=True)
                        op('act', lambda: A.activation(out=st[:, 4:8], in_=st[:, 0:4], func=AF.Sqrt, bias=epsT[:, 0:1], scale=1.0),
                           reads=[st, epsT], writes=[st])
                        op('dve', lambda: V.reciprocal(out=st[:, 8:12], in_=st[:, 4:8]), reads=[st], writes=[st])
                        for g in range(4):
                            op('dve', lambda: V.tensor_scalar(out=yb[:, g * 256:(g + 1) * 256], in0=y_t[:, g * 256:(g + 1) * 256],
                                                              scalar1=st[:, 8 + g:9 + g], scalar2=None, op0=ALU.mult),
                               reads=[y_t, st], writes=[yb])

                    def po_s1(i):
                        b = i % 2
                        x_t, st, yb, hb = xt[i % 3], sts[b], ybs[b], hbs[b]
                        for kc in range(8):
                            op('pe', lambda: T.transpose(out=pT[0][:, kc * 128:(kc + 1) * 128], in_=yb[:, kc * 128:(kc + 1) * 128],
                                                         identity=ident[:]), reads=[yb, ident], writes=[pT[0]])
                        op('act', lambda: A.copy(out=mT[:], in_=pT[0][:].rearrange("p (k t) -> p k t", k=8)), reads=[pT[0]], writes=[mT])
                        for nh in range(2):
                            po = PO[(i % 2) * 2 + nh]
                            for kc in range(8):
                                op('pe', lambda: T.matmul(po[:], lhsT=mT[:, kc, :], rhs=WO[:, kc, nh * 512:(nh + 1) * 512],
                                                          start=(kc == 0), stop=(kc == 7)), reads=[mT, WO], writes=[po])
                            hs = slice(nh * 512, (nh + 1) * 512)
                            op('dve', lambda: V.tensor_tensor(out=tmp[:, hs], in0=po[:], in1=MOD[:, 2, hs], op=ALU.mult),
                               reads=[po, MOD], writes=[tmp])
                            op('dve', lambda: V.tensor_tensor(out=x_t[:, hs], in0=x_t[:, hs], in1=tmp[:, hs], op=ALU.add),
                               reads=[x_t, tmp], writes=[x_t])
                        Sc.dma('pool', xout[i * 128:(i + 1) * 128, :], x_t[:], reads=[x_t])
                        op('act', lambda: A.activation(out=junk[:], in_=x_t[:], func=AF.Square, scale=1.0 / 32.0,
                                                       accum_out=st[:, 12:13]), reads=[x_t], writes=[junk, st], multi=True)
                        op('act', lambda: A.activation(out=st[:, 13:14], in_=st[:, 12:13], func=AF.Sqrt, bias=epsT[:, 0:1], scale=1.0),
                           reads=[st, epsT], writes=[st])
                        op('dve', lambda: V.reciprocal(out=st[:, 14:15], in_=st[:, 13:14]), reads=[st], writes=[st])
                        op('dve', lambda: V.scalar_tensor_tensor(out=tmp[:], in0=x_t[:], scalar=st[:, 14:15], in1=MOD[:, 4, :],
                                                                 op0=ALU.mult, op1=ALU.mult), reads=[x_t, st, MOD], writes=[tmp])
                        op('dve', lambda: V.tensor_tensor(out=hb[:], in0=tmp[:], in1=MOD[:, 3, :], op=ALU.add),
                           reads=[tmp, MOD], writes=[hb])

                    def po_s2(i):
                        b = i % 2
                        hb = hbs[b]
                        for kc in range(8):
                            op('pe', lambda: T.transpose(out=pT[1][:, kc * 128:(kc + 1) * 128], in_=hb[:, kc * 128:(kc + 1) * 128],
                                                         identity=ident[:]), reads=[hb, ident], writes=[pT[1]])
                        op('act', lambda: A.copy(out=h2s[b][:], in_=pT[1][:].rearrange("p (k t) -> p k t", k=8)),
                           reads=[pT[1]], writes=[h2s[b]])
                        Sc.dma('pool', h2T[:, i * 128:(i + 1) * 128].rearrange("(c p) t -> p c t", p=128), h2s[b][:], reads=[h2s[b]])

                    po_s0(0)
                    for i in range(NT):
                        if i + 1 < NT:
                            po_s0(i + 1)
                        po_s1(i)
                        if i >= 1:
                            po_s2(i - 1)
                    po_s2(NT - 1)
                Sc.barrier()

            if want('ffn'):
                for fp in range(2):
                    with ExitStack() as ph:
                        W1 = sb(ph, "W1", [128, 8, FH], BF16)
                        W3 = sb(ph, "W3", [128, 8, FH], BF16)
                        W2 = sb(ph, "W2", [128, NFC, D], BF16)
                        stg = [sb(ph, f"stgf{i}", [128, FH], F32) for i in range(2)]
                        H2 = [sb(ph, f"H2{i}", [128, 8, 512], BF16) for i in range(2)]
                        AT = sb(ph, "AT", [128, NFC, 512], BF16)
                        sl = [sb(ph, f"sl{i}", [128, 512], F32) for i in range(2)]
                        xt = [sb(ph, f"xtf{i}", [128, D], F32) for i in range(2)]
                        tmp = sb(ph, "tmpf", [128, D], F32)
                        ost = [sb(ph, f"ost{i}", [128, D], F32) for i in range(2)]
                        junk = sb(ph, "junkf", [128, D], F32)
                        gfb = sb(ph, "gfb", [128, D], F32)
                        st = sb(ph, "stf", [128, 4], F32)
                        G1p = [ps(ph, f"G1p{i}") for i in range(2)]
                        G3p = [ps(ph, f"G3p{i}") for i in range(2)]
                        PD = [ps(ph, f"PD{i}") for i in range(4)]
                        f0 = fp * FH
                        for kc in range(8):
                            b = kc % 2
                            Sc.dma('sp', stg[b][:], Wl['w1'][kc * 128:(kc + 1) * 128, f0:f0 + FH], writes=[stg[b]])
                            cast(W1[:, kc, :], stg[b][:], [stg[b]], [W1])
                        for kc in range(8):
                            b = kc % 2
                            Sc.dma('sp', stg[b][:], Wl['w3'][kc * 128:(kc + 1) * 128, f0:f0 + FH], writes=[stg[b]])
                            cast(W3[:, kc, :], stg[b][:], [stg[b]], [W3])
                        for fc in range(NFC):
                            b = fc % 2
                            Sc.dma('sp', stg[b][:, 0:D], Wl['w2'][f0 + fc * 128:f0 + (fc + 1) * 128, :], writes=[stg[b]])
                            cast(W2[:, fc, :], stg[b][:, 0:D], [stg[b]], [W2])
                        if last and fp == 1:
                            Sc.dma('sp', gfb[:], gfin_in[0].partition_broadcast(128), writes=[gfb])
                        for g in range(NG):
                            hh = H2[g % 2]
                            Sc.dma('sp', hh[:], h2T[:, g * 512:(g + 1) * 512].rearrange("(c p) t -> p c t", p=128), writes=[hh])
                            for fc in range(NFC):
                                g1, g3, s_ = G1p[fc % 2], G3p[fc % 2], sl[fc % 2]
                                for kc in range(8):
                                    op('pe', lambda: T.matmul(g1[:], lhsT=W1[:, kc, fc * 128:(fc + 1) * 128], rhs=hh[:, kc, :],
                                                              start=(kc == 0), stop=(kc == 7)), reads=[W1, hh], writes=[g1])
                                for kc in range(8):
                                    op('pe', lambda: T.matmul(g3[:], lhsT=W3[:, kc, fc * 128:(fc + 1) * 128], rhs=hh[:, kc, :],
                                                              start=(kc == 0), stop=(kc == 7)), reads=[W3, hh], writes=[g3])
                                op('act', lambda: A.activation(out=s_[:], in_=g1[:], func=AF.Silu), reads=[g1], writes=[s_])
                                op('dve', lambda: V.tensor_tensor(out=AT[:, fc, :], in0=s_[:], in1=g3[:], op=ALU.mult),
                                   reads=[s_, g3], writes=[AT])
                            for r in range(4):
                                i = g * 4 + r
                                x_t = xt[i % 2]
                                Sc.dma('sp', x_t[:], xout[i * 128:(i + 1) * 128, :], writes=[x_t])
                                for nh in range(2):
                                    pd = PD[(i % 2) * 2 + nh]
                                    hs = slice(nh * 512, (nh + 1) * 512)
                                    for fc in range(NFC):
                                        op('pe', lambda: T.matmul(pd[:], lhsT=AT[:, fc, r * 128:(r + 1) * 128], rhs=W2[:, fc, hs],
                                                                  start=(fc == 0), stop=(fc == NFC - 1)), reads=[AT, W2], writes=[pd])
                                    op('dve', lambda: V.tensor_tensor(out=tmp[:, hs], in0=pd[:], in1=MOD[:, 5, hs], op=ALU.mult),
                                       reads=[pd, MOD], writes=[tmp])
                                    op('dve', lambda: V.tensor_tensor(out=x_t[:, hs], in0=x_t[:, hs], in1=tmp[:, hs], op=ALU.add),
                                       reads=[x_t, tmp], writes=[x_t])
                                Sc.dma('pool', xout[i * 128:(i + 1) * 128, :], x_t[:], reads=[x_t])
                                if last and fp == 1:
                                    o_t = ost[i % 2]
                                    op('act', lambda: A.activation(out=junk[:], in_=x_t[:], func=AF.Square, scale=1.0 / 32.0,
                                                                   accum_out=st[:, 0:1]), reads=[x_t], writes=[junk, st], multi=True)
                                    op('act', lambda: A.activation(out=st[:, 1:2], in_=st[:, 0:1], func=AF.Sqrt, bias=epsT[:, 0:1],
                                                                   scale=1.0), reads=[st, epsT], writes=[st])
                                    op('dve', lambda: V.reciprocal(out=st[:, 2:3], in_=st[:, 1:2]), reads=[st], writes=[st])
                                    op('dve', lambda: V.scalar_tensor_tensor(out=o_t[:], in0=x_t[:], scalar=st[:, 2:3], in1=gfb[:],
                                                                             op0=ALU.mult, op1=ALU.mult), reads=[x_t, st, gfb], writes=[o_t])
                                    Sc.dma('pool', out[seq, i * 128:(i + 1) * 128, :], o_t[:], reads=[o_t])
                    Sc.barrier()
        Sc.barrier()
        print("program instructions:", Sc.ninstr, Sc.cnt)
    return nc


_PROG_CACHE = {}


def _get_prog(S, nseq, nlayer, steps):
    key = (S, nseq, nlayer, tuple(steps))
    if key not in _PROG_CACHE:
        _PROG_CACHE[key] = build(S, nseq, nlayer, list(steps))
    return _PROG_CACHE[key]


def kernel(**inputs):
    x = np.asarray(inputs['x'], dtype=np.float32)
    c = np.asarray(inputs['c'], dtype=np.float32)
    B, S, _ = x.shape
    L = np.asarray(inputs['w_mod']).shape[0]
    per = B // N_CORES
    consts = host_consts(S)
    t5 = np.ascontiguousarray(np.asarray(inputs['t5_table'], dtype=np.float32))
    gfin = np.ascontiguousarray(np.asarray(inputs['g_final'], dtype=np.float32)).reshape(1, -1)
    lw = [layout_weights(inputs, l) for l in range(L)]
    steps = tuple((sl, l, l == L - 1) for l in range(L) for sl in range(per))
    nc = _get_prog(S, per, L, steps)
    in_maps = []
    for core in range(N_CORES):
        m = dict(x=np.ascontiguousarray(x[core * per:(core + 1) * per]),
                 c=np.ascontiguousarray(c[core * per:(core + 1) * per]), t5=t5, g_final=gfin)
        for l in range(L):
            m.update({f"{k}_{l}": v for k, v in lw[l].items()})
        m.update(consts)
        in_maps.append(m)
    res = run_bass_kernel_spmd(nc, in_maps, core_ids=list(range(N_CORES)))
    full = np.empty((B, S, D), np.float32)
    for core in range(N_CORES):
        full[core * per:(core + 1) * per] = res.results[core]["out"]
    return full
```

```python
import math
from contextlib import ExitStack

import numpy as np
import ml_dtypes
import concourse.bass as bass
import concourse.mybir as mybir
from concourse.bass_types import AP
from concourse.bass_utils import run_bass_kernel_spmd

AF = mybir.ActivationFunctionType
ALU = mybir.AluOpType
AX = mybir.AxisListType
F32 = mybir.dt.float32
BF16 = mybir.dt.bfloat16

D = 1024
DFF = 2816
FH = DFF // 2
NFC = FH // 128
EPS = 1e-6
TMW = 968
FMW = 2048
NBIS = 12
TOPK = 256
NEG = -1.0e30
EPOCH = 16000
N_CORES = 8


class Buf:
    def __init__(self, h, name):
        self.h = h
        self.name = name
        self.w = None
        self.r = {}

    def __getitem__(self, idx):
        return self.h[idx]


class Sched:
    ENG = ('pe', 'act', 'dve', 'pool', 'sp')

    def __init__(self, nc, es):
        self.nc = nc
        self.es = es
        self.engs = {'pe': nc.tensor, 'act': nc.scalar, 'dve': nc.vector, 'pool': nc.gpsimd, 'sp': nc.sync}
        self.sems = {}
        self.cnt = {k: 0 for k in self.ENG}
        self.seen = {k: {} for k in self.ENG}
        self.dq = {}
        for q, n in (('sp', 10), ('pool', 6), ('act', 4)):
            self.dq[q] = dict(sems=[es.enter_context(nc.semaphore(f"dma_{q}{i}")) for i in range(n)],
                              cnt=[0] * n, i=0)
        self.ninstr = 0

    def _sem(self, key, n):
        ep = n // EPOCH
        k = (key, ep)
        if k not in self.sems:
            self.sems[k] = self.es.enter_context(self.nc.semaphore(f"s_{key}_{ep}"))
        return self.sems[k], n % EPOCH + 1

    def _semval(self, ev):
        key, n = ev
        if isinstance(key, tuple):
            return self.dq[key[1]]['sems'][key[2]], 16 * (n + 1)
        return self._sem(key, n)

    def _need(self, eng, evs):
        out = {}
        for key, n in evs:
            if self.seen[eng].get(key, -1) >= n:
                continue
            if out.get(key, -1) < n:
                out[key] = n
        return list(out.items())

    def _emit_waits(self, eng, evs, ins_fn=None):
        need = self._need(eng, evs)
        for key, n in need:
            self.seen[eng][key] = n
        if ins_fn is None:
            for ev in need:
                sem, v = self._semval(ev)
                self.engs[eng].wait_ge(sem, v)
                self.ninstr += 1
            return None
        for ev in need[:-1]:
            sem, v = self._semval(ev)
            self.engs[eng].wait_ge(sem, v)
            self.ninstr += 1
        ins = ins_fn()
        if need:
            sem, v = self._semval(need[-1])
            ins._wait_ge(sem, v)
        return ins

    def _deps(self, eng, reads, writes):
        evs = []
        for b in reads:
            if b.w is not None:
                evs.append(b.w)
        for b in writes:
            if b.w is not None and b.w[0] != eng:
                evs.append(b.w)
            for k, n in b.r.items():
                if k != eng:
                    evs.append((k, n))
        return evs

    def op(self, eng, fn, reads=(), writes=(), multi=False):
        evs = self._deps(eng, reads, writes)
        if multi or eng == 'pe':
            self._emit_waits(eng, evs)
            ins = fn()
        else:
            ins = self._emit_waits(eng, evs, fn)
        n = self.cnt[eng]
        self.cnt[eng] += 1
        sem, _ = self._sem(eng, n)
        ins.then_inc(sem, 1)
        self.ninstr += 1
        for b in reads:
            b.r[eng] = n
        for b in writes:
            b.w = (eng, n)
            b.r = {}
        return ins

    def dma(self, q, out_ap, in_ap, reads=(), writes=(), **kw):
        dq = self.dq[q]
        i = dq['i'] % len(dq['sems'])
        dq['i'] += 1
        key = ('dma', q, i)
        evs = self._deps(q, reads, writes)
        if dq['cnt'][i] > 0:
            evs.append((key, dq['cnt'][i] - 1))
        self._emit_waits(q, evs)
        ins = self.engs[q].dma_start(out=out_ap, in_=in_ap, **kw)
        ins.then_inc(dq['sems'][i], 16)
        n = dq['cnt'][i]
        dq['cnt'][i] += 1
        self.ninstr += 1
        for b in reads:
            b.r[key] = n
        for b in writes:
            b.w = (key, n)
            b.r = {}

    def barrier(self):
        evs = []
        for k in self.ENG:
            if k != 'sp' and self.cnt[k] > 0:
                evs.append((k, self.cnt[k] - 1))
        for q, dq in self.dq.items():
            for i, c in enumerate(dq['cnt']):
                if c > 0:
                    evs.append((('dma', q, i), c - 1))
        self._emit_waits('sp', evs)
        ins = self.nc.sync.nop()
        n = self.cnt['sp']
        self.cnt['sp'] += 1
        sem, _ = self._sem('sp', n)
        ins.then_inc(sem, 1)
        self.ninstr += 1
        for k in self.ENG:
            if k != 'sp':
                self._emit_waits(k, [('sp', n)])
                for k2 in self.ENG:
                    if self.cnt[k2] > 0:
                        self.seen[k][k2] = max(self.seen[k].get(k2, -1), self.cnt[k2] - 1 if k2 != 'sp' else n)
                for q, dq in self.dq.items():
                    for i, c in enumerate(dq['cnt']):
                        if c > 0:
                            self.seen[k][('dma', q, i)] = c - 1


def bc_mid(ap, n):
    a = [list(x) for x in ap.ap]
    return AP(tensor=ap.tensor, offset=ap.offset, ap=[a[0], [0, n]] + a[1:])


def dram_ap(t, offset, pairs):
    return AP(tensor=t.tensor, offset=offset, ap=[list(p) for p in pairs])


def _t5_bucket_np(rel):
    nb = 16
    max_exact = 8
    ret = np.where(rel > 0, nb, 0)
    n = np.abs(rel)
    nf = np.maximum(n, 1).astype(np.float32)
    large = max_exact + (np.log(nf / np.float32(max_exact)) / np.float32(math.log(128 / max_exact))
                         * np.float32(nb - max_exact)).astype(np.int32)
    large = np.minimum(large, nb - 1)
    return ret + np.where(n < max_exact, n, large)


def host_consts(S):
    bf = ml_dtypes.bfloat16
    pa = np.zeros((128, 12, 128), np.float32)
    s = np.arange(128)[:, None]
    t = np.arange(128)[None, :]
    for gi, w in enumerate((2, 4, 8, 16)):
        inwin = ((s <= t) & (s > t - w)).astype(np.float32)
        pa[:, gi, :] = inwin / w - (s == t)
        cnt = np.minimum(t + 1, w).astype(np.float32)
        pa[:, 8 + gi, :] = inwin / cnt - (s == t)
        inprev = ((s - 128) > t - w).astype(np.float32)
        pa[:, 4 + gi, :] = inprev / w
    bm = np.zeros((128, 5, 128), np.float32)
    for d in range(5):
        diff = 2 * d + (t // 64) - (s // 64)
        bm[:, d, :] = ((diff >= 0) & (diff <= 8)).astype(np.float32)
    m = np.arange(384)
    bk = _t5_bucket_np((127 - m).astype(np.int32))
    oh = np.zeros((32, 384), np.float32)
    oh[bk, m] = 1.0
    half = 16
    freqs = (10000.0 ** (-np.arange(half, dtype=np.float32) / half)).astype(np.float32)
    ang = np.arange(S, dtype=np.float32)[None, :] * freqs[:, None]
    cs = np.zeros((2, 32, S), np.float32)
    cs[0, :16] = np.cos(ang)
    cs[0, 16:] = np.cos(ang)
    cs[1, :16] = -np.sin(ang)
    cs[1, 16:] = np.sin(ang)
    p2 = np.tile((0.5 ** np.arange(1, NBIS + 1, dtype=np.float32))[None, :], (128, 1)).astype(np.float32)
    return dict(c_poolA=pa.astype(bf), c_bandm=bm.astype(bf), c_t5oh=oh, c_cs=cs.astype(bf), c_pow2=p2)


def _in_cols():
    o = {}
    off = 0
    for n, w in (('pool_u', 256), ('ca_q', 256), ('ca_k', 256), ('ca_v', 256), ('sa_q', 256), ('sa_k', 64),
                 ('sa_v', 64), ('idx_q', 512), ('idx_k', 64), ('idx_w', 8), ('mla_cq', 256), ('mla_ckv', 128),
                 ('mla_kr', 32)):
        o[n] = np.arange(off, off + w)
        off += w
    return o


def layout_weights(inp, l):
    o = _in_cols()
    w_in = np.asarray(inp['w_in'][l])
    tm = np.concatenate([o['pool_u'], o['ca_v'], o['sa_v'], o['idx_w'], o['mla_cq'], o['mla_ckv']])
    w_tm = np.ascontiguousarray(w_in[:, tm])
    w_fm = np.zeros((D, FMW), np.float32)
    fm = np.concatenate([o['ca_q'], o['ca_k'], o['sa_q'], o['idx_q'], o['sa_k'], o['idx_k'], o['mla_cq'], o['mla_ckv']])
    w_fm[:, :fm.size] = w_in[:, fm]
    kr = o['mla_kr']
    w_fm[:, 1792 + 64:1792 + 96] = w_in[:, kr]
    w_fm[:, 1920 + 64:1920 + 96] = w_in[:, np.concatenate([kr[16:], kr[:16]])]
    wuq = np.asarray(inp['mla_w_uq'][l])
    wqa = np.ascontiguousarray(wuq.reshape(256, 384))
    wqb = np.zeros((256, 4, 96), np.float32)
    wqb[:, :, 64:80] = wuq[:, :, 80:96]
    wqb[:, :, 80:96] = wuq[:, :, 64:80]
    wukv = np.asarray(inp['mla_w_ukv'][l])
    wk = np.ascontiguousarray(wukv[:, :, :64].reshape(128, 256))
    wv = np.ascontiguousarray(wukv[:, :, 64:].reshape(128, 256))
    f = lambda a: np.ascontiguousarray(np.asarray(a), dtype=np.float32)
    return dict(
        w_mod=f(inp['w_mod'][l]), b_mod=f(inp['b_mod'][l]).reshape(1, -1), g_mix=f(inp['g_mix'][l]).reshape(1, -1),
        w_tm=w_tm, w_fm=w_fm, pool_w=f(inp['pool_w'][l]), pool_scale=f(inp['pool_scale'][l]).reshape(1, -1),
        ca_rel=f(inp['ca_rel'][l]), g_cq=f(inp['mla_g_cq'][l]).reshape(1, -1), g_ckv=f(inp['mla_g_ckv'][l]).reshape(1, -1),
        wqa=wqa, wqb=wqb.reshape(256, 384), wk=wk, wv=wv, g_group=f(inp['g_group'][l]).reshape(1, -1),
        w_out=f(inp['w_out'][l]), g_ffn=f(inp['g_ffn'][l]).reshape(1, -1), w1=f(inp['ffn_w1'][l]),
        w3=f(inp['ffn_w3'][l]), w2=f(inp['ffn_w2'][l]))


WSHAPES = dict(w_mod=[D, 6 * D], b_mod=[1, 6 * D], g_mix=[1, D], w_tm=[D, TMW], w_fm=[D, FMW], pool_w=[4, 64, 64],
               pool_scale=[1, 256], ca_rel=[4, 320], g_cq=[1, 256], g_ckv=[1, 128], wqa=[256, 384], wqb=[256, 384],
               wk=[128, 256], wv=[128, 256], g_group=[1, D], w_out=[D, D], g_ffn=[1, D], w1=[D, DFF], w3=[D, DFF],
               w2=[DFF, D])


def build(S, nseq, nlayer, steps, debug=False, phases=None):
    NT = S // 128
    NG = S // 512
    nc = bass.Bass("TRN2", target_bir_lowering=False)
    es = ExitStack()
    with es:
        def dram(name, shape, dt, kind="Internal"):
            return nc.dram_tensor(name, shape, dt, kind=kind).ap()

        x_in = dram("x", [nseq, S, D], F32, "ExternalInput")
        c_in = dram("c", [nseq, D], F32, "ExternalInput")
        t5_in = dram("t5", [32, 4], F32, "ExternalInput")
        gfin_in = dram("g_final", [1, D], F32, "ExternalInput")
        W = []
        for l in range(nlayer):
            W.append({k: dram(f"{k}_{l}", shp, F32, "ExternalInput") for k, shp in WSHAPES.items()})
        c_poolA = dram("c_poolA", [128, 12, 128], BF16, "ExternalInput")
        c_bandm = dram("c_bandm", [128, 5, 128], BF16, "ExternalInput")
        c_t5oh = dram("c_t5oh", [32, 384], F32, "ExternalInput")
        c_cs = dram("c_cs", [2, 32, S], BF16, "ExternalInput")
        c_pow2 = dram("c_pow2", [128, NBIS], F32, "ExternalInput")
        xo = dram("xo", [nseq, S, D], F32, "ExternalOutput")
        out = dram("out", [nseq, S, D], F32, "ExternalOutput")
        dk = "ExternalOutput" if debug else "Internal"
        ztm = dram("ztm", [S, 576], BF16, dk)
        zsm = dram("zsm", [S, 8], F32, dk)
        zrq = dram("zrq", [1, S], F32, dk)
        zrk = dram("zrk", [1, S], F32, dk)
        zfm = dram("zfm", [FMW, S], BF16, dk)
        ybuf = dram("ybuf", [S, D], F32, dk)
        h2T = dram("h2T", [D, S], BF16, dk)
        ext = dram("ext", [4, 768], F32, "Internal")
        t5v = dram("t5v", [4, 384], F32, "Internal")

        Sc = Sched(nc, es)
        op = Sc.op
        V, A, P, T = nc.vector, nc.scalar, nc.gpsimd, nc.tensor

        uid = [0]

        def sb(st, name, shape, dt):
            uid[0] += 1
            name = f"{name}_{uid[0]}"
            return Buf(st.enter_context(nc.sbuf_tensor(name, shape, dt)), name)

        def ps(st, name, dt=F32):
            uid[0] += 1
            name = f"{name}_{uid[0]}"
            shape = [128, 512] if dt == F32 else [128, 1024]
            return Buf(st.enter_context(nc.psum_tensor(name, shape, dt)), name)

        def want(p):
            return phases is None or p in phases

        ident = sb(es, "ident", [128, 128], BF16)
        identf = sb(es, "identf", [128, 128], F32)
        onesf = sb(es, "onesf", [128, 128], F32)
        epsT = sb(es, "epsT", [128, 1], F32)
        MOD = sb(es, "MOD", [128, 6, D], F32)
        op('pool', lambda: P.memset(identf[:], 1.0), writes=[identf])
        op('pool', lambda: P.affine_select(out=identf[:], in_=identf[:], pattern=[[-1, 128]], compare_op=ALU.is_equal,
                                           fill=0.0, base=0, channel_multiplier=1), reads=[identf], writes=[identf])
        op('dve', lambda: V.tensor_copy(out=ident[:], in_=identf[:]), reads=[identf], writes=[ident])
        Jf = sb(es, "Jf", [128, 128], F32)
        op('pool', lambda: P.memset(Jf[:], 1.0), writes=[Jf])
        op('pool', lambda: P.affine_select(out=Jf[:], in_=Jf[:], pattern=[[1, 128]], compare_op=ALU.is_equal,
                                           fill=0.0, base=-127, channel_multiplier=1), reads=[Jf], writes=[Jf])
        op('dve', lambda: V.memset(onesf[:], 1.0), writes=[onesf])
        op('dve', lambda: V.memset(epsT[:], EPS), writes=[epsT])

        rr = [0]

        def cast(out_ap, in_ap, reads, writes):
            k = rr[0] % 3
            rr[0] += 1
            if k == 0:
                op('pool', lambda: P.tensor_copy(out=out_ap, in_=in_ap), reads=reads, writes=writes)
            elif k == 1:
                op('act', lambda: A.copy(out=out_ap, in_=in_ap), reads=reads, writes=writes)
            else:
                op('dve', lambda: V.tensor_copy(out=out_ap, in_=in_ap), reads=reads, writes=writes)

        def rstd_from(ms, sq, rs, n=1):
            op('act', lambda: A.activation(out=sq[:, 0:n], in_=ms[:, 0:n], func=AF.Sqrt, bias=epsT[:, 0:1], scale=1.0),
               reads=[ms, epsT], writes=[sq])
            op('dve', lambda: V.reciprocal(out=rs[:, 0:n], in_=sq[:, 0:n]), reads=[sq], writes=[rs])

        for (seq, l, last) in steps:
            Wl = W[l]
            xin = x_in[seq] if l == 0 else xo[seq]
            xout = xo[seq]

            if want('mod'):
                with ExitStack() as ph:
                    cT = sb(ph, "cT", [128, 8], F32)
                    cact = sb(ph, "cact", [128, 8], F32)
                    cbc = sb(ph, "cbc", [128, 8, 128], F32)
                    wst = [sb(ph, f"wst{i}", [128, 8, 512], F32) for i in range(2)]
                    bst = [sb(ph, f"bst{i}", [128, 512], F32) for i in range(2)]
                    gbc = [sb(ph, f"gbc{i}", [128, D], F32) for i in range(2)]
                    pm = [ps(ph, f"pm{i}") for i in range(2)]
                    Sc.dma('sp', cT[:], dram_ap(c_in, seq * D, [[1, 128], [128, 8]]), writes=[cT],
                           allow_slow_non_contiguous=True)
                    Sc.dma('sp', gbc[0][:], Wl['g_mix'][0].partition_broadcast(128), writes=[gbc[0]])
                    Sc.dma('sp', gbc[1][:], Wl['g_ffn'][0].partition_broadcast(128), writes=[gbc[1]])
                    op('act', lambda: A.activation(out=cact[:], in_=cT[:], func=AF.Silu), reads=[cT], writes=[cact])
                    for kc in range(8):
                        op('dve', lambda: V.tensor_scalar(out=cbc[:, kc, :], in0=onesf[:], scalar1=cact[:, kc:kc + 1],
                                                          scalar2=None, op0=ALU.mult), reads=[onesf, cact], writes=[cbc])
                    MODf = MOD[:].rearrange("p a d -> p (a d)")
                    for n in range(12):
                        b = n % 2
                        Sc.dma('sp', wst[b][:], Wl['w_mod'][:, n * 512:(n + 1) * 512].rearrange("(k p) n -> p k n", p=128),
                               writes=[wst[b]])
                        Sc.dma('sp', bst[b][:], Wl['b_mod'][0, n * 512:(n + 1) * 512].partition_broadcast(128),
                               writes=[bst[b]])
                        for kc in range(8):
                            op('pe', lambda: T.matmul(pm[b][:], lhsT=cbc[:, kc, :], rhs=wst[b][:, kc, :], start=(kc == 0),
                                                      stop=(kc == 7)), reads=[cbc, wst[b]], writes=[pm[b]])
                        op('dve', lambda: V.tensor_tensor(out=MODf[:, n * 512:(n + 1) * 512], in0=pm[b][:], in1=bst[b][:],
                                                          op=ALU.add), reads=[pm[b], bst[b]], writes=[MOD])
                    for (j, g) in ((1, 0), (4, 1)):
                        op('dve', lambda: V.scalar_tensor_tensor(out=MOD[:, j, :], in0=MOD[:, j, :], scalar=1.0,
                                                                 in1=gbc[g][:], op0=ALU.add, op1=ALU.mult),
                           reads=[MOD, gbc[g]], writes=[MOD])
                Sc.barrier()

            if want('inproj'):
                with ExitStack() as ph:
                    WTM = sb(ph, "WTM", [128, 8, TMW], BF16)
                    WFM = sb(ph, "WFM", [128, 8, FMW], BF16)
                    stg = [sb(ph, f"stg{i}", [128, FMW], F32) for i in range(2)]
                    xb = [sb(ph, f"xb{i}", [128, D], F32) for i in range(2)]
                    t1 = sb(ph, "t1", [128, D], F32)
                    junk = sb(ph, "junk", [128, D], F32)
                    hbs = [sb(ph, f"hb{i}", [128, D], BF16) for i in range(2)]
                    hT4 = [sb(ph, f"hT4{i}", [128, 8, 512], BF16) for i in range(2)]
                    sts = [sb(ph, f"st{i}", [128, 8], F32) for i in range(2)]
                    stm = [sb(ph, f"stm{i}", [128, 576], BF16) for i in range(2)]
                    ssm = [sb(ph, f"ssm{i}", [128, 16], F32) for i in range(2)]
                    sfm = [sb(ph, f"sfm{i}", [128, 16, 512], BF16) for i in range(2)]
                    pT = ps(ph, "pT", BF16)
                    ptm = [ps(ph, f"ptm{i}") for i in range(2)]
                    pfm = [ps(ph, f"pfm{i}") for i in range(3)]
                    for kc in range(8):
                        b = kc % 2
                        Sc.dma('sp', stg[b][:, 0:TMW], Wl['w_tm'][kc * 128:(kc + 1) * 128, :], writes=[stg[b]])
                        cast(WTM[:, kc, :], stg[b][:, 0:TMW], [stg[b]], [WTM])
                    for kc in range(8):
                        b = kc % 2
                        Sc.dma('sp', stg[b][:], Wl['w_fm'][kc * 128:(kc + 1) * 128, :], writes=[stg[b]])
                        cast(WFM[:, kc, :], stg[b][:], [stg[b]], [WFM])
                    def pa_s1(i):
                        x_t = xb[i % 2]
                        hb = hbs[i % 2]
                        st = sts[i % 2]
                        Sc.dma('sp', x_t[:], xin[i * 128:(i + 1) * 128, :], writes=[x_t])
                        op('act', lambda: A.activation(out=junk[:], in_=x_t[:], func=AF.Square, scale=1.0 / 32.0,
                                                       accum_out=st[:, 0:1]), reads=[x_t], writes=[junk, st], multi=True)
                        op('act', lambda: A.activation(out=st[:, 1:2], in_=st[:, 0:1], func=AF.Sqrt, bias=epsT[:, 0:1],
                                                       scale=1.0), reads=[st, epsT], writes=[st])
                        op('dve', lambda: V.reciprocal(out=st[:, 2:3], in_=st[:, 1:2]), reads=[st], writes=[st])
                        op('dve', lambda: V.scalar_tensor_tensor(out=t1[:], in0=x_t[:], scalar=st[:, 2:3], in1=MOD[:, 1, :],
                                                                 op0=ALU.mult, op1=ALU.mult), reads=[x_t, st, MOD], writes=[t1])
                        op('dve', lambda: V.tensor_tensor(out=hb[:], in0=t1[:], in1=MOD[:, 0, :], op=ALU.add),
                           reads=[t1, MOD], writes=[hb])

                    def pa_s2(i):
                        g, r = i // 4, i % 4
                        hT = hT4[g % 2]
                        hb = hbs[i % 2]
                        for kc in range(8):
                            op('pe', lambda: T.transpose(out=pT[:, kc * 128:(kc + 1) * 128], in_=hb[:, kc * 128:(kc + 1) * 128],
                                                         identity=ident[:]), reads=[hb, ident], writes=[pT])
                        op('act', lambda: A.copy(out=hT[:, :, r * 128:(r + 1) * 128],
                                                 in_=pT[:].rearrange("p (k t) -> p k t", k=8)), reads=[pT], writes=[hT])
                        for kc in range(8):
                            op('pe', lambda: T.matmul(ptm[0][:], lhsT=hT[:, kc, r * 128:(r + 1) * 128], rhs=WTM[:, kc, 0:512],
                                                      start=(kc == 0), stop=(kc == 7)), reads=[hT, WTM], writes=[ptm[0]])
                        for kc in range(8):
                            op('pe', lambda: T.matmul(ptm[1][:, 0:456], lhsT=hT[:, kc, r * 128:(r + 1) * 128],
                                                      rhs=WTM[:, kc, 512:968], start=(kc == 0), stop=(kc == 7)),
                               reads=[hT, WTM], writes=[ptm[1]])
                        sm_, ss_ = stm[i % 2], ssm[i % 2]
                        op('act', lambda: A.copy(out=sm_[:, 0:512], in_=ptm[0][:]), reads=[ptm[0]], writes=[sm_])
                        op('dve', lambda: V.tensor_copy(out=sm_[:, 512:576], in_=ptm[1][:, 0:64]), reads=[ptm[1]], writes=[sm_])
                        op('dve', lambda: V.tensor_copy(out=ss_[:, 0:8], in_=ptm[1][:, 64:72]), reads=[ptm[1]], writes=[ss_])
                        op('act', lambda: A.activation(out=junk[:, 0:256], in_=ptm[1][:, 72:328], func=AF.Square,
                                                       scale=1.0 / 16.0, accum_out=ss_[:, 8:9]),
                           reads=[ptm[1]], writes=[junk, ss_], multi=True)
                        op('act', lambda: A.activation(out=junk[:, 0:128], in_=ptm[1][:, 328:456], func=AF.Square,
                                                       scale=1.0 / math.sqrt(128.0), accum_out=ss_[:, 9:10]),
                           reads=[ptm[1]], writes=[junk, ss_], multi=True)
                        op('act', lambda: A.activation(out=ss_[:, 10:12], in_=ss_[:, 8:10], func=AF.Sqrt, bias=epsT[:, 0:1],
                                                       scale=1.0), reads=[ss_, epsT], writes=[ss_])
                        op('dve', lambda: V.reciprocal(out=ss_[:, 12:14], in_=ss_[:, 10:12]), reads=[ss_], writes=[ss_])
                        Sc.dma('pool', ztm[i * 128:(i + 1) * 128, :], sm_[:], reads=[sm_])
                        Sc.dma('pool', zsm[i * 128:(i + 1) * 128, :], ss_[:, 0:8], reads=[ss_])
                        Sc.dma('pool', dram_ap(zrq, i * 128, [[1, 128], [1, 1]]), ss_[:, 12:13], reads=[ss_])
                        Sc.dma('pool', dram_ap(zrk, i * 128, [[1, 128], [1, 1]]), ss_[:, 13:14], reads=[ss_])
                        if r == 3:
                            sf = sfm[g % 2]
                            for ch in range(16):
                                pf = pfm[ch % 3]
                                for kc in range(8):
                                    op('pe', lambda: T.matmul(pf[:], lhsT=WFM[:, kc, ch * 128:(ch + 1) * 128], rhs=hT[:, kc, :],
                                                              start=(kc == 0), stop=(kc == 7)), reads=[WFM, hT], writes=[pf])
                                if ch % 2 == 0:
                                    op('act', lambda: A.copy(out=sf[:, ch, :], in_=pf[:]), reads=[pf], writes=[sf])
                                else:
                                    op('dve', lambda: V.tensor_copy(out=sf[:, ch, :], in_=pf[:]), reads=[pf], writes=[sf])
                            Sc.dma('pool', zfm[:, g * 512:(g + 1) * 512].rearrange("(c p) t -> p c t", p=128), sf[:], reads=[sf])
                    pa_s1(0)
                    for i in range(NT):
                        if i + 1 < NT:
                            pa_s1(i + 1)
                        pa_s2(i)
                Sc.barrier()

            if want('pool'):
                with ExitStack() as ph:
                    U = sb(ph, "U", [128, NT, 256], BF16)
                    PA = sb(ph, "PA", [128, 12, 128], BF16)
                    pwf = sb(ph, "pwf", [64, 4, 64], F32)
                    scb = sb(ph, "scb", [64, 256], F32)
                    PW = sb(ph, "PW", [64, 4, 64], BF16)
                    dT = [sb(ph, f"dT{i}", [64, 4, 128], BF16) for i in range(2)]
                    yst = [sb(ph, f"yst{i}", [128, 256], F32) for i in range(2)]
                    pd = [ps(ph, f"pd{i}") for i in range(2)]
                    py = [ps(ph, f"py{i}") for i in range(2)]
                    Sc.dma('sp', U[:], ztm[:, 0:256].rearrange("(n p) c -> p n c", p=128), writes=[U])
                    Sc.dma('sp', PA[:], c_poolA, writes=[PA])
                    Sc.dma('sp', pwf[:], Wl['pool_w'].rearrange("g c d -> c g d"), writes=[pwf])
                    Sc.dma('sp', scb[:], Wl['pool_scale'][0].partition_broadcast(64), writes=[scb])
                    op('dve', lambda: V.tensor_tensor(out=PW[:], in0=pwf[:], in1=scb[:].rearrange("p (g d) -> p g d", g=4),
                                                      op=ALU.mult), reads=[pwf, scb], writes=[PW])
                    for i in range(NT):
                        b = i % 2
                        for g in range(4):
                            a0 = 8 + g if i == 0 else g
                            op('pe', lambda: T.matmul(pd[b][0:64, g * 128:(g + 1) * 128], lhsT=U[:, i, g * 64:(g + 1) * 64],
                                                      rhs=PA[:, a0, :], start=True, stop=(i == 0)), reads=[U, PA], writes=[pd[b]])
                            if i > 0:
                                op('pe', lambda: T.matmul(pd[b][0:64, g * 128:(g + 1) * 128], lhsT=U[:, i - 1, g * 64:(g + 1) * 64],
                                                          rhs=PA[:, 4 + g, :], start=False, stop=True), reads=[U, PA], writes=[pd[b]])
                        op('act', lambda: A.copy(out=dT[b][:], in_=pd[b][0:64, :].rearrange("p (g t) -> p g t", g=4)),
                           reads=[pd[b]], writes=[dT[b]])
                        for g in range(4):
                            op('pe', lambda: T.matmul(py[b][:, g * 64:(g + 1) * 64], lhsT=dT[b][:, g, :], rhs=PW[:, g, :],
                                                      start=True, stop=True), reads=[dT[b], PW], writes=[py[b]])
                        op('dve', lambda: V.tensor_copy(out=yst[b][:], in_=py[b][:, 0:256]), reads=[py[b]], writes=[yst[b]])
                        Sc.dma('pool', ybuf[i * 128:(i + 1) * 128, 0:256], yst[b][:], reads=[yst[b]])
                Sc.barrier()

            if want('band'):
                with ExitStack() as ph:
                    KT = sb(ph, "KT", [64, 4, S], BF16)
                    QT = sb(ph, "QT", [64, 4, S], BF16)
                    VA = sb(ph, "VA", [128, NT, 4, 65], BF16)
                    raw = sb(ph, "raw", [128, 20, 128], F32)
                    raw2 = sb(ph, "raw2", [128, 20, 128], F32)
                    EB = sb(ph, "EB", [128, 20, 128], BF16)
                    bmk = sb(ph, "bmk", [128, 5, 128], BF16)
                    E = [sb(ph, f"E{i}", [128, 20, 128], BF16) for i in range(2)]
                    Pm = [sb(ph, f"Pm{i}", [128, 20, 128], BF16) for i in range(2)]
                    rden = sb(ph, "rden", [128, 4], F32)
                    yst = [sb(ph, f"ystb{i}", [128, 4, 64], F32) for i in range(2)]
                    SCp = [ps(ph, f"SCp{i}") for i in range(5)]
                    Op = [ps(ph, f"Op{i}") for i in range(2)]
                    Sc.dma('sp', QT[:], zfm[0:256, :].rearrange("(h d) t -> d h t", h=4), writes=[QT])
                    Sc.dma('sp', KT[:], zfm[256:512, :].rearrange("(h d) t -> d h t", h=4), writes=[KT])
                    for h in range(4):
                        Sc.dma('sp', VA[:, :, h, 0:64], ztm[:, 256 + h * 64:256 + (h + 1) * 64].rearrange("(n p) d -> p n d", p=128),
                               writes=[VA])
                    op('pool', lambda: P.memset(VA[:, :, :, 64:65], 1.0), writes=[VA])
                    Sc.dma('sp', bmk[:], c_bandm, writes=[bmk])
                    crs = sb(ph, "crs", [4, 320], F32)
                    exs = sb(ph, "exs", [4, 768], F32)
                    Sc.dma('sp', crs[:], Wl['ca_rel'], writes=[crs])
                    op('dve', lambda: V.tensor_copy(out=exs[:, 64:384], in_=crs[:]), reads=[crs], writes=[exs])
                    op('dve', lambda: V.tensor_copy(out=exs[:, 0:64], in_=crs[:, 0:1].to_broadcast([4, 64])), reads=[crs], writes=[exs])
                    op('dve', lambda: V.tensor_copy(out=exs[:, 384:768], in_=crs[:, 319:320].to_broadcast([4, 384])),
                       reads=[crs], writes=[exs])
                    Sc.dma('sp', ext, exs[:], reads=[exs])
                    Sc.barrier()
                    for h in range(4):
                        Sc.dma('sp', raw[:, h * 5:(h + 1) * 5, :], dram_ap(ext, h * 768, [[1, 128], [1, 640]]), writes=[raw])
                    for h in range(4):
                        op('pe', lambda: T.matmul(SCp[0][:], lhsT=Jf[:], rhs=raw[:, h * 5:h * 5 + 4, :].rearrange("p a t -> p (a t)"),
                                                  start=True, stop=True), reads=[Jf, raw], writes=[SCp[0]])
                        op('pe', lambda: T.matmul(SCp[1][:, 0:128], lhsT=Jf[:], rhs=raw[:, h * 5 + 4, :], start=True, stop=True),
                           reads=[Jf, raw], writes=[SCp[1]])
                        op('act', lambda: A.activation(out=raw2[:, h * 5:h * 5 + 4, :], in_=SCp[0][:].rearrange("p (a t) -> p a t", a=4),
                                                       func=AF.Exp), reads=[SCp[0]], writes=[raw2])
                        op('act', lambda: A.activation(out=raw2[:, h * 5 + 4, :], in_=SCp[1][:, 0:128], func=AF.Exp),
                           reads=[SCp[1]], writes=[raw2])
                        op('dve', lambda: V.tensor_tensor(out=EB[:, h * 5:(h + 1) * 5, :], in0=raw2[:, h * 5:(h + 1) * 5, :],
                                                          in1=bmk[:], op=ALU.mult), reads=[raw2, bmk], writes=[EB])
                    for i in range(NT):
                        b = i % 2
                        nd = min(i, 4) + 1
                        for h in range(4):
                            for d in range(nd):
                                j = i - d
                                blk = h * 5 + d
                                pb = SCp[blk // 4]
                                op('pe', lambda: T.matmul(pb[:, (blk % 4) * 128:(blk % 4 + 1) * 128], lhsT=KT[:, h, j * 128:(j + 1) * 128],
                                                          rhs=QT[:, h, i * 128:(i + 1) * 128], start=True, stop=True),
                                   reads=[KT, QT], writes=[pb])
                        for k in range(5):
                            op('act', lambda: A.activation(out=E[b][:, 4 * k:4 * k + 4, :],
                                                           in_=SCp[k][:].rearrange("p (a t) -> p a t", a=4), func=AF.Exp,
                                                           scale=0.125), reads=[SCp[k]], writes=[E[b]])
                        for hh in range(2):
                            op('dve', lambda: V.tensor_tensor(out=Pm[b][:, hh * 10:(hh + 1) * 10, :], in0=E[b][:, hh * 10:(hh + 1) * 10, :],
                                                              in1=EB[:, hh * 10:(hh + 1) * 10, :], op=ALU.mult),
                               reads=[E[b], EB], writes=[Pm[b]])
                        for h in range(4):
                            for d in range(nd):
                                j = i - d
                                op('pe', lambda: T.matmul(Op[b][:, h * 65:(h + 1) * 65], lhsT=Pm[b][:, h * 5 + d, :],
                                                          rhs=VA[:, j, h, :], start=(d == 0), stop=(d == nd - 1)),
                                   reads=[Pm[b], VA], writes=[Op[b]])
                        ov = Op[b][:, 0:260].rearrange("p (h d) -> p h d", h=4)
                        op('dve', lambda: V.reciprocal(out=rden[:], in_=ov[:, :, 64]), reads=[Op[b]], writes=[rden])
                        op('dve', lambda: V.tensor_tensor(out=yst[b][:], in0=ov[:, :, 0:64],
                                                          in1=rden[:].unsqueeze(2).to_broadcast([128, 4, 64]), op=ALU.mult),
                           reads=[Op[b], rden], writes=[yst[b]])
                        Sc.dma('pool', ybuf[i * 128:(i + 1) * 128, 256:512], yst[b][:].rearrange("p h d -> p (h d)"), reads=[yst[b]])
                Sc.barrier()

            if want('dsa'):
                with ExitStack() as ph:
                    BIG = 30000.0
                    IKT = sb(ph, "IKT", [64, S], BF16)
                    SKT = sb(ph, "SKT", [64, S], BF16)
                    VC = sb(ph, "VC", [128, NT, 65], BF16)
                    IQT = [sb(ph, f"IQT{i}", [64, 8, 128], BF16) for i in range(2)]
                    SQT = [sb(ph, f"SQT{i}", [64, 4, 128], BF16) for i in range(3)]
                    IW = [sb(ph, f"IW{i}", [128, 8], F32) for i in range(2)]
                    Dg = [sb(ph, f"Dg{i}", [128, 8, 128], BF16) for i in range(2)]
                    R = [sb(ph, f"R{i}", [128, 512], BF16) for i in range(3)]
                    SCs = [sb(ph, f"SCs{i}", [128, S], F32) for i in range(2)]
                    cjunk = sb(ph, "cjunk", [128, S], BF16)
                    MK = [sb(ph, f"MK{i}", [128, S], BF16) for i in range(2)]
                    PEN = sb(ph, "PEN", [128, NT, 128], BF16)
                    T5B = sb(ph, "T5B", [128, 8, 128], BF16)
                    rawt = sb(ph, "rawt", [128, 8, 128], F32)
                    t5s = sb(ph, "t5s", [32, 4], F32)
                    ohs = sb(ph, "ohs", [32, 384], F32)
                    t5r = sb(ph, "t5r", [4, 384], F32)
                    t5bc = sb(ph, "t5bc", [128, 128], F32)
                    nt5 = sb(ph, "nt5", [128, 4], F32)
                    negbig = sb(ph, "negbig", [128, 1], F32)
                    pw2 = sb(ph, "pw2", [128, NBIS], F32)
                    wts = sb(ph, "wts", [128, NBIS], F32)
                    bs = sb(ph, "bs", [128, 8], F32)
                    Eb = [sb(ph, f"Eb{i}", [128, 512], BF16) for i in range(3)]
                    rden = sb(ph, "rdenc", [128, 4], F32)
                    yst = [sb(ph, f"ystc{i}", [128, 4, 64], F32) for i in range(2)]
                    Lp = [ps(ph, f"Lp{i}") for i in range(2)]
                    SPp = ps(ph, "SPp")
                    MTp = ps(ph, "MTp", BF16)
                    STp = [ps(ph, f"STp{i}") for i in range(2)]
                    Op = [ps(ph, f"Opc{i}") for i in range(2)]
                    Sc.dma('sp', SKT[:], zfm[1280:1344, :], writes=[SKT])
                    Sc.dma('sp', IKT[:], zfm[1344:1408, :], writes=[IKT])
                    Sc.dma('sp', VC[:, :, 0:64], ztm[:, 512:576].rearrange("(n p) d -> p n d", p=128), writes=[VC])
                    op('pool', lambda: P.memset(VC[:, :, 64:65], 1.0), writes=[VC])
                    op('pool', lambda: P.memset(negbig[:], -BIG), writes=[negbig])
                    Sc.dma('sp', pw2[:], c_pow2, writes=[pw2])
                    Sc.dma('sp', t5s[:], t5_in, writes=[t5s])
                    Sc.dma('sp', ohs[:], c_t5oh, writes=[ohs])
                    Sc.dma('sp', t5bc[:], t5_in.rearrange("a b -> (a b)").partition_broadcast(128), writes=[t5bc])
                    op('pe', lambda: T.matmul(SPp[0:4, 0:384], lhsT=t5s[:], rhs=ohs[:], start=True, stop=True),
                       reads=[t5s, ohs], writes=[SPp])
                    op('dve', lambda: V.tensor_copy(out=t5r[:], in_=SPp[0:4, 0:384]), reads=[SPp], writes=[t5r])
                    Sc.dma('sp', t5v, t5r[:], reads=[t5r])
                    op('dve', lambda: V.tensor_scalar(out=nt5[:], in0=t5bc[:, 60:64], scalar1=-8.0, scalar2=None, op0=ALU.mult),
                       reads=[t5bc], writes=[nt5])
                    Sc.barrier()
                    for dd in range(2):
                        for h in range(4):
                            Sc.dma('sp', rawt[:, dd * 4 + h, :], dram_ap(t5v, h * 384 + 128 * dd, [[1, 128], [1, 128]]),
                                   writes=[rawt])
                    for dd in range(2):
                        op('pe', lambda: T.matmul(Lp[dd][:], lhsT=Jf[:], rhs=rawt[:, dd * 4:dd * 4 + 4, :].rearrange("p a t -> p (a t)"),
                                                  start=True, stop=True), reads=[Jf, rawt], writes=[Lp[dd]])
                        for h in range(4):
                            op('act', lambda: A.activation(out=T5B[:, dd * 4 + h, :], in_=Lp[dd][:, h * 128:(h + 1) * 128],
                                                           func=AF.Identity, bias=nt5[:, h:h + 1], scale=8.0),
                               reads=[Lp[dd], nt5], writes=[T5B])
                    rcnt = [0]

                    def stageA(i):
                        b = i % 2
                        N = 128 * (i + 1)
                        sq = SQT[i % 3]
                        Sc.dma('sp', IQT[b][:], zfm[768:1280, i * 128:(i + 1) * 128].rearrange("(h d) t -> d h t", h=8), writes=[IQT[b]])
                        Sc.dma('sp', sq[:], zfm[512:768, i * 128:(i + 1) * 128].rearrange("(h d) t -> d h t", h=4), writes=[sq])
                        Sc.dma('sp', IW[b][:], zsm[i * 128:(i + 1) * 128, :], writes=[IW[b]])
                        for h in range(8):
                            op('pool', lambda: P.tensor_scalar(out=Dg[b][:, h, :], in0=ident[:], scalar1=IW[b][:, h:h + 1],
                                                               scalar2=None, op0=ALU.mult), reads=[ident, IW[b]], writes=[Dg[b]])
                        nblk = (N + 511) // 512
                        for kb in range(nblk):
                            wb = min(512, N - kb * 512)
                            base = rcnt[0]

                            def acc(h):
                                rb = R[(base + h) % 3]
                                op('pe', lambda: T.matmul(SPp[:, 0:wb], lhsT=Dg[b][:, h, :], rhs=rb[:, 0:wb], start=(h == 0),
                                                          stop=(h == 7)), reads=[Dg[b], rb], writes=[SPp])
                            for h in range(8):
                                lp = Lp[(base + h) % 2]
                                rb = R[(base + h) % 3]
                                op('pe', lambda: T.matmul(lp[:, 0:wb], lhsT=IQT[b][:, h, :], rhs=IKT[:, kb * 512:kb * 512 + wb],
                                                          start=True, stop=True), reads=[IQT[b], IKT], writes=[lp])
                                op('act', lambda: A.activation(out=rb[:, 0:wb], in_=lp[:, 0:wb], func=AF.Relu),
                                   reads=[lp], writes=[rb])
                                if h >= 1:
                                    acc(h - 1)
                            acc(7)
                            rcnt[0] += 8
                            op('act', lambda: A.copy(out=SCs[b][:, kb * 512:kb * 512 + wb], in_=SPp[:, 0:wb]), reads=[SPp], writes=[SCs[b]])
                        op('pool', lambda: P.memset(SCs[b][0:64, N - 64:N], NEG), writes=[SCs[b]])

                    def stageB(i):
                        b = i % 2
                        N = 128 * (i + 1)
                        sc = SCs[b]
                        if i >= 2:
                            op('dve', lambda: V.tensor_reduce(out=bs[:, 0:1], in_=sc[:, 0:N], axis=AX.X, op=ALU.max),
                               reads=[sc], writes=[bs])
                            op('dve', lambda: V.tensor_reduce(out=bs[:, 3:4], in_=sc[:, 0:N - 64], axis=AX.X, op=ALU.min),
                               reads=[sc], writes=[bs])
                            op('dve', lambda: V.tensor_tensor(out=bs[:, 2:3], in0=bs[:, 0:1], in1=bs[:, 3:4], op=ALU.subtract),
                               reads=[bs], writes=[bs])
                            op('dve', lambda: V.tensor_scalar(out=wts[:], in0=pw2[:], scalar1=bs[:, 2:3], scalar2=None,
                                                              op0=ALU.mult), reads=[pw2, bs], writes=[wts])
                            for it in range(NBIS):
                                op('dve', lambda: V.tensor_tensor(out=bs[:, 4:5], in0=bs[:, 3:4], in1=wts[:, it:it + 1], op=ALU.add),
                                   reads=[bs, wts], writes=[bs])
                                op('dve', lambda: V.tensor_scalar(out=cjunk[:, 0:N], in0=sc[:, 0:N], scalar1=bs[:, 4:5], scalar2=0.0,
                                                                  op0=ALU.is_ge, op1=ALU.add, accum_out=bs[:, 5:6]),
                                   reads=[sc, bs], writes=[cjunk, bs], multi=True)
                                op('dve', lambda: V.tensor_scalar(out=bs[:, 6:7], in0=bs[:, 5:6], scalar1=TOPK - 0.5,
                                                                  scalar2=wts[:, it:it + 1], op0=ALU.is_gt, op1=ALU.mult),
                                   reads=[bs, wts], writes=[bs])
                                op('dve', lambda: V.tensor_tensor(out=bs[:, 3:4], in0=bs[:, 3:4], in1=bs[:, 6:7], op=ALU.add),
                                   reads=[bs], writes=[bs])
                        else:
                            op('dve', lambda: V.memset(bs[:, 3:4], -1.0e29), writes=[bs])
                        op('dve', lambda: V.tensor_scalar(out=MK[b][:, 0:N], in0=sc[:, 0:N], scalar1=bs[:, 3:4], scalar2=None,
                                                          op0=ALU.is_ge), reads=[sc, bs], writes=[MK[b]])

                    ccnt = [0]

                    def stageC(i):
                        b = i % 2
                        mk = MK[b]
                        sq = SQT[i % 3]
                        for j0 in range(0, i + 1, 8):
                            nb = min(8, i + 1 - j0)
                            for jj in range(nb):
                                j = j0 + jj
                                op('pe', lambda: T.transpose(out=MTp[:, jj * 128:(jj + 1) * 128], in_=mk[:, j * 128:(j + 1) * 128],
                                                             identity=ident[:]), reads=[mk, ident], writes=[MTp])
                            op('act', lambda: A.activation(out=PEN[:, j0:j0 + nb, :],
                                                           in_=MTp[:, 0:nb * 128].rearrange("p (a t) -> p a t", a=nb),
                                                           func=AF.Identity, bias=negbig[:, 0:1], scale=BIG),
                               reads=[MTp, negbig], writes=[PEN])
                        ob = Op[b]
                        k0 = ccnt[0]
                        ccnt[0] += i + 1

                        def qk(j):
                            k = k0 + j
                            sp_, eb_ = STp[k % 2], Eb[k % 3]
                            op('pe', lambda: T.matmul(sp_[:], lhsT=SKT[:, j * 128:(j + 1) * 128],
                                                      rhs=sq[:].rearrange("p h t -> p (h t)"), start=True, stop=False),
                               reads=[SKT, sq], writes=[sp_])
                            near = i - j
                            op('pe', lambda: T.matmul(sp_[:], lhsT=ident[:], rhs=bc_mid(PEN[:, j, :], 4), start=False,
                                                      stop=(near > 1)), reads=[ident, PEN], writes=[sp_])
                            if near <= 1:
                                op('pe', lambda: T.matmul(sp_[:], lhsT=ident[:],
                                                          rhs=T5B[:, near * 4:near * 4 + 4, :].rearrange("p a t -> p (a t)"),
                                                          start=False, stop=True), reads=[ident, T5B], writes=[sp_])
                            op('act', lambda: A.activation(out=eb_[:], in_=sp_[:], func=AF.Exp, scale=0.125), reads=[sp_], writes=[eb_])

                        def pv(j):
                            eb_ = Eb[(k0 + j) % 3]
                            for h in range(4):
                                op('pe', lambda: T.matmul(ob[:, h * 65:(h + 1) * 65], lhsT=eb_[:, h * 128:(h + 1) * 128], rhs=VC[:, j, :],
                                                          start=(j == 0 and h == 0), stop=(j == i), skip_group_check=True),
                                   reads=[eb_, VC], writes=[ob])

                        qk(0)
                        for j in range(i + 1):
                            if j + 1 <= i:
                                qk(j + 1)
                            pv(j)
                        ov = ob[:, 0:260].rearrange("p (h d) -> p h d", h=4)
                        op('dve', lambda: V.reciprocal(out=rden[:], in_=ov[:, :, 64]), reads=[ob], writes=[rden])
                        op('dve', lambda: V.tensor_tensor(out=yst[b][:], in0=ov[:, :, 0:64],
                                                          in1=rden[:].unsqueeze(2).to_broadcast([128, 4, 64]), op=ALU.mult),
                           reads=[ob, rden], writes=[yst[b]])
                        Sc.dma('pool', ybuf[i * 128:(i + 1) * 128, 512:768], yst[b][:].rearrange("p h d -> p (h d)"), reads=[yst[b]])

                    stageA(0)
                    if NT > 1:
                        stageA(1)
                    stageB(0)
                    for i in range(NT):
                        if i + 2 < NT:
                            stageA(i + 2)
                        if i + 1 < NT:
                            stageB(i + 1)
                        stageC(i)
                Sc.barrier()

            if want('mla'):
                with ExitStack() as ph:
                    CQTb = [sb(ph, f"CQT{i}", [128, 2, 512], BF16) for i in range(2)]
                    CKTb = [sb(ph, f"CKT{i}", [128, 512], BF16) for i in range(2)]
                    KRRb = [sb(ph, f"KRR{i}", [96, 2, 512], BF16) for i in range(2)]
                    CSb = [sb(ph, f"CS{i}", [96, 2, 512], BF16) for i in range(2)]
                    QT = sb(ph, "QTd", [96, 4, S], BF16)
                    KT = sb(ph, "KTd", [96, 4, S], BF16)
                    VD = sb(ph, "VD", [128, NT, 4, 65], BF16)
                    wst = sb(ph, "wstd", [128, 2, 384], F32)
                    gq = sb(ph, "gq", [128, 2], F32)
                    gk = sb(ph, "gk", [128, 1], F32)
                    WQA = sb(ph, "WQA", [128, 2, 384], BF16)
                    WQB = sb(ph, "WQB", [128, 2, 384], BF16)
                    WK = sb(ph, "WK", [128, 256], BF16)
                    WV = sb(ph, "WV", [128, 256], BF16)
                    RKc = sb(ph, "RKc", [128, NT], F32)
                    RQb = [sb(ph, f"RQb{i}", [96, 512], F32) for i in range(2)]
                    RKb = [sb(ph, f"RKb{i}", [64, 512], F32) for i in range(2)]
                    cr = sb(ph, "cr", [96, 512], F32)
                    sr = sb(ph, "sr", [96, 512], F32)
                    ta = sb(ph, "ta", [96, 512], F32)
                    tb = sb(ph, "tb", [96, 512], F32)
                    tk1 = sb(ph, "tk1", [96, 512], F32)
                    tk2 = sb(ph, "tk2", [96, 512], F32)
                    Eb = [sb(ph, f"Ed{i}", [128, 512], BF16) for i in range(3)]
                    rden = sb(ph, "rdend", [128, 4], F32)
                    yst = [sb(ph, f"ystd{i}", [128, 4, 64], F32) for i in range(2)]
                    PAp = [ps(ph, f"PAp{i}") for i in range(2)]
                    PBp = [ps(ph, f"PBp{i}") for i in range(2)]
                    STp = [ps(ph, f"STd{i}") for i in range(2)]
                    Op = [ps(ph, f"Opd{i}") for i in range(2)]
                    Sc.dma('sp', RKc[:], dram_ap(zrk, 0, [[1, 128], [128, NT]]), writes=[RKc], allow_slow_non_contiguous=True)
                    Sc.dma('sp', gq[:], dram_ap(Wl['g_cq'], 0, [[1, 128], [128, 2]]), writes=[gq], allow_slow_non_contiguous=True)
                    Sc.dma('sp', gk[:], dram_ap(Wl['g_ckv'], 0, [[1, 128], [1, 1]]), writes=[gk])
                    for (src, dst) in ((Wl['wqa'], WQA), (Wl['wqb'], WQB)):
                        Sc.dma('sp', wst[:], src.rearrange("(c p) n -> p c n", p=128), writes=[wst])
                        for c2 in range(2):
                            op('dve', lambda: V.tensor_scalar(out=dst[:, c2, :], in0=wst[:, c2, :], scalar1=gq[:, c2:c2 + 1],
                                                              scalar2=None, op0=ALU.mult), reads=[wst, gq], writes=[dst])
                    for (src, dst) in ((Wl['wk'], WK), (Wl['wv'], WV)):
                        Sc.dma('sp', wst[:, 0, 0:256], src, writes=[wst])
                        op('dve', lambda: V.tensor_scalar(out=dst[:], in0=wst[:, 0, 0:256], scalar1=gk[:, 0:1], scalar2=None,
                                                          op0=ALU.mult), reads=[wst, gk], writes=[dst])
                    op('pool', lambda: P.memset(VD[:, :, :, 64:65], 1.0), writes=[VD])
                    for m in range(NG):
                        b = m % 2
                        gblk = slice(m * 512, (m + 1) * 512)
                        blk = slice(0, 512)
                        CQT, CKT, KRR, CS = CQTb[b], CKTb[b], KRRb[b], CSb[b]
                        Sc.dma('sp', CQT[:], zfm[1408:1664, gblk].rearrange("(c p) t -> p c t", p=128), writes=[CQT])
                        Sc.dma('sp', CKT[:], zfm[1664:1792, gblk], writes=[CKT])
                        Sc.dma('sp', KRR[64:96, 0, :], zfm[1792 + 64:1792 + 96, gblk], writes=[KRR])
                        Sc.dma('sp', KRR[64:96, 1, :], zfm[1920 + 64:1920 + 96, gblk], writes=[KRR])
                        Sc.dma('sp', CS[64:96, 0, :], c_cs[0, :, gblk], writes=[CS])
                        Sc.dma('sp', CS[64:96, 1, :], c_cs[1, :, gblk], writes=[CS])
                        Sc.dma('sp', RQb[b][:], zrq[0, gblk].partition_broadcast(96), writes=[RQb[b]])
                        Sc.dma('sp', RKb[b][:], zrk[0, gblk].partition_broadcast(64), writes=[RKb[b]])
                        op('dve', lambda: V.tensor_tensor(out=cr[64:96, :], in0=CS[64:96, 0, blk], in1=RQb[b][64:96, :], op=ALU.mult),
                           reads=[CS, RQb[b]], writes=[cr])
                        op('dve', lambda: V.tensor_tensor(out=sr[64:96, :], in0=CS[64:96, 1, blk], in1=RQb[b][64:96, :], op=ALU.mult),
                           reads=[CS, RQb[b]], writes=[sr])
                        for h in range(4):
                            pa, pb2 = PAp[h % 2], PBp[h % 2]
                            for c2 in range(2):
                                op('pe', lambda: T.matmul(pa[0:96, :], lhsT=WQA[:, c2, h * 96:(h + 1) * 96], rhs=CQT[:, c2, blk],
                                                          start=(c2 == 0), stop=(c2 == 1)), reads=[WQA, CQT], writes=[pa])
                            for c2 in range(2):
                                op('pe', lambda: T.matmul(pb2[0:96, :], lhsT=WQB[:, c2, h * 96:(h + 1) * 96], rhs=CQT[:, c2, blk],
                                                          start=(c2 == 0), stop=(c2 == 1)), reads=[WQB, CQT], writes=[pb2])
                            op('dve', lambda: V.tensor_tensor(out=QT[0:64, h, gblk], in0=pa[0:64, :], in1=RQb[b][0:64, :], op=ALU.mult),
                               reads=[pa, RQb[b]], writes=[QT])
                            op('dve', lambda: V.tensor_tensor(out=ta[64:96, :], in0=pa[64:96, :], in1=cr[64:96, :], op=ALU.mult),
                               reads=[pa, cr], writes=[ta])
                            op('dve', lambda: V.tensor_tensor(out=tb[64:96, :], in0=pb2[64:96, :], in1=sr[64:96, :], op=ALU.mult),
                               reads=[pb2, sr], writes=[tb])
                            op('dve', lambda: V.tensor_tensor(out=QT[64:96, h, gblk], in0=ta[64:96, :], in1=tb[64:96, :], op=ALU.add),
                               reads=[ta, tb], writes=[QT])
                        for h in range(4):
                            pa = PAp[h % 2]
                            op('pe', lambda: T.matmul(pa[0:64, :], lhsT=WK[:, h * 64:(h + 1) * 64], rhs=CKT[:, blk], start=True,
                                                      stop=True), reads=[WK, CKT], writes=[pa])
                            op('dve', lambda: V.tensor_tensor(out=KT[0:64, h, gblk], in0=pa[0:64, :], in1=RKb[b][0:64, :], op=ALU.mult),
                               reads=[pa, RKb[b]], writes=[KT])
                        op('dve', lambda: V.tensor_tensor(out=tk1[64:96, :], in0=KRR[64:96, 0, blk], in1=CS[64:96, 0, blk], op=ALU.mult),
                           reads=[KRR, CS], writes=[tk1])
                        op('dve', lambda: V.tensor_tensor(out=tk2[64:96, :], in0=KRR[64:96, 1, blk], in1=CS[64:96, 1, blk], op=ALU.mult),
                           reads=[KRR, CS], writes=[tk2])
                        for h in range(4):
                            op('dve', lambda: V.tensor_tensor(out=KT[64:96, h, gblk], in0=tk1[64:96, :], in1=tk2[64:96, :], op=ALU.add),
                               reads=[tk1, tk2], writes=[KT])
                        for r in range(4):
                            tl = m * 4 + r
                            pv = PBp[r % 2]
                            op('pe', lambda: T.matmul(pv[:, 0:256], lhsT=CKT[:, r * 128:(r + 1) * 128], rhs=WV[:], start=True,
                                                      stop=True), reads=[CKT, WV], writes=[pv])
                            op('dve', lambda: V.tensor_scalar(out=VD[:, tl, :, 0:64], in0=pv[:, 0:256].rearrange("p (h d) -> p h d", h=4),
                                                              scalar1=RKc[:, tl:tl + 1], scalar2=None, op0=ALU.mult),
                               reads=[pv, RKc], writes=[VD])
                    Sc.barrier()
                    sc_mla = 1.0 / math.sqrt(96.0)
                    STq = [STp[0], STp[1], PAp[0], PAp[1]]
                    Ebq = Eb + [sb(ph, "Ed3", [128, 512], BF16)]
                    items = [(h, m, j) for h in range(4) for m in range(NG) for j in range(4 * m + 4)]

                    def qk(k):
                        h, m, j = items[k]
                        rlo = max(0, j - 4 * m)
                        tlo = rlo * 128
                        n = 512 - tlo
                        sp_, eb_ = STq[k % 4], Ebq[k % 4]
                        op('pe', lambda: T.matmul(sp_[:, 0:n], lhsT=KT[:, h, j * 128:(j + 1) * 128],
                                                  rhs=QT[:, h, m * 512 + tlo:(m + 1) * 512], start=True, stop=True),
                           reads=[KT, QT], writes=[sp_])
                        op('act', lambda: A.activation(out=eb_[:, 0:n], in_=sp_[:, 0:n], func=AF.Exp, scale=sc_mla),
                           reads=[sp_], writes=[eb_])
                        if j >= 4 * m:
                            op('pool', lambda: P.memset(eb_[64:128, 0:64], 0.0), writes=[eb_])

                    def pv(k):
                        h, m, j = items[k]
                        rlo = max(0, j - 4 * m)
                        eb_ = Ebq[k % 4]
                        ob = Op[(h * NG + m) % 2]
                        for r in range(rlo, 4):
                            op('pe', lambda: T.matmul(ob[:, r * 65:(r + 1) * 65], lhsT=eb_[:, (r - rlo) * 128:(r - rlo + 1) * 128],
                                                      rhs=VD[:, j, h, :], start=(j == 0 and r == 0), stop=(j == 4 * m + r),
                                                      skip_group_check=True), reads=[eb_, VD], writes=[ob])
                        if j == 4 * m + 3:
                            ov = ob[:, 0:260].rearrange("p (r d) -> p r d", r=4)
                            yb = yst[(h * NG + m) % 2]
                            op('dve', lambda: V.reciprocal(out=rden[:], in_=ov[:, :, 64]), reads=[ob], writes=[rden])
                            op('dve', lambda: V.tensor_tensor(out=yb[:], in0=ov[:, :, 0:64],
                                                              in1=rden[:].unsqueeze(2).to_broadcast([128, 4, 64]), op=ALU.mult),
                               reads=[ob, rden], writes=[yb])
                            Sc.dma('pool', ybuf[m * 512:(m + 1) * 512, 768 + h * 64:768 + (h + 1) * 64].rearrange("(r p) d -> p r d", p=128),
                                   yb[:], reads=[yb])

                    LA = 2
                    for k in range(min(LA, len(items))):
                        qk(k)
                    for k in range(len(items)):
                        if k + LA < len(items):
                            qk(k + LA)
                        pv(k)
                Sc.barrier()

            if want('oproj'):
                with ExitStack() as ph:
                    WO = sb(ph, "WO", [128, 8, D], BF16)
                    gg = sb(ph, "gg", [128, 8], F32)
                    stg = [sb(ph, f"stgo{i}", [128, D], F32) for i in range(2)]
                    yt = [sb(ph, f"yt{i}", [128, D], F32) for i in range(2)]
                    xt = [sb(ph, f"xt{i}", [128, D], F32) for i in range(3)]
                    junk = sb(ph, "junko", [128, D], F32)
                    tmp = sb(ph, "tmpo", [128, D], F32)
                    ybs = [sb(ph, f"yb{i}", [128, D], BF16) for i in range(2)]
                    mT = sb(ph, "mT", [128, 8, 128], BF16)
                    hbs = [sb(ph, f"hbo{i}", [128, D], BF16) for i in range(2)]
                    h2s = [sb(ph, f"h2s{i}", [128, 8, 128], BF16) for i in range(2)]
                    sts = [sb(ph, f"sto{i}", [128, 16], F32) for i in range(2)]
                    pT = [ps(ph, f"pTo{i}", BF16) for i in range(2)]
                    PO = [ps(ph, f"PO{i}") for i in range(4)]
                    Sc.dma('sp', gg[:], dram_ap(Wl['g_group'], 0, [[1, 128], [128, 8]]), writes=[gg], allow_slow_non_contiguous=True)
                    for kc in range(8):
                        b = kc % 2
                        Sc.dma('sp', stg[b][:], Wl['w_out'][kc * 128:(kc + 1) * 128, :], writes=[stg[b]])
                        if kc % 2 == 0:
                            op('dve', lambda: V.tensor_scalar(out=WO[:, kc, :], in0=stg[b][:], scalar1=gg[:, kc:kc + 1], scalar2=None,
                                                              op0=ALU.mult), reads=[stg[b], gg], writes=[WO])
                        else:
                            op('pool', lambda: P.tensor_scalar(out=WO[:, kc, :], in0=stg[b][:], scalar1=gg[:, kc:kc + 1], scalar2=None,
                                                               op0=ALU.mult), reads=[stg[b], gg], writes=[WO])

                    def po_s0(i):
                        b = i % 2
                        y_t, x_t, st, yb = yt[b], xt[i % 3], sts[b], ybs[b]
                        Sc.dma('sp', y_t[:], ybuf[i * 128:(i + 1) * 128, :], writes=[y_t])
                        Sc.dma('sp', x_t[:], xin[i * 128:(i + 1) * 128, :], writes=[x_t])
                        for g in range(4):
                            op('act', lambda: A.activation(out=junk[:, 0:256], in_=y_t[:, g * 256:(g + 1) * 256], func=AF.Square,
                                                           scale=1.0 / 16.0, accum_out=st[:, g:g + 1]), reads=[y_t], writes=[junk, st],
                               multi=True)
                        op('act', lambda: A.activation(out=st[:, 4:8], in_=st[:, 0:4], func=AF.Sqrt, bias=epsT[:, 0:1], scale=1.0),
                           reads=[st, epsT], writes=[st])
                        op('dve', lambda: V.reciprocal(out=st[:, 8:12], in_=st[:, 4:8]), reads=[st], writes=[st])
                        for g in range(4):
                            op('dve', lambda: V.tensor_scalar(out=yb[:, g * 256:(g + 1) * 256], in0=y_t[:, g * 256:(g + 1) * 256],
                                                              scalar1=st[:, 8 + g:9 + g], scalar2=None, op0=ALU.mult),
                               reads=[y_t, st], writes=[yb])

                    def po_s1(i):
                        b = i % 2
                        x_t, st, yb, hb = xt[i % 3], sts[b], ybs[b], hbs[b]
                        for kc in range(8):
                            op('pe', lambda: T.transpose(out=pT[0][:, kc * 128:(kc + 1) * 128], in_=yb[:, kc * 128:(kc + 1) * 128],
                                                         identity=ident[:]), reads=[yb, ident], writes=[pT[0]])
                        op('act', lambda: A.copy(out=mT[:], in_=pT[0][:].rearrange("p (k t) -> p k t", k=8)), reads=[pT[0]], writes=[mT])
                        for nh in range(2):
                            po = PO[(i % 2) * 2 + nh]
                            for kc in range(8):
                                op('pe', lambda: T.matmul(po[:], lhsT=mT[:, kc, :], rhs=WO[:, kc, nh * 512:(nh + 1) * 512],
                                                          start=(kc == 0), stop=(kc == 7)), reads=[mT, WO], writes=[po])
                            hs = slice(nh * 512, (nh + 1) * 512)
                            op('dve', lambda: V.tensor_tensor(out=tmp[:, hs], in0=po[:], in1=MOD[:, 2, hs], op=ALU.mult),
                               reads=[po, MOD], writes=[tmp])
                            op('dve', lambda: V.tensor_tensor(out=x_t[:, hs], in0=x_t[:, hs], in1=tmp[:, hs], op=ALU.add),
                               reads=[x_t, tmp], writes=[x_t])
                        Sc.dma('pool', xout[i * 128:(i + 1) * 128, :], x_t[:], reads=[x_t])
                        op('act', lambda: A.activation(out=junk[:], in_=x_t[:], func=AF.Square, scale=1.0 / 32.0,
                                                       accum_out=st[:, 12:13]), reads=[x_t], writes=[junk, st], multi=True)
                        op('act', lambda: A.activation(out=st[:, 13:14], in_=st[:, 12:13], func=AF.Sqrt, bias=epsT[:, 0:1], scale=1.0),
                           reads=[st, epsT], writes=[st])
                        op('dve', lambda: V.reciprocal(out=st[:, 14:15], in_=st[:, 13:14]), reads=[st], writes=[st])
                        op('dve', lambda: V.scalar_tensor_tensor(out=tmp[:], in0=x_t[:], scalar=st[:, 14:15], in1=MOD[:, 4, :],
                                                                 op0=ALU.mult, op1=ALU.mult), reads=[x_t, st, MOD], writes=[tmp])
                        op('dve', lambda: V.tensor_tensor(out=hb[:], in0=tmp[:], in1=MOD[:, 3, :], op=ALU.add),
                           reads=[tmp, MOD], writes=[hb])

                    def po_s2(i):
                        b = i % 2
                        hb = hbs[b]
                        for kc in range(8):
                            op('pe', lambda: T.transpose(out=pT[1][:, kc * 128:(kc + 1) * 128], in_=hb[:, kc * 128:(kc + 1) * 128],
                                                         identity=ident[:]), reads=[hb, ident], writes=[pT[1]])
                        op('act', lambda: A.copy(out=h2s[b][:], in_=pT[1][:].rearrange("p (k t) -> p k t", k=8)),
                           reads=[pT[1]], writes=[h2s[b]])
                        Sc.dma('pool', h2T[:, i * 128:(i + 1) * 128].rearrange("(c p) t -> p c t", p=128), h2s[b][:], reads=[h2s[b]])

                    po_s0(0)
                    for i in range(NT):
                        if i + 1 < NT:
                            po_s0(i + 1)
                        po_s1(i)
                        if i >= 1:
                            po_s2(i - 1)
                    po_s2(NT - 1)
                Sc.barrier()

            if want('ffn'):
                for fp in range(2):
                    with ExitStack() as ph:
                        W1 = sb(ph, "W1", [128, 8, FH], BF16)
                        W3 = sb(ph, "W3", [128, 8, FH], BF16)
                        W2 = sb(ph, "W2", [128, NFC, D], BF16)
                        stg = [sb(ph, f"stgf{i}", [128, FH], F32) for i in range(2)]
                        H2 = [sb(ph, f"H2{i}", [128, 8, 512], BF16) for i in range(2)]
                        AT = sb(ph, "AT", [128, NFC, 512], BF16)
                        sl = [sb(ph, f"sl{i}", [128, 512], F32) for i in range(2)]
                        xt = [sb(ph, f"xtf{i}", [128, D], F32) for i in range(2)]
                        tmp = sb(ph, "tmpf", [128, D], F32)
                        ost = [sb(ph, f"ost{i}", [128, D], F32) for i in range(2)]
                        junk = sb(ph, "junkf", [128, D], F32)
                        gfb = sb(ph, "gfb", [128, D], F32)
                        st = sb(ph, "stf", [128, 4], F32)
                        G1p = [ps(ph, f"G1p{i}") for i in range(2)]
                        G3p = [ps(ph, f"G3p{i}") for i in range(2)]
                        PD = [ps(ph, f"PD{i}") for i in range(4)]
                        f0 = fp * FH
                        for kc in range(8):
                            b = kc % 2
                            Sc.dma('sp', stg[b][:], Wl['w1'][kc * 128:(kc + 1) * 128, f0:f0 + FH], writes=[stg[b]])
                            cast(W1[:, kc, :], stg[b][:], [stg[b]], [W1])
                        for kc in range(8):
                            b = kc % 2
                            Sc.dma('sp', stg[b][:], Wl['w3'][kc * 128:(kc + 1) * 128, f0:f0 + FH], writes=[stg[b]])
                            cast(W3[:, kc, :], stg[b][:], [stg[b]], [W3])
                        for fc in range(NFC):
                            b = fc % 2
                            Sc.dma('sp', stg[b][:, 0:D], Wl['w2'][f0 + fc * 128:f0 + (fc + 1) * 128, :], writes=[stg[b]])
                            cast(W2[:, fc, :], stg[b][:, 0:D], [stg[b]], [W2])
                        if last and fp == 1:
                            Sc.dma('sp', gfb[:], gfin_in[0].partition_broadcast(128), writes=[gfb])
                        for g in range(NG):
                            hh = H2[g % 2]
                            Sc.dma('sp', hh[:], h2T[:, g * 512:(g + 1) * 512].rearrange("(c p) t -> p c t", p=128), writes=[hh])
                            for fc in range(NFC):
                                g1, g3, s_ = G1p[fc % 2], G3p[fc % 2], sl[fc % 2]
                                for kc in range(8):
                                    op('pe', lambda: T.matmul(g1[:], lhsT=W1[:, kc, fc * 128:(fc + 1) * 128], rhs=hh[:, kc, :],
                                                              start=(kc == 0), stop=(kc == 7)), reads=[W1, hh], writes=[g1])
                                for kc in range(8):
                                    op('pe', lambda: T.matmul(g3[:], lhsT=W3[:, kc, fc * 128:(fc + 1) * 128], rhs=hh[:, kc, :],
                                                              start=(kc == 0), stop=(kc == 7)), reads=[W3, hh], writes=[g3])
                                op('act', lambda: A.activation(out=s_[:], in_=g1[:], func=AF.Silu), reads=[g1], writes=[s_])
                                op('dve', lambda: V.tensor_tensor(out=AT[:, fc, :], in0=s_[:], in1=g3[:], op=ALU.mult),
                                   reads=[s_, g3], writes=[AT])
                            for r in range(4):
                                i = g * 4 + r
                                x_t = xt[i % 2]
                                Sc.dma('sp', x_t[:], xout[i * 128:(i + 1) * 128, :], writes=[x_t])
                                for nh in range(2):
                                    pd = PD[(i % 2) * 2 + nh]
                                    hs = slice(nh * 512, (nh + 1) * 512)
                                    for fc in range(NFC):
                                        op('pe', lambda: T.matmul(pd[:], lhsT=AT[:, fc, r * 128:(r + 1) * 128], rhs=W2[:, fc, hs],
                                                                  start=(fc == 0), stop=(fc == NFC - 1)), reads=[AT, W2], writes=[pd])
                                    op('dve', lambda: V.tensor_tensor(out=tmp[:, hs], in0=pd[:], in1=MOD[:, 5, hs], op=ALU.mult),
                                       reads=[pd, MOD], writes=[tmp])
                                    op('dve', lambda: V.tensor_tensor(out=x_t[:, hs], in0=x_t[:, hs], in1=tmp[:, hs], op=ALU.add),
                                       reads=[x_t, tmp], writes=[x_t])
                                Sc.dma('pool', xout[i * 128:(i + 1) * 128, :], x_t[:], reads=[x_t])
                                if last and fp == 1:
                                    o_t = ost[i % 2]
                                    op('act', lambda: A.activation(out=junk[:], in_=x_t[:], func=AF.Square, scale=1.0 / 32.0,
                                                                   accum_out=st[:, 0:1]), reads=[x_t], writes=[junk, st], multi=True)
                                    op('act', lambda: A.activation(out=st[:, 1:2], in_=st[:, 0:1], func=AF.Sqrt, bias=epsT[:, 0:1],
                                                                   scale=1.0), reads=[st, epsT], writes=[st])
                                    op('dve', lambda: V.reciprocal(out=st[:, 2:3], in_=st[:, 1:2]), reads=[st], writes=[st])
                                    op('dve', lambda: V.scalar_tensor_tensor(out=o_t[:], in0=x_t[:], scalar=st[:, 2:3], in1=gfb[:],
                                                                             op0=ALU.mult, op1=ALU.mult), reads=[x_t, st, gfb], writes=[o_t])
                                    Sc.dma('pool', out[seq, i * 128:(i + 1) * 128, :], o_t[:], reads=[o_t])
                    Sc.barrier()
        Sc.barrier()
        print("program instructions:", Sc.ninstr, Sc.cnt)
    return nc


_PROG_CACHE = {}


def _get_prog(S, nseq, nlayer, steps):
    key = (S, nseq, nlayer, tuple(steps))
    if key not in _PROG_CACHE:
        _PROG_CACHE[key] = build(S, nseq, nlayer, list(steps))
    return _PROG_CACHE[key]


def kernel(**inputs):
    x = np.asarray(inputs['x'], dtype=np.float32)
    c = np.asarray(inputs['c'], dtype=np.float32)
    B, S, _ = x.shape
    L = np.asarray(inputs['w_mod']).shape[0]
    per = B // N_CORES
    consts = host_consts(S)
    t5 = np.ascontiguousarray(np.asarray(inputs['t5_table'], dtype=np.float32))
    gfin = np.ascontiguousarray(np.asarray(inputs['g_final'], dtype=np.float32)).reshape(1, -1)
    lw = [layout_weights(inputs, l) for l in range(L)]
    steps = tuple((sl, l, l == L - 1) for l in range(L) for sl in range(per))
    nc = _get_prog(S, per, L, steps)
    in_maps = []
    for core in range(N_CORES):
        m = dict(x=np.ascontiguousarray(x[core * per:(core + 1) * per]),
                 c=np.ascontiguousarray(c[core * per:(core + 1) * per]), t5=t5, g_final=gfin)
        for l in range(L):
            m.update({f"{k}_{l}": v for k, v in lw[l].items()})
        m.update(consts)
        in_maps.append(m)
    res = run_bass_kernel_spmd(nc, in_maps, core_ids=list(range(N_CORES)))
    full = np.empty((B, S, D), np.float32)
    for core in range(N_CORES):
        full[core * per:(core + 1) * per] = res.results[core]["out"]
    return full
```

```python
import math
from contextlib import ExitStack

import numpy as np
import ml_dtypes
import concourse.bass as bass
import concourse.mybir as mybir
from concourse.bass_types import AP
from concourse.bass_utils import run_bass_kernel_spmd

AF = mybir.ActivationFunctionType
ALU = mybir.AluOpType
AX = mybir.AxisListType
F32 = mybir.dt.float32
BF16 = mybir.dt.bfloat16

D = 1024
DFF = 2816
FH = DFF // 2
NFC = FH // 128
EPS = 1e-6
TMW = 968
FMW = 2048
NBIS = 12
TOPK = 256
NEG = -1.0e30
EPOCH = 16000
N_CORES = 8


class Buf:
    def __init__(self, h, name):
        self.h = h
        self.name = name
        self.w = None
        self.r = {}

    def __getitem__(self, idx):
        return self.h[idx]


class Sched:
    ENG = ('pe', 'act', 'dve', 'pool', 'sp')

    def __init__(self, nc, es):
        self.nc = nc
        self.es = es
        self.engs = {'pe': nc.tensor, 'act': nc.scalar, 'dve': nc.vector, 'pool': nc.gpsimd, 'sp': nc.sync}
        self.sems = {}
        self.cnt = {k: 0 for k in self.ENG}
        self.seen = {k: {} for k in self.ENG}
        self.dq = {}
        for q, n in (('sp', 10), ('pool', 6), ('act', 4)):
            self.dq[q] = dict(sems=[es.enter_context(nc.semaphore(f"dma_{q}{i}")) for i in range(n)],
                              cnt=[0] * n, i=0)
        self.ninstr = 0

    def _sem(self, key, n):
        ep = n // EPOCH
        k = (key, ep)
        if k not in self.sems:
            self.sems[k] = self.es.enter_context(self.nc.semaphore(f"s_{key}_{ep}"))
        return self.sems[k], n % EPOCH + 1

    def _semval(self, ev):
        key, n = ev
        if isinstance(key, tuple):
            return self.dq[key[1]]['sems'][key[2]], 16 * (n + 1)
        return self._sem(key, n)

    def _need(self, eng, evs):
        out = {}
        for key, n in evs:
            if self.seen[eng].get(key, -1) >= n:
                continue
            if out.get(key, -1) < n:
                out[key] = n
        return list(out.items())

    def _emit_waits(self, eng, evs, ins_fn=None):
        need = self._need(eng, evs)
        for key, n in need:
            self.seen[eng][key] = n
        if ins_fn is None:
            for ev in need:
                sem, v = self._semval(ev)
                self.engs[eng].wait_ge(sem, v)
                self.ninstr += 1
            return None
        for ev in need[:-1]:
            sem, v = self._semval(ev)
            self.engs[eng].wait_ge(sem, v)
            self.ninstr += 1
        ins = ins_fn()
        if need:
            sem, v = self._semval(need[-1])
            ins._wait_ge(sem, v)
        return ins

    def _deps(self, eng, reads, writes):
        evs = []
        for b in reads:
            if b.w is not None:
                evs.append(b.w)
        for b in writes:
            if b.w is not None and b.w[0] != eng:
                evs.append(b.w)
            for k, n in b.r.items():
                if k != eng:
                    evs.append((k, n))
        return evs

    def op(self, eng, fn, reads=(), writes=(), multi=False):
        evs = self._deps(eng, reads, writes)
        if multi or eng == 'pe':
            self._emit_waits(eng, evs)
            ins = fn()
        else:
            ins = self._emit_waits(eng, evs, fn)
        n = self.cnt[eng]
        self.cnt[eng] += 1
        sem, _ = self._sem(eng, n)
        ins.then_inc(sem, 1)
        self.ninstr += 1
        for b in reads:
            b.r[eng] = n
        for b in writes:
            b.w = (eng, n)
            b.r = {}
        return ins

    def dma(self, q, out_ap, in_ap, reads=(), writes=(), **kw):
        dq = self.dq[q]
        i = dq['i'] % len(dq['sems'])
        dq['i'] += 1
        key = ('dma', q, i)
        evs = self._deps(q, reads, writes)
        if dq['cnt'][i] > 0:
            evs.append((key, dq['cnt'][i] - 1))
        self._emit_waits(q, evs)
        ins = self.engs[q].dma_start(out=out_ap, in_=in_ap, **kw)
        ins.then_inc(dq['sems'][i], 16)
        n = dq['cnt'][i]
        dq['cnt'][i] += 1
        self.ninstr += 1
        for b in reads:
            b.r[key] = n
        for b in writes:
            b.w = (key, n)
            b.r = {}

    def barrier(self):
        evs = []
        for k in self.ENG:
            if k != 'sp' and self.cnt[k] > 0:
                evs.append((k, self.cnt[k] - 1))
        for q, dq in self.dq.items():
            for i, c in enumerate(dq['cnt']):
                if c > 0:
                    evs.append((('dma', q, i), c - 1))
        self._emit_waits('sp', evs)
        ins = self.nc.sync.nop()
        n = self.cnt['sp']
        self.cnt['sp'] += 1
        sem, _ = self._sem('sp', n)
        ins.then_inc(sem, 1)
        self.ninstr += 1
        for k in self.ENG:
            if k != 'sp':
                self._emit_waits(k, [('sp', n)])
                for k2 in self.ENG:
                    if self.cnt[k2] > 0:
                        self.seen[k][k2] = max(self.seen[k].get(k2, -1), self.cnt[k2] - 1 if k2 != 'sp' else n)
                for q, dq in self.dq.items():
                    for i, c in enumerate(dq['cnt']):
                        if c > 0:
                            self.seen[k][('dma', q, i)] = c - 1


def bc_mid(ap, n):
    a = [list(x) for x in ap.ap]
    return AP(tensor=ap.tensor, offset=ap.offset, ap=[a[0], [0, n]] + a[1:])


def dram_ap(t, offset, pairs):
    return AP(tensor=t.tensor, offset=offset, ap=[list(p) for p in pairs])


def _t5_bucket_np(rel):
    nb = 16
    max_exact = 8
    ret = np.where(rel > 0, nb, 0)
    n = np.abs(rel)
    nf = np.maximum(n, 1).astype(np.float32)
    large = max_exact + (np.log(nf / np.float32(max_exact)) / np.float32(math.log(128 / max_exact))
                         * np.float32(nb - max_exact)).astype(np.int32)
    large = np.minimum(large, nb - 1)
    return ret + np.where(n < max_exact, n, large)


def host_consts(S):
    bf = ml_dtypes.bfloat16
    pa = np.zeros((128, 12, 128), np.float32)
    s = np.arange(128)[:, None]
    t = np.arange(128)[None, :]
    for gi, w in enumerate((2, 4, 8, 16)):
        inwin = ((s <= t) & (s > t - w)).astype(np.float32)
        pa[:, gi, :] = inwin / w - (s == t)
        cnt = np.minimum(t + 1, w).astype(np.float32)
        pa[:, 8 + gi, :] = inwin / cnt - (s == t)
        inprev = ((s - 128) > t - w).astype(np.float32)
        pa[:, 4 + gi, :] = inprev / w
    bm = np.zeros((128, 5, 128), np.float32)
    for d in range(5):
        diff = 2 * d + (t // 64) - (s // 64)
        bm[:, d, :] = ((diff >= 0) & (diff <= 8)).astype(np.float32)
    m = np.arange(384)
    bk = _t5_bucket_np((127 - m).astype(np.int32))
    oh = np.zeros((32, 384), np.float32)
    oh[bk, m] = 1.0
    half = 16
    freqs = (10000.0 ** (-np.arange(half, dtype=np.float32) / half)).astype(np.float32)
    ang = np.arange(S, dtype=np.float32)[None, :] * freqs[:, None]
    cs = np.zeros((2, 32, S), np.float32)
    cs[0, :16] = np.cos(ang)
    cs[0, 16:] = np.cos(ang)
    cs[1, :16] = -np.sin(ang)
    cs[1, 16:] = np.sin(ang)
    p2 = np.tile((0.5 ** np.arange(1, NBIS + 1, dtype=np.float32))[None, :], (128, 1)).astype(np.float32)
    return dict(c_poolA=pa.astype(bf), c_bandm=bm.astype(bf), c_t5oh=oh, c_cs=cs.astype(bf), c_pow2=p2)


def _in_cols():
    o = {}
    off = 0
    for n, w in (('pool_u', 256), ('ca_q', 256), ('ca_k', 256), ('ca_v', 256), ('sa_q', 256), ('sa_k', 64),
                 ('sa_v', 64), ('idx_q', 512), ('idx_k', 64), ('idx_w', 8), ('mla_cq', 256), ('mla_ckv', 128),
                 ('mla_kr', 32)):
        o[n] = np.arange(off, off + w)
        off += w
    return o


def layout_weights(inp, l):
    o = _in_cols()
    w_in = np.asarray(inp['w_in'][l])
    tm = np.concatenate([o['pool_u'], o['ca_v'], o['sa_v'], o['idx_w'], o['mla_cq'], o['mla_ckv']])
    w_tm = np.ascontiguousarray(w_in[:, tm])
    w_fm = np.zeros((D, FMW), np.float32)
    fm = np.concatenate([o['ca_q'], o['ca_k'], o['sa_q'], o['idx_q'], o['sa_k'], o['idx_k'], o['mla_cq'], o['mla_ckv']])
    w_fm[:, :fm.size] = w_in[:, fm]
    kr = o['mla_kr']
    w_fm[:, 1792 + 64:1792 + 96] = w_in[:, kr]
    w_fm[:, 1920 + 64:1920 + 96] = w_in[:, np.concatenate([kr[16:], kr[:16]])]
    wuq = np.asarray(inp['mla_w_uq'][l])
    wqa = np.ascontiguousarray(wuq.reshape(256, 384))
    wqb = np.zeros((256, 4, 96), np.float32)
    wqb[:, :, 64:80] = wuq[:, :, 80:96]
    wqb[:, :, 80:96] = wuq[:, :, 64:80]
    wukv = np.asarray(inp['mla_w_ukv'][l])
    wk = np.ascontiguousarray(wukv[:, :, :64].reshape(128, 256))
    wv = np.ascontiguousarray(wukv[:, :, 64:].reshape(128, 256))
    f = lambda a: np.ascontiguousarray(np.asarray(a), dtype=np.float32)
    return dict(
        w_mod=f(inp['w_mod'][l]), b_mod=f(inp['b_mod'][l]).reshape(1, -1), g_mix=f(inp['g_mix'][l]).reshape(1, -1),
        w_tm=w_tm, w_fm=w_fm, pool_w=f(inp['pool_w'][l]), pool_scale=f(inp['pool_scale'][l]).reshape(1, -1),
        ca_rel=f(inp['ca_rel'][l]), g_cq=f(inp['mla_g_cq'][l]).reshape(1, -1), g_ckv=f(inp['mla_g_ckv'][l]).reshape(1, -1),
        wqa=wqa, wqb=wqb.reshape(256, 384), wk=wk, wv=wv, g_group=f(inp['g_group'][l]).reshape(1, -1),
        w_out=f(inp['w_out'][l]), g_ffn=f(inp['g_ffn'][l]).reshape(1, -1), w1=f(inp['ffn_w1'][l]),
        w3=f(inp['ffn_w3'][l]), w2=f(inp['ffn_w2'][l]))


WSHAPES = dict(w_mod=[D, 6 * D], b_mod=[1, 6 * D], g_mix=[1, D], w_tm=[D, TMW], w_fm=[D, FMW], pool_w=[4, 64, 64],
               pool_scale=[1, 256], ca_rel=[4, 320], g_cq=[1, 256], g_ckv=[1, 128], wqa=[256, 384], wqb=[256, 384],
               wk=[128, 256], wv=[128, 256], g_group=[1, D], w_out=[D, D], g_ffn=[1, D], w1=[D, DFF], w3=[D, DFF],
               w2=[DFF, D])


def build(S, nseq, nlayer, steps, debug=False, phases=None):
    NT = S // 128
    NG = S // 512
    nc = bass.Bass("TRN2", target_bir_lowering=False)
    es = ExitStack()
    with es:
        def dram(name, shape, dt, kind="Internal"):
            return nc.dram_tensor(name, shape, dt, kind=kind).ap()

        x_in = dram("x", [nseq, S, D], F32, "ExternalInput")
        c_in = dram("c", [nseq, D], F32, "ExternalInput")
        t5_in = dram("t5", [32, 4], F32, "ExternalInput")
        gfin_in = dram("g_final", [1, D], F32, "ExternalInput")
        W = []
        for l in range(nlayer):
            W.append({k: dram(f"{k}_{l}", shp, F32, "ExternalInput") for k, shp in WSHAPES.items()})
        c_poolA = dram("c_poolA", [128, 12, 128], BF16, "ExternalInput")
        c_bandm = dram("c_bandm", [128, 5, 128], BF16, "ExternalInput")
        c_t5oh = dram("c_t5oh", [32, 384], F32, "ExternalInput")
        c_cs = dram("c_cs", [2, 32, S], BF16, "ExternalInput")
        c_pow2 = dram("c_pow2", [128, NBIS], F32, "ExternalInput")
        xo = dram("xo", [nseq, S, D], F32, "ExternalOutput")
        out = dram("out", [nseq, S, D], F32, "ExternalOutput")
        dk = "ExternalOutput" if debug else "Internal"
        ztm = dram("ztm", [S, 576], BF16, dk)
        zsm = dram("zsm", [S, 8], F32, dk)
        zrq = dram("zrq", [1, S], F32, dk)
        zrk = dram("zrk", [1, S], F32, dk)
        zfm = dram("zfm", [FMW, S], BF16, dk)
        ybuf = dram("ybuf", [S, D], F32, dk)
        h2T = dram("h2T", [D, S], BF16, dk)
        ext = dram("ext", [4, 768], F32, "Internal")
        t5v = dram("t5v", [4, 384], F32, "Internal")

        Sc = Sched(nc, es)
        op = Sc.op
        V, A, P, T = nc.vector, nc.scalar, nc.gpsimd, nc.tensor

        uid = [0]

        def sb(st, name, shape, dt):
            uid[0] += 1
            name = f"{name}_{uid[0]}"
            return Buf(st.enter_context(nc.sbuf_tensor(name, shape, dt)), name)

        def ps(st, name, dt=F32):
            uid[0] += 1
            name = f"{name}_{uid[0]}"
            shape = [128, 512] if dt == F32 else [128, 1024]
            return Buf(st.enter_context(nc.psum_tensor(name, shape, dt)), name)

        def want(p):
            return phases is None or p in phases

        ident = sb(es, "ident", [128, 128], BF16)
        identf = sb(es, "identf", [128, 128], F32)
        onesf = sb(es, "onesf", [128, 128], F32)
        epsT = sb(es, "epsT", [128, 1], F32)
        MOD = sb(es, "MOD", [128, 6, D], F32)
        op('pool', lambda: P.memset(identf[:], 1.0), writes=[identf])
        op('pool', lambda: P.affine_select(out=identf[:], in_=identf[:], pattern=[[-1, 128]], compare_op=ALU.is_equal,
                                           fill=0.0, base=0, channel_multiplier=1), reads=[identf], writes=[identf])
        op('dve', lambda: V.tensor_copy(out=ident[:], in_=identf[:]), reads=[identf], writes=[ident])
        Jf = sb(es, "Jf", [128, 128], F32)
        op('pool', lambda: P.memset(Jf[:], 1.0), writes=[Jf])
        op('pool', lambda: P.affine_select(out=Jf[:], in_=Jf[:], pattern=[[1, 128]], compare_op=ALU.is_equal,
                                           fill=0.0, base=-127, channel_multiplier=1), reads=[Jf], writes=[Jf])
        op('dve', lambda: V.memset(onesf[:], 1.0), writes=[onesf])
        op('dve', lambda: V.memset(epsT[:], EPS), writes=[epsT])

        rr = [0]

        def cast(out_ap, in_ap, reads, writes):
            k = rr[0] % 3
            rr[0] += 1
            if k == 0:
                op('pool', lambda: P.tensor_copy(out=out_ap, in_=in_ap), reads=reads, writes=writes)
            elif k == 1:
                op('act', lambda: A.copy(out=out_ap, in_=in_ap), reads=reads, writes=writes)
            else:
                op('dve', lambda: V.tensor_copy(out=out_ap, in_=in_ap), reads=reads, writes=writes)

        def rstd_from(ms, sq, rs, n=1):
            op('act', lambda: A.activation(out=sq[:, 0:n], in_=ms[:, 0:n], func=AF.Sqrt, bias=epsT[:, 0:1], scale=1.0),
               reads=[ms, epsT], writes=[sq])
            op('dve', lambda: V.reciprocal(out=rs[:, 0:n], in_=sq[:, 0:n]), reads=[sq], writes=[rs])

        for (seq, l, last) in steps:
            Wl = W[l]
            xin = x_in[seq] if l == 0 else xo[seq]
            xout = xo[seq]

            if want('mod'):
                with ExitStack() as ph:
                    cT = sb(ph, "cT", [128, 8], F32)
                    cact = sb(ph, "cact", [128, 8], F32)
                    cbc = sb(ph, "cbc", [128, 8, 128], F32)
                    wst = [sb(ph, f"wst{i}", [128, 8, 512], F32) for i in range(2)]
                    bst = [sb(ph, f"bst{i}", [128, 512], F32) for i in range(2)]
                    gbc = [sb(ph, f"gbc{i}", [128, D], F32) for i in range(2)]
                    pm = [ps(ph, f"pm{i}") for i in range(2)]
                    Sc.dma('sp', cT[:], dram_ap(c_in, seq * D, [[1, 128], [128, 8]]), writes=[cT],
                           allow_slow_non_contiguous=True)
                    Sc.dma('sp', gbc[0][:], Wl['g_mix'][0].partition_broadcast(128), writes=[gbc[0]])
                    Sc.dma('sp', gbc[1][:], Wl['g_ffn'][0].partition_broadcast(128), writes=[gbc[1]])
                    op('act', lambda: A.activation(out=cact[:], in_=cT[:], func=AF.Silu), reads=[cT], writes=[cact])
                    for kc in range(8):
                        op('dve', lambda: V.tensor_scalar(out=cbc[:, kc, :], in0=onesf[:], scalar1=cact[:, kc:kc + 1],
                                                          scalar2=None, op0=ALU.mult), reads=[onesf, cact], writes=[cbc])
                    MODf = MOD[:].rearrange("p a d -> p (a d)")
                    for n in range(12):
                        b = n % 2
                        Sc.dma('sp', wst[b][:], Wl['w_mod'][:, n * 512:(n + 1) * 512].rearrange("(k p) n -> p k n", p=128),
                               writes=[wst[b]])
                        Sc.dma('sp', bst[b][:], Wl['b_mod'][0, n * 512:(n + 1) * 512].partition_broadcast(128),
                               writes=[bst[b]])
                        for kc in range(8):
                            op('pe', lambda: T.matmul(pm[b][:], lhsT=cbc[:, kc, :], rhs=wst[b][:, kc, :], start=(kc == 0),
                                                      stop=(kc == 7)), reads=[cbc, wst[b]], writes=[pm[b]])
                        op('dve', lambda: V.tensor_tensor(out=MODf[:, n * 512:(n + 1) * 512], in0=pm[b][:], in1=bst[b][:],
                                                          op=ALU.add), reads=[pm[b], bst[b]], writes=[MOD])
                    for (j, g) in ((1, 0), (4, 1)):
                        op('dve', lambda: V.scalar_tensor_tensor(out=MOD[:, j, :], in0=MOD[:, j, :], scalar=1.0,
                                                                 in1=gbc[g][:], op0=ALU.add, op1=ALU.mult),
                           reads=[MOD, gbc[g]], writes=[MOD])
                Sc.barrier()

            if want('inproj'):
                with ExitStack() as ph:
                    WTM = sb(ph, "WTM", [128, 8, TMW], BF16)
                    WFM = sb(ph, "WFM", [128, 8, FMW], BF16)
                    stg = [sb(ph, f"stg{i}", [128, FMW], F32) for i in range(2)]
                    xb = [sb(ph, f"xb{i}", [128, D], F32) for i in range(2)]
                    t1 = sb(ph, "t1", [128, D], F32)
                    junk = sb(ph, "junk", [128, D], F32)
                    hbs = [sb(ph, f"hb{i}", [128, D], BF16) for i in range(2)]
                    hT4 = [sb(ph, f"hT4{i}", [128, 8, 512], BF16) for i in range(2)]
                    sts = [sb(ph, f"st{i}", [128, 8], F32) for i in range(2)]
                    stm = [sb(ph, f"stm{i}", [128, 576], BF16) for i in range(2)]
                    ssm = [sb(ph, f"ssm{i}", [128, 16], F32) for i in range(2)]
                    sfm = [sb(ph, f"sfm{i}", [128, 16, 512], BF16) for i in range(2)]
                    pT = ps(ph, "pT", BF16)
                    ptm = [ps(ph, f"ptm{i}") for i in range(2)]
                    pfm = [ps(ph, f"pfm{i}") for i in range(3)]
                    for kc in range(8):
                        b = kc % 2
                        Sc.dma('sp', stg[b][:, 0:TMW], Wl['w_tm'][kc * 128:(kc + 1) * 128, :], writes=[stg[b]])
                        cast(WTM[:, kc, :], stg[b][:, 0:TMW], [stg[b]], [WTM])
                    for kc in range(8):
                        b = kc % 2
                        Sc.dma('sp', stg[b][:], Wl['w_fm'][kc * 128:(kc + 1) * 128, :], writes=[stg[b]])
                        cast(WFM[:, kc, :], stg[b][:], [stg[b]], [WFM])
                    def pa_s1(i):
                        x_t = xb[i % 2]
                        hb = hbs[i % 2]
                        st = sts[i % 2]
                        Sc.dma('sp', x_t[:], xin[i * 128:(i + 1) * 128, :], writes=[x_t])
                        op('act', lambda: A.activation(out=junk[:], in_=x_t[:], func=AF.Square, scale=1.0 / 32.0,
                                                       accum_out=st[:, 0:1]), reads=[x_t], writes=[junk, st], multi=True)
                        op('act', lambda: A.activation(out=st[:, 1:2], in_=st[:, 0:1], func=AF.Sqrt, bias=epsT[:, 0:1],
                                                       scale=1.0), reads=[st, epsT], writes=[st])
                        op('dve', lambda: V.reciprocal(out=st[:, 2:3], in_=st[:, 1:2]), reads=[st], writes=[st])
                        op('dve', lambda: V.scalar_tensor_tensor(out=t1[:], in0=x_t[:], scalar=st[:, 2:3], in1=MOD[:, 1, :],
                                                                 op0=ALU.mult, op1=ALU.mult), reads=[x_t, st, MOD], writes=[t1])
                        op('dve', lambda: V.tensor_tensor(out=hb[:], in0=t1[:], in1=MOD[:, 0, :], op=ALU.add),
                           reads=[t1, MOD], writes=[hb])

                    def pa_s2(i):
                        g, r = i // 4, i % 4
                        hT = hT4[g % 2]
                        hb = hbs[i % 2]
                        for kc in range(8):
                            op('pe', lambda: T.transpose(out=pT[:, kc * 128:(kc + 1) * 128], in_=hb[:, kc * 128:(kc + 1) * 128],
                                                         identity=ident[:]), reads=[hb, ident], writes=[pT])
                        op('act', lambda: A.copy(out=hT[:, :, r * 128:(r + 1) * 128],
                                                 in_=pT[:].rearrange("p (k t) -> p k t", k=8)), reads=[pT], writes=[hT])
                        for kc in range(8):
                            op('pe', lambda: T.matmul(ptm[0][:], lhsT=hT[:, kc, r * 128:(r + 1) * 128], rhs=WTM[:, kc, 0:512],
                                                      start=(kc == 0), stop=(kc == 7)), reads=[hT, WTM], writes=[ptm[0]])
                        for kc in range(8):
                            op('pe', lambda: T.matmul(ptm[1][:, 0:456], lhsT=hT[:, kc, r * 128:(r + 1) * 128],
                                                      rhs=WTM[:, kc, 512:968], start=(kc == 0), stop=(kc == 7)),
                               reads=[hT, WTM], writes=[ptm[1]])
                        sm_, ss_ = stm[i % 2], ssm[i % 2]
                        op('act', lambda: A.copy(out=sm_[:, 0:512], in_=ptm[0][:]), reads=[ptm[0]], writes=[sm_])
                        op('dve', lambda: V.tensor_copy(out=sm_[:, 512:576], in_=ptm[1][:, 0:64]), reads=[ptm[1]], writes=[sm_])
                        op('dve', lambda: V.tensor_copy(out=ss_[:, 0:8], in_=ptm[1][:, 64:72]), reads=[ptm[1]], writes=[ss_])
                        op('act', lambda: A.activation(out=junk[:, 0:256], in_=ptm[1][:, 72:328], func=AF.Square,
                                                       scale=1.0 / 16.0, accum_out=ss_[:, 8:9]),
                           reads=[ptm[1]], writes=[junk, ss_], multi=True)
                        op('act', lambda: A.activation(out=junk[:, 0:128], in_=ptm[1][:, 328:456], func=AF.Square,
                                                       scale=1.0 / math.sqrt(128.0), accum_out=ss_[:, 9:10]),
                           reads=[ptm[1]], writes=[junk, ss_], multi=True)
                        op('act', lambda: A.activation(out=ss_[:, 10:12], in_=ss_[:, 8:10], func=AF.Sqrt, bias=epsT[:, 0:1],
                                                       scale=1.0), reads=[ss_, epsT], writes=[ss_])
                        op('dve', lambda: V.reciprocal(out=ss_[:, 12:14], in_=ss_[:, 10:12]), reads=[ss_], writes=[ss_])
                        Sc.dma('pool', ztm[i * 128:(i + 1) * 128, :], sm_[:], reads=[sm_])
                        Sc.dma('pool', zsm[i * 128:(i + 1) * 128, :], ss_[:, 0:8], reads=[ss_])
                        Sc.dma('pool', dram_ap(zrq, i * 128, [[1, 128], [1, 1]]), ss_[:, 12:13], reads=[ss_])
                        Sc.dma('pool', dram_ap(zrk, i * 128, [[1, 128], [1, 1]]), ss_[:, 13:14], reads=[ss_])
                        if r == 3:
                            sf = sfm[g % 2]
                            for ch in range(16):
                                pf = pfm[ch % 3]
                                for kc in range(8):
                                    op('pe', lambda: T.matmul(pf[:], lhsT=WFM[:, kc, ch * 128:(ch + 1) * 128], rhs=hT[:, kc, :],
                                                              start=(kc == 0), stop=(kc == 7)), reads=[WFM, hT], writes=[pf])
                                if ch % 2 == 0:
                                    op('act', lambda: A.copy(out=sf[:, ch, :], in_=pf[:]), reads=[pf], writes=[sf])
                                else:
                                    op('dve', lambda: V.tensor_copy(out=sf[:, ch, :], in_=pf[:]), reads=[pf], writes=[sf])
                            Sc.dma('pool', zfm[:, g * 512:(g + 1) * 512].rearrange("(c p) t -> p c t", p=128), sf[:], reads=[sf])
                    pa_s1(0)
                    for i in range(NT):
                        if i + 1 < NT:
                            pa_s1(i + 1)
                        pa_s2(i)
                Sc.barrier()

            if want('pool'):
                with ExitStack() as ph:
                    U = sb(ph, "U", [128, NT, 256], BF16)
                    PA = sb(ph, "PA", [128, 12, 128], BF16)
                    pwf = sb(ph, "pwf", [64, 4, 64], F32)
                    scb = sb(ph, "scb", [64, 256], F32)
                    PW = sb(ph, "PW", [64, 4, 64], BF16)
                    dT = [sb(ph, f"dT{i}", [64, 4, 128], BF16) for i in range(2)]
                    yst = [sb(ph, f"yst{i}", [128, 256], F32) for i in range(2)]
                    pd = [ps(ph, f"pd{i}") for i in range(2)]
                    py = [ps(ph, f"py{i}") for i in range(2)]
                    Sc.dma('sp', U[:], ztm[:, 0:256].rearrange("(n p) c -> p n c", p=128), writes=[U])
                    Sc.dma('sp', PA[:], c_poolA, writes=[PA])
                    Sc.dma('sp', pwf[:], Wl['pool_w'].rearrange("g c d -> c g d"), writes=[pwf])
                    Sc.dma('sp', scb[:], Wl['pool_scale'][0].partition_broadcast(64), writes=[scb])
                    op('dve', lambda: V.tensor_tensor(out=PW[:], in0=pwf[:], in1=scb[:].rearrange("p (g d) -> p g d", g=4),
                                                      op=ALU.mult), reads=[pwf, scb], writes=[PW])
                    for i in range(NT):
                        b = i % 2
                        for g in range(4):
                            a0 = 8 + g if i == 0 else g
                            op('pe', lambda: T.matmul(pd[b][0:64, g * 128:(g + 1) * 128], lhsT=U[:, i, g * 64:(g + 1) * 64],
                                                      rhs=PA[:, a0, :], start=True, stop=(i == 0)), reads=[U, PA], writes=[pd[b]])
                            if i > 0:
                                op('pe', lambda: T.matmul(pd[b][0:64, g * 128:(g + 1) * 128], lhsT=U[:, i - 1, g * 64:(g + 1) * 64],
                                                          rhs=PA[:, 4 + g, :], start=False, stop=True), reads=[U, PA], writes=[pd[b]])
                        op('act', lambda: A.copy(out=dT[b][:], in_=pd[b][0:64, :].rearrange("p (g t) -> p g t", g=4)),
                           reads=[pd[b]], writes=[dT[b]])
                        for g in range(4):
                            op('pe', lambda: T.matmul(py[b][:, g * 64:(g + 1) * 64], lhsT=dT[b][:, g, :], rhs=PW[:, g, :],
                                                      start=True, stop=True), reads=[dT[b], PW], writes=[py[b]])
                        op('dve', lambda: V.tensor_copy(out=yst[b][:], in_=py[b][:, 0:256]), reads=[py[b]], writes=[yst[b]])
                        Sc.dma('pool', ybuf[i * 128:(i + 1) * 128, 0:256], yst[b][:], reads=[yst[b]])
                Sc.barrier()

            if want('band'):
                with ExitStack() as ph:
                    KT = sb(ph, "KT", [64, 4, S], BF16)
                    QT = sb(ph, "QT", [64, 4, S], BF16)
                    VA = sb(ph, "VA", [128, NT, 4, 65], BF16)
                    raw = sb(ph, "raw", [128, 20, 128], F32)
                    raw2 = sb(ph, "raw2", [128, 20, 128], F32)
                    EB = sb(ph, "EB", [128, 20, 128], BF16)
                    bmk = sb(ph, "bmk", [128, 5, 128], BF16)
                    E = [sb(ph, f"E{i}", [128, 20, 128], BF16) for i in range(2)]
                    Pm = [sb(ph, f"Pm{i}", [128, 20, 128], BF16) for i in range(2)]
                    rden = sb(ph, "rden", [128, 4], F32)
                    yst = [sb(ph, f"ystb{i}", [128, 4, 64], F32) for i in range(2)]
                    SCp = [ps(ph, f"SCp{i}") for i in range(5)]
                    Op = [ps(ph, f"Op{i}") for i in range(2)]
                    Sc.dma('sp', QT[:], zfm[0:256, :].rearrange("(h d) t -> d h t", h=4), writes=[QT])
                    Sc.dma('sp', KT[:], zfm[256:512, :].rearrange("(h d) t -> d h t", h=4), writes=[KT])
                    for h in range(4):
                        Sc.dma('sp', VA[:, :, h, 0:64], ztm[:, 256 + h * 64:256 + (h + 1) * 64].rearrange("(n p) d -> p n d", p=128),
                               writes=[VA])
                    op('pool', lambda: P.memset(VA[:, :, :, 64:65], 1.0), writes=[VA])
                    Sc.dma('sp', bmk[:], c_bandm, writes=[bmk])
                    crs = sb(ph, "crs", [4, 320], F32)
                    exs = sb(ph, "exs", [4, 768], F32)
                    Sc.dma('sp', crs[:], Wl['ca_rel'], writes=[crs])
                    op('dve', lambda: V.tensor_copy(out=exs[:, 64:384], in_=crs[:]), reads=[crs], writes=[exs])
                    op('dve', lambda: V.tensor_copy(out=exs[:, 0:64], in_=crs[:, 0:1].to_broadcast([4, 64])), reads=[crs], writes=[exs])
                    op('dve', lambda: V.tensor_copy(out=exs[:, 384:768], in_=crs[:, 319:320].to_broadcast([4, 384])),
                       reads=[crs], writes=[exs])
                    Sc.dma('sp', ext, exs[:], reads=[exs])
                    Sc.barrier()
                    for h in range(4):
                        Sc.dma('sp', raw[:, h * 5:(h + 1) * 5, :], dram_ap(ext, h * 768, [[1, 128], [1, 640]]), writes=[raw])
                    for h in range(4):
                        op('pe', lambda: T.matmul(SCp[0][:], lhsT=Jf[:], rhs=raw[:, h * 5:h * 5 + 4, :].rearrange("p a t -> p (a t)"),
                                                  start=True, stop=True), reads=[Jf, raw], writes=[SCp[0]])
                        op('pe', lambda: T.matmul(SCp[1][:, 0:128], lhsT=Jf[:], rhs=raw[:, h * 5 + 4, :], start=True, stop=True),
                           reads=[Jf, raw], writes=[SCp[1]])
                        op('act', lambda: A.activation(out=raw2[:, h * 5:h * 5 + 4, :], in_=SCp[0][:].rearrange("p (a t) -> p a t", a=4),
                                                       func=AF.Exp), reads=[SCp[0]], writes=[raw2])
                        op('act', lambda: A.activation(out=raw2[:, h * 5 + 4, :], in_=SCp[1][:, 0:128], func=AF.Exp),
                           reads=[SCp[1]], writes=[raw2])
                        op('dve', lambda: V.tensor_tensor(out=EB[:, h * 5:(h + 1) * 5, :], in0=raw2[:, h * 5:(h + 1) * 5, :],
                                                          in1=bmk[:], op=ALU.mult), reads=[raw2, bmk], writes=[EB])
                    for i in range(NT):
                        b = i % 2
                        nd = min(i, 4) + 1
                        for h in range(4):
                            for d in range(nd):
                                j = i - d
                                blk = h * 5 + d
                                pb = SCp[blk // 4]
                                op('pe', lambda: T.matmul(pb[:, (blk % 4) * 128:(blk % 4 + 1) * 128], lhsT=KT[:, h, j * 128:(j + 1) * 128],
                                                          rhs=QT[:, h, i * 128:(i + 1) * 128], start=True, stop=True),
                                   reads=[KT, QT], writes=[pb])
                        for k in range(5):
                            op('act', lambda: A.activation(out=E[b][:, 4 * k:4 * k + 4, :],
                                                           in_=SCp[k][:].rearrange("p (a t) -> p a t", a=4), func=AF.Exp,
                                                           scale=0.125), reads=[SCp[k]], writes=[E[b]])
                        for hh in range(2):
                            op('dve', lambda: V.tensor_tensor(out=Pm[b][:, hh * 10:(hh + 1) * 10, :], in0=E[b][:, hh * 10:(hh + 1) * 10, :],
                                                              in1=EB[:, hh * 10:(hh + 1) * 10, :], op=ALU.mult),
                               reads=[E[b], EB], writes=[Pm[b]])
                        for h in range(4):
                            for d in range(nd):
                                j = i - d
                                op('pe', lambda: T.matmul(Op[b][:, h * 65:(h + 1) * 65], lhsT=Pm[b][:, h * 5 + d, :],
                                                          rhs=VA[:, j, h, :], start=(d == 0), stop=(d == nd - 1)),
                                   reads=[Pm[b], VA], writes=[Op[b]])
                        ov = Op[b][:, 0:260].rearrange("p (h d) -> p h d", h=4)
                        op('dve', lambda: V.reciprocal(out=rden[:], in_=ov[:, :, 64]), reads=[Op[b]], writes=[rden])
                        op('dve', lambda: V.tensor_tensor(out=yst[b][:], in0=ov[:, :, 0:64],
                                                          in1=rden[:].unsqueeze(2).to_broadcast([128, 4, 64]), op=ALU.mult),
                           reads=[Op[b], rden], writes=[yst[b]])
                        Sc.dma('pool', ybuf[i * 128:(i + 1) * 128, 256:512], yst[b][:].rearrange("p h d -> p (h d)"), reads=[yst[b]])
                Sc.barrier()

            if want('dsa'):
                with ExitStack() as ph:
                    BIG = 30000.0
                    IKT = sb(ph, "IKT", [64, S], BF16)
                    SKT = sb(ph, "SKT", [64, S], BF16)
                    VC = sb(ph, "VC", [128, NT, 65], BF16)
                    IQT = [sb(ph, f"IQT{i}", [64, 8, 128], BF16) for i in range(2)]
                    SQT = [sb(ph, f"SQT{i}", [64, 4, 128], BF16) for i in range(3)]
                    IW = [sb(ph, f"IW{i}", [128, 8], F32) for i in range(2)]
                    Dg = [sb(ph, f"Dg{i}", [128, 8, 128], BF16) for i in range(2)]
                    R = [sb(ph, f"R{i}", [128, 512], BF16) for i in range(5)]
                    SCs = [sb(ph, f"SCs{i}", [128, S], F32) for i in range(2)]
                    cjunk = sb(ph, "cjunk", [128, S], BF16)
                    MK = [sb(ph, f"MK{i}", [128, S], BF16) for i in range(2)]
                    PEN = sb(ph, "PEN", [128, NT, 128], BF16)
                    T5B = sb(ph, "T5B", [128, 8, 128], BF16)
                    rawt = sb(ph, "rawt", [128, 8, 128], F32)
                    t5s = sb(ph, "t5s", [32, 4], F32)
                    ohs = sb(ph, "ohs", [32, 384], F32)
                    t5r = sb(ph, "t5r", [4, 384], F32)
                    t5bc = sb(ph, "t5bc", [128, 128], F32)
                    nt5 = sb(ph, "nt5", [128, 4], F32)
                    negbig = sb(ph, "negbig", [128, 1], F32)
                    pw2 = sb(ph, "pw2", [128, NBIS], F32)
                    wts = sb(ph, "wts", [128, NBIS], F32)
                    bs = sb(ph, "bs", [128, 8], F32)
                    Eb = [sb(ph, f"Eb{i}", [128, 512], BF16) for i in range(3)]
                    rden = sb(ph, "rdenc", [128, 4], F32)
                    yst = [sb(ph, f"ystc{i}", [128, 4, 64], F32) for i in range(2)]
                    Lp = [ps(ph, f"Lp{i}") for i in range(2)]
                    SPp = ps(ph, "SPp")
                    MTp = ps(ph, "MTp", BF16)
                    STp = [ps(ph, f"STp{i}") for i in range(2)]
                    Op = [ps(ph, f"Opc{i}") for i in range(2)]
                    Lq = [Lp[0], Lp[1], STp[0], STp[1]]
                    Sc.dma('sp', SKT[:], zfm[1280:1344, :], writes=[SKT])
                    Sc.dma('sp', IKT[:], zfm[1344:1408, :], writes=[IKT])
                    Sc.dma('sp', VC[:, :, 0:64], ztm[:, 512:576].rearrange("(n p) d -> p n d", p=128), writes=[VC])
                    op('pool', lambda: P.memset(VC[:, :, 64:65], 1.0), writes=[VC])
                    op('pool', lambda: P.memset(negbig[:], -BIG), writes=[negbig])
                    Sc.dma('sp', pw2[:], c_pow2, writes=[pw2])
                    Sc.dma('sp', t5s[:], t5_in, writes=[t5s])
                    Sc.dma('sp', ohs[:], c_t5oh, writes=[ohs])
                    Sc.dma('sp', t5bc[:], t5_in.rearrange("a b -> (a b)").partition_broadcast(128), writes=[t5bc])
                    op('pe', lambda: T.matmul(SPp[0:4, 0:384], lhsT=t5s[:], rhs=ohs[:], start=True, stop=True),
                       reads=[t5s, ohs], writes=[SPp])
                    op('dve', lambda: V.tensor_copy(out=t5r[:], in_=SPp[0:4, 0:384]), reads=[SPp], writes=[t5r])
                    Sc.dma('sp', t5v, t5r[:], reads=[t5r])
                    op('dve', lambda: V.tensor_scalar(out=nt5[:], in0=t5bc[:, 60:64], scalar1=-8.0, scalar2=None, op0=ALU.mult),
                       reads=[t5bc], writes=[nt5])
                    Sc.barrier()
                    for dd in range(2):
                        for h in range(4):
                            Sc.dma('sp', rawt[:, dd * 4 + h, :], dram_ap(t5v, h * 384 + 128 * dd, [[1, 128], [1, 128]]),
                                   writes=[rawt])
                    for dd in range(2):
                        op('pe', lambda: T.matmul(Lp[dd][:], lhsT=Jf[:], rhs=rawt[:, dd * 4:dd * 4 + 4, :].rearrange("p a t -> p (a t)"),
                                                  start=True, stop=True), reads=[Jf, rawt], writes=[Lp[dd]])
                        for h in range(4):
                            op('act', lambda: A.activation(out=T5B[:, dd * 4 + h, :], in_=Lp[dd][:, h * 128:(h + 1) * 128],
                                                           func=AF.Identity, bias=nt5[:, h:h + 1], scale=8.0),
                               reads=[Lp[dd], nt5], writes=[T5B])
                    rcnt = [0]

                    def stageA(i):
                        b = i % 2
                        N = 128 * (i + 1)
                        sq = SQT[i % 3]
                        Sc.dma('sp', IQT[b][:], zfm[768:1280, i * 128:(i + 1) * 128].rearrange("(h d) t -> d h t", h=8), writes=[IQT[b]])
                        Sc.dma('sp', sq[:], zfm[512:768, i * 128:(i + 1) * 128].rearrange("(h d) t -> d h t", h=4), writes=[sq])
                        Sc.dma('sp', IW[b][:], zsm[i * 128:(i + 1) * 128, :], writes=[IW[b]])
                        for h in range(8):
                            op('pool', lambda: P.tensor_scalar(out=Dg[b][:, h, :], in0=ident[:], scalar1=IW[b][:, h:h + 1],
                                                               scalar2=None, op0=ALU.mult), reads=[ident, IW[b]], writes=[Dg[b]])
                        nblk = (N + 511) // 512
                        for kb in range(nblk):
                            wb = min(512, N - kb * 512)
                            base = rcnt[0]

                            def acc(h):
                                rb = R[(base + h) % 5]
                                op('pe', lambda: T.matmul(SPp[:, 0:wb], lhsT=Dg[b][:, h, :], rhs=rb[:, 0:wb], start=(h == 0),
                                                          stop=(h == 7)), reads=[Dg[b], rb], writes=[SPp])
                            for h in range(8):
                                lp = Lq[(base + h) % 4]
                                rb = R[(base + h) % 5]
                                op('pe', lambda: T.matmul(lp[:, 0:wb], lhsT=IQT[b][:, h, :], rhs=IKT[:, kb * 512:kb * 512 + wb],
                                                          start=True, stop=True), reads=[IQT[b], IKT], writes=[lp])
                                op('act', lambda: A.activation(out=rb[:, 0:wb], in_=lp[:, 0:wb], func=AF.Relu),
                                   reads=[lp], writes=[rb])
                                if h >= 2:
                                    acc(h - 2)
                            acc(6)
                            acc(7)
                            rcnt[0] += 8
                            op('act', lambda: A.copy(out=SCs[b][:, kb * 512:kb * 512 + wb], in_=SPp[:, 0:wb]), reads=[SPp], writes=[SCs[b]])
                        op('pool', lambda: P.memset(SCs[b][0:64, N - 64:N], NEG), writes=[SCs[b]])

                    def stageB(i):
                        b = i % 2
                        N = 128 * (i + 1)
                        sc = SCs[b]
                        if i >= 2:
                            op('dve', lambda: V.tensor_reduce(out=bs[:, 0:1], in_=sc[:, 0:N], axis=AX.X, op=ALU.max),
                               reads=[sc], writes=[bs])
                            op('dve', lambda: V.tensor_reduce(out=bs[:, 3:4], in_=sc[:, 0:N - 64], axis=AX.X, op=ALU.min),
                               reads=[sc], writes=[bs])
                            op('dve', lambda: V.tensor_tensor(out=bs[:, 2:3], in0=bs[:, 0:1], in1=bs[:, 3:4], op=ALU.subtract),
                               reads=[bs], writes=[bs])
                            op('dve', lambda: V.tensor_scalar(out=wts[:], in0=pw2[:], scalar1=bs[:, 2:3], scalar2=None,
                                                              op0=ALU.mult), reads=[pw2, bs], writes=[wts])
                            for it in range(NBIS):
                                op('dve', lambda: V.tensor_tensor(out=bs[:, 4:5], in0=bs[:, 3:4], in1=wts[:, it:it + 1], op=ALU.add),
                                   reads=[bs, wts], writes=[bs])
                                op('dve', lambda: V.tensor_scalar(out=cjunk[:, 0:N], in0=sc[:, 0:N], scalar1=bs[:, 4:5], scalar2=0.0,
                                                                  op0=ALU.is_ge, op1=ALU.add, accum_out=bs[:, 5:6]),
                                   reads=[sc, bs], writes=[cjunk, bs], multi=True)
                                op('dve', lambda: V.tensor_scalar(out=bs[:, 6:7], in0=bs[:, 5:6], scalar1=TOPK - 0.5,
                                                                  scalar2=wts[:, it:it + 1], op0=ALU.is_gt, op1=ALU.mult),
                                   reads=[bs, wts], writes=[bs])
                                op('dve', lambda: V.tensor_tensor(out=bs[:, 3:4], in0=bs[:, 3:4], in1=bs[:, 6:7], op=ALU.add),
                                   reads=[bs], writes=[bs])
                        else:
                            op('dve', lambda: V.memset(bs[:, 3:4], -1.0e29), writes=[bs])
                        op('dve', lambda: V.tensor_scalar(out=MK[b][:, 0:N], in0=sc[:, 0:N], scalar1=bs[:, 3:4], scalar2=None,
                                                          op0=ALU.is_ge), reads=[sc, bs], writes=[MK[b]])

                    ccnt = [0]

                    def stageC(i):
                        b = i % 2
                        mk = MK[b]
                        sq = SQT[i % 3]
                        for j0 in range(0, i + 1, 8):
                            nb = min(8, i + 1 - j0)
                            for jj in range(nb):
                                j = j0 + jj
                                op('pe', lambda: T.transpose(out=MTp[:, jj * 128:(jj + 1) * 128], in_=mk[:, j * 128:(j + 1) * 128],
                                                             identity=ident[:]), reads=[mk, ident], writes=[MTp])
                            op('act', lambda: A.activation(out=PEN[:, j0:j0 + nb, :],
                                                           in_=MTp[:, 0:nb * 128].rearrange("p (a t) -> p a t", a=nb),
                                                           func=AF.Identity, bias=negbig[:, 0:1], scale=BIG),
                               reads=[MTp, negbig], writes=[PEN])
                        ob = Op[b]
                        k0 = ccnt[0]
                        ccnt[0] += i + 1

                        def qk(j):
                            k = k0 + j
                            sp_, eb_ = STp[k % 2], Eb[k % 3]
                            op('pe', lambda: T.matmul(sp_[:], lhsT=SKT[:, j * 128:(j + 1) * 128],
                                                      rhs=sq[:].rearrange("p h t -> p (h t)"), start=True, stop=False),
                               reads=[SKT, sq], writes=[sp_])
                            near = i - j
                            op('pe', lambda: T.matmul(sp_[:], lhsT=ident[:], rhs=bc_mid(PEN[:, j, :], 4), start=False,
                                                      stop=(near > 1)), reads=[ident, PEN], writes=[sp_])
                            if near <= 1:
                                op('pe', lambda: T.matmul(sp_[:], lhsT=ident[:],
                                                          rhs=T5B[:, near * 4:near * 4 + 4, :].rearrange("p a t -> p (a t)"),
                                                          start=False, stop=True), reads=[ident, T5B], writes=[sp_])
                            op('act', lambda: A.activation(out=eb_[:], in_=sp_[:], func=AF.Exp, scale=0.125), reads=[sp_], writes=[eb_])

                        def pv(j):
                            eb_ = Eb[(k0 + j) % 3]
                            for h in range(4):
                                op('pe', lambda: T.matmul(ob[:, h * 65:(h + 1) * 65], lhsT=eb_[:, h * 128:(h + 1) * 128], rhs=VC[:, j, :],
                                                          start=(j == 0 and h == 0), stop=(j == i), skip_group_check=True),
                                   reads=[eb_, VC], writes=[ob])

                        qk(0)
                        for j in range(i + 1):
                            if j + 1 <= i:
                                qk(j + 1)
                            pv(j)
                        ov = ob[:, 0:260].rearrange("p (h d) -> p h d", h=4)
                        op('dve', lambda: V.reciprocal(out=rden[:], in_=ov[:, :, 64]), reads=[ob], writes=[rden])
                        op('dve', lambda: V.tensor_tensor(out=yst[b][:], in0=ov[:, :, 0:64],
                                                          in1=rden[:].unsqueeze(2).to_broadcast([128, 4, 64]), op=ALU.mult),
                           reads=[ob, rden], writes=[yst[b]])
                        Sc.dma('pool', ybuf[i * 128:(i + 1) * 128, 512:768], yst[b][:].rearrange("p h d -> p (h d)"), reads=[yst[b]])

                    stageA(0)
                    if NT > 1:
                        stageA(1)
                    stageB(0)
                    for i in range(NT):
                        if i + 2 < NT:
                            stageA(i + 2)
                        if i + 1 < NT:
                            stageB(i + 1)
                        stageC(i)
                Sc.barrier()

            if want('mla'):
                with ExitStack() as ph:
                    CQTb = [sb(ph, f"CQT{i}", [128, 2, 512], BF16) for i in range(2)]
                    CKTb = [sb(ph, f"CKT{i}", [128, 512], BF16) for i in range(2)]
                    KRRb = [sb(ph, f"KRR{i}", [96, 2, 512], BF16) for i in range(2)]
                    CSb = [sb(ph, f"CS{i}", [96, 2, 512], BF16) for i in range(2)]
                    QT = sb(ph, "QTd", [96, 4, S], BF16)
                    KT = sb(ph, "KTd", [96, 4, S], BF16)
                    VD = sb(ph, "VD", [128, NT, 4, 65], BF16)
                    wst = sb(ph, "wstd", [128, 2, 384], F32)
                    gq = sb(ph, "gq", [128, 2], F32)
                    gk = sb(ph, "gk", [128, 1], F32)
                    WQA = sb(ph, "WQA", [128, 2, 384], BF16)
                    WQB = sb(ph, "WQB", [128, 2, 384], BF16)
                    WK = sb(ph, "WK", [128, 256], BF16)
                    WV = sb(ph, "WV", [128, 256], BF16)
                    RKc = sb(ph, "RKc", [128, NT], F32)
                    RQb = [sb(ph, f"RQb{i}", [96, 512], F32) for i in range(2)]
                    RKb = [sb(ph, f"RKb{i}", [64, 512], F32) for i in range(2)]
                    cr = sb(ph, "cr", [96, 512], F32)
                    sr = sb(ph, "sr", [96, 512], F32)
                    ta = sb(ph, "ta", [96, 512], F32)
                    tb = sb(ph, "tb", [96, 512], F32)
                    tk1 = sb(ph, "tk1", [96, 512], F32)
                    tk2 = sb(ph, "tk2", [96, 512], F32)
                    Eb = [sb(ph, f"Ed{i}", [128, 512], BF16) for i in range(3)]
                    rden = sb(ph, "rdend", [128, 4], F32)
                    yst = [sb(ph, f"ystd{i}", [128, 4, 64], F32) for i in range(2)]
                    PAp = [ps(ph, f"PAp{i}") for i in range(2)]
                    PBp = [ps(ph, f"PBp{i}") for i in range(2)]
                    STp = [ps(ph, f"STd{i}") for i in range(2)]
                    Op = [ps(ph, f"Opd{i}") for i in range(2)]
                    Sc.dma('sp', RKc[:], dram_ap(zrk, 0, [[1, 128], [128, NT]]), writes=[RKc], allow_slow_non_contiguous=True)
                    Sc.dma('sp', gq[:], dram_ap(Wl['g_cq'], 0, [[1, 128], [128, 2]]), writes=[gq], allow_slow_non_contiguous=True)
                    Sc.dma('sp', gk[:], dram_ap(Wl['g_ckv'], 0, [[1, 128], [1, 1]]), writes=[gk])
                    for (src, dst) in ((Wl['wqa'], WQA), (Wl['wqb'], WQB)):
                        Sc.dma('sp', wst[:], src.rearrange("(c p) n -> p c n", p=128), writes=[wst])
                        for c2 in range(2):
                            op('dve', lambda: V.tensor_scalar(out=dst[:, c2, :], in0=wst[:, c2, :], scalar1=gq[:, c2:c2 + 1],
                                                              scalar2=None, op0=ALU.mult), reads=[wst, gq], writes=[dst])
                    for (src, dst) in ((Wl['wk'], WK), (Wl['wv'], WV)):
                        Sc.dma('sp', wst[:, 0, 0:256], src, writes=[wst])
                        op('dve', lambda: V.tensor_scalar(out=dst[:], in0=wst[:, 0, 0:256], scalar1=gk[:, 0:1], scalar2=None,
                                                          op0=ALU.mult), reads=[wst, gk], writes=[dst])
                    op('pool', lambda: P.memset(VD[:, :, :, 64:65], 1.0), writes=[VD])
                    for m in range(NG):
                        b = m % 2
                        gblk = slice(m * 512, (m + 1) * 512)
                        blk = slice(0, 512)
                        CQT, CKT, KRR, CS = CQTb[b], CKTb[b], KRRb[b], CSb[b]
                        Sc.dma('sp', CQT[:], zfm[1408:1664, gblk].rearrange("(c p) t -> p c t", p=128), writes=[CQT])
                        Sc.dma('sp', CKT[:], zfm[1664:1792, gblk], writes=[CKT])
                        Sc.dma('sp', KRR[64:96, 0, :], zfm[1792 + 64:1792 + 96, gblk], writes=[KRR])
                        Sc.dma('sp', KRR[64:96, 1, :], zfm[1920 + 64:1920 + 96, gblk], writes=[KRR])
                        Sc.dma('sp', CS[64:96, 0, :], c_cs[0, :, gblk], writes=[CS])
                        Sc.dma('sp', CS[64:96, 1, :], c_cs[1, :, gblk], writes=[CS])
                        Sc.dma('sp', RQb[b][:], zrq[0, gblk].partition_broadcast(96), writes=[RQb[b]])
                        Sc.dma('sp', RKb[b][:], zrk[0, gblk].partition_broadcast(64), writes=[RKb[b]])
                        op('dve', lambda: V.tensor_tensor(out=cr[64:96, :], in0=CS[64:96, 0, blk], in1=RQb[b][64:96, :], op=ALU.mult),
                           reads=[CS, RQb[b]], writes=[cr])
                        op('dve', lambda: V.tensor_tensor(out=sr[64:96, :], in0=CS[64:96, 1, blk], in1=RQb[b][64:96, :], op=ALU.mult),
                           reads=[CS, RQb[b]], writes=[sr])
                        for h in range(4):
                            pa, pb2 = PAp[h % 2], PBp[h % 2]
                            for c2 in range(2):
                                op('pe', lambda: T.matmul(pa[0:96, :], lhsT=WQA[:, c2, h * 96:(h + 1) * 96], rhs=CQT[:, c2, blk],
                                                          start=(c2 == 0), stop=(c2 == 1)), reads=[WQA, CQT], writes=[pa])
                            for c2 in range(2):
                                op('pe', lambda: T.matmul(pb2[0:96, :], lhsT=WQB[:, c2, h * 96:(h + 1) * 96], rhs=CQT[:, c2, blk],
                                                          start=(c2 == 0), stop=(c2 == 1)), reads=[WQB, CQT], writes=[pb2])
                            op('dve', lambda: V.tensor_tensor(out=QT[0:64, h, gblk], in0=pa[0:64, :], in1=RQb[b][0:64, :], op=ALU.mult),
                               reads=[pa, RQb[b]], writes=[QT])
                            op('dve', lambda: V.tensor_tensor(out=ta[64:96, :], in0=pa[64:96, :], in1=cr[64:96, :], op=ALU.mult),
                               reads=[pa, cr], writes=[ta])
                            op('dve', lambda: V.tensor_tensor(out=tb[64:96, :], in0=pb2[64:96, :], in1=sr[64:96, :], op=ALU.mult),
                               reads=[pb2, sr], writes=[tb])
                            op('dve', lambda: V.tensor_tensor(out=QT[64:96, h, gblk], in0=ta[64:96, :], in1=tb[64:96, :], op=ALU.add),
                               reads=[ta, tb], writes=[QT])
                        for h in range(4):
                            pa = PAp[h % 2]
                            op('pe', lambda: T.matmul(pa[0:64, :], lhsT=WK[:, h * 64:(h + 1) * 64], rhs=CKT[:, blk], start=True,
                                                      stop=True), reads=[WK, CKT], writes=[pa])
                            op('dve', lambda: V.tensor_tensor(out=KT[0:64, h, gblk], in0=pa[0:64, :], in1=RKb[b][0:64, :], op=ALU.mult),
                               reads=[pa, RKb[b]], writes=[KT])
                        op('dve', lambda: V.tensor_tensor(out=tk1[64:96, :], in0=KRR[64:96, 0, blk], in1=CS[64:96, 0, blk], op=ALU.mult),
                           reads=[KRR, CS], writes=[tk1])
                        op('dve', lambda: V.tensor_tensor(out=tk2[64:96, :], in0=KRR[64:96, 1, blk], in1=CS[64:96, 1, blk], op=ALU.mult),
                           reads=[KRR, CS], writes=[tk2])
                        for h in range(4):
                            op('dve', lambda: V.tensor_tensor(out=KT[64:96, h, gblk], in0=tk1[64:96, :], in1=tk2[64:96, :], op=ALU.add),
                               reads=[tk1, tk2], writes=[KT])
                        for r in range(4):
                            tl = m * 4 + r
                            pv = PBp[r % 2]
                            op('pe', lambda: T.matmul(pv[:, 0:256], lhsT=CKT[:, r * 128:(r + 1) * 128], rhs=WV[:], start=True,
                                                      stop=True), reads=[CKT, WV], writes=[pv])
                            op('dve', lambda: V.tensor_scalar(out=VD[:, tl, :, 0:64], in0=pv[:, 0:256].rearrange("p (h d) -> p h d", h=4),
                                                              scalar1=RKc[:, tl:tl + 1], scalar2=None, op0=ALU.mult),
                               reads=[pv, RKc], writes=[VD])
                    Sc.barrier()
                    sc_mla = 1.0 / math.sqrt(96.0)
                    STq = [STp[0], STp[1], PAp[0], PAp[1]]
                    Ebq = Eb + [sb(ph, "Ed3", [128, 512], BF16)]
                    items = [(h, m, j) for h in range(4) for m in range(NG) for j in range(4 * m + 4)]

                    def qk(k):
                        h, m, j = items[k]
                        rlo = max(0, j - 4 * m)
                        tlo = rlo * 128
                        n = 512 - tlo
                        sp_, eb_ = STq[k % 4], Ebq[k % 4]
                        op('pe', lambda: T.matmul(sp_[:, 0:n], lhsT=KT[:, h, j * 128:(j + 1) * 128],
                                                  rhs=QT[:, h, m * 512 + tlo:(m + 1) * 512], start=True, stop=True),
                           reads=[KT, QT], writes=[sp_])
                        op('act', lambda: A.activation(out=eb_[:, 0:n], in_=sp_[:, 0:n], func=AF.Exp, scale=sc_mla),
                           reads=[sp_], writes=[eb_])
                        if j >= 4 * m:
                            op('pool', lambda: P.memset(eb_[64:128, 0:64], 0.0), writes=[eb_])

                    def pv(k):
                        h, m, j = items[k]
                        rlo = max(0, j - 4 * m)
                        eb_ = Ebq[k % 4]
                        ob = Op[(h * NG + m) % 2]
                        for r in range(rlo, 4):
                            op('pe', lambda: T.matmul(ob[:, r * 65:(r + 1) * 65], lhsT=eb_[:, (r - rlo) * 128:(r - rlo + 1) * 128],
                                                      rhs=VD[:, j, h, :], start=(j == 0 and r == 0), stop=(j == 4 * m + r),
                                                      skip_group_check=True), reads=[eb_, VD], writes=[ob])
                        if j == 4 * m + 3:
                            ov = ob[:, 0:260].rearrange("p (r d) -> p r d", r=4)
                            yb = yst[(h * NG + m) % 2]
                            op('dve', lambda: V.reciprocal(out=rden[:], in_=ov[:, :, 64]), reads=[ob], writes=[rden])
                            op('dve', lambda: V.tensor_tensor(out=yb[:], in0=ov[:, :, 0:64],
                                                              in1=rden[:].unsqueeze(2).to_broadcast([128, 4, 64]), op=ALU.mult),
                               reads=[ob, rden], writes=[yb])
                            Sc.dma('pool', ybuf[m * 512:(m + 1) * 512, 768 + h * 64:768 + (h + 1) * 64].rearrange("(r p) d -> p r d", p=128),
                                   yb[:], reads=[yb])

                    LA = 2
                    for k in range(min(LA, len(items))):
                        qk(k)
                    for k in range(len(items)):
                        if k + LA < len(items):
                            qk(k + LA)
                        pv(k)
                Sc.barrier()

            if want('oproj'):
                with ExitStack() as ph:
                    WO = sb(ph, "WO", [128, 8, D], BF16)
                    gg = sb(ph, "gg", [128, 8], F32)
                    stg = [sb(ph, f"stgo{i}", [128, D], F32) for i in range(2)]
                    yt = [sb(ph, f"yt{i}", [128, D], F32) for i in range(2)]
                    xt = [sb(ph, f"xt{i}", [128, D], F32) for i in range(3)]
                    junk = sb(ph, "junko", [128, D], F32)
                    tmp = sb(ph, "tmpo", [128, D], F32)
                    ybs = [sb(ph, f"yb{i}", [128, D], BF16) for i in range(2)]
                    mT = sb(ph, "mT", [128, 8, 128], BF16)
                    hbs = [sb(ph, f"hbo{i}", [128, D], BF16) for i in range(2)]
                    h2s = [sb(ph, f"h2s{i}", [128, 8, 128], BF16) for i in range(2)]
                    sts = [sb(ph, f"sto{i}", [128, 16], F32) for i in range(2)]
                    pT = [ps(ph, f"pTo{i}", BF16) for i in range(2)]
                    PO = [ps(ph, f"PO{i}") for i in range(4)]
                    Sc.dma('sp', gg[:], dram_ap(Wl['g_group'], 0, [[1, 128], [128, 8]]), writes=[gg], allow_slow_non_contiguous=True)
                    for kc in range(8):
                        b = kc % 2
                        Sc.dma('sp', stg[b][:], Wl['w_out'][kc * 128:(kc + 1) * 128, :], writes=[stg[b]])
                        if kc % 2 == 0:
                            op('dve', lambda: V.tensor_scalar(out=WO[:, kc, :], in0=stg[b][:], scalar1=gg[:, kc:kc + 1], scalar2=None,
                                                              op0=ALU.mult), reads=[stg[b], gg], writes=[WO])
                        else:
                            op('pool', lambda: P.tensor_scalar(out=WO[:, kc, :], in0=stg[b][:], scalar1=gg[:, kc:kc + 1], scalar2=None,
                                                               op0=ALU.mult), reads=[stg[b], gg], writes=[WO])

                    def po_s0(i):
                        b = i % 2
                        y_t, x_t, st, yb = yt[b], xt[i % 3], sts[b], ybs[b]
                        Sc.dma('sp', y_t[:], ybuf[i * 128:(i + 1) * 128, :], writes=[y_t])
                        Sc.dma('sp', x_t[:], xin[i * 128:(i + 1) * 128, :], writes=[x_t])
                        for g in range(4):
                            op('act', lambda: A.activation(out=junk[:, 0:256], in_=y_t[:, g * 256:(g + 1) * 256], func=AF.Square,
                                                           scale=1.0 / 16.0, accum_out=st[:, g:g + 1]), reads=[y_t], writes=[junk, st],
                               multi=True)
                        op('act', lambda: A.activation(out=st[:, 4:8], in_=st[:, 0:4], func=AF.Sqrt, bias=epsT[:, 0:1], scale=1.0),
                           reads=[st, epsT], writes=[st])
                        op('dve', lambda: V.reciprocal(out=st[:, 8:12], in_=st[:, 4:8]), reads=[st], writes=[st])
                        for g in range(4):
                            op('dve', lambda: V.tensor_scalar(out=yb[:, g * 256:(g + 1) * 256], in0=y_t[:, g * 256:(g + 1) * 256],
                                                              scalar1=st[:, 8 + g:9 + g], scalar2=None, op0=ALU.mult),
                               reads=[y_t, st], writes=[yb])

                    def po_s1(i):
                        b = i % 2
                        x_t, st, yb, hb = xt[i % 3], sts[b], ybs[b], hbs[b]
                        for kc in range(8):
                            op('pe', lambda: T.transpose(out=pT[0][:, kc * 128:(kc + 1) * 128], in_=yb[:, kc * 128:(kc + 1) * 128],
                                                         identity=ident[:]), reads=[yb, ident], writes=[pT[0]])
                        op('act', lambda: A.copy(out=mT[:], in_=pT[0][:].rearrange("p (k t) -> p k t", k=8)), reads=[pT[0]], writes=[mT])
                        for nh in range(2):
                            po = PO[(i % 2) * 2 + nh]
                            for kc in range(8):
                                op('pe', lambda: T.matmul(po[:], lhsT=mT[:, kc, :], rhs=WO[:, kc, nh * 512:(nh + 1) * 512],
                                                          start=(kc == 0), stop=(kc == 7)), reads=[mT, WO], writes=[po])
                            hs = slice(nh * 512, (nh + 1) * 512)
                            op('dve', lambda: V.tensor_tensor(out=tmp[:, hs], in0=po[:], in1=MOD[:, 2, hs], op=ALU.mult),
                               reads=[po, MOD], writes=[tmp])
                            op('dve', lambda: V.tensor_tensor(out=x_t[:, hs], in0=x_t[:, hs], in1=tmp[:, hs], op=ALU.add),
                               reads=[x_t, tmp], writes=[x_t])
                        Sc.dma('pool', xout[i * 128:(i + 1) * 128, :], x_t[:], reads=[x_t])
                        op('act', lambda: A.activation(out=junk[:], in_=x_t[:], func=AF.Square, scale=1.0 / 32.0,
                                                       accum_out=st[:, 12:13]), reads=[x_t], writes=[junk, st], multi=True)
                        op('act', lambda: A.activation(out=st[:, 13:14], in_=st[:, 12:13], func=AF.Sqrt, bias=epsT[:, 0:1], scale=1.0),
                           reads=[st, epsT], writes=[st])
                        op('dve', lambda: V.reciprocal(out=st[:, 14:15], in_=st[:, 13:14]), reads=[st], writes=[st])
                        op('dve', lambda: V.scalar_tensor_tensor(out=tmp[:], in0=x_t[:], scalar=st[:, 14:15], in1=MOD[:, 4, :],
                                                                 op0=ALU.mult, op1=ALU.mult), reads=[x_t, st, MOD], writes=[tmp])
                        op('dve', lambda: V.tensor_tensor(out=hb[:], in0=tmp[:], in1=MOD[:, 3, :], op=ALU.add),
                           reads=[tmp, MOD], writes=[hb])

                    def po_s2(i):
                        b = i % 2
                        hb = hbs[b]
                        for kc in range(8):
                            op('pe', lambda: T.transpose(out=pT[1][:, kc * 128:(kc + 1) * 128], in_=hb[:, kc * 128:(kc + 1) * 128],
                                                         identity=ident[:]), reads=[hb, ident], writes=[pT[1]])
                        op('act', lambda: A.copy(out=h2s[b][:], in_=pT[1][:].rearrange("p (k t) -> p k t", k=8)),
                           reads=[pT[1]], writes=[h2s[b]])
                        Sc.dma('pool', h2T[:, i * 128:(i + 1) * 128].rearrange("(c p) t -> p c t", p=128), h2s[b][:], reads=[h2s[b]])

                    po_s0(0)
                    for i in range(NT):
                        if i + 1 < NT:
                            po_s0(i + 1)
                        po_s1(i)
                        if i >= 1:
                            po_s2(i - 1)
                    po_s2(NT - 1)
                Sc.barrier()

            if want('ffn'):
                for fp in range(2):
                    with ExitStack() as ph:
                        W1 = sb(ph, "W1", [128, 8, FH], BF16)
                        W3 = sb(ph, "W3", [128, 8, FH], BF16)
                        W2 = sb(ph, "W2", [128, NFC, D], BF16)
                        stg = [sb(ph, f"stgf{i}", [128, FH], F32) for i in range(2)]
                        H2 = [sb(ph, f"H2{i}", [128, 8, 512], BF16) for i in range(2)]
                        AT = sb(ph, "AT", [128, NFC, 512], BF16)
                        sl = [sb(ph, f"sl{i}", [128, 512], F32) for i in range(2)]
                        xt = [sb(ph, f"xtf{i}", [128, D], F32) for i in range(2)]
                        tmp = sb(ph, "tmpf", [128, D], F32)
                        ost = [sb(ph, f"ost{i}", [128, D], F32) for i in range(2)]
                        junk = sb(ph, "junkf", [128, D], F32)
                        gfb = sb(ph, "gfb", [128, D], F32)
                        st = sb(ph, "stf", [128, 4], F32)
                        G1p = [ps(ph, f"G1p{i}") for i in range(2)]
                        G3p = [ps(ph, f"G3p{i}") for i in range(2)]
                        PD = [ps(ph, f"PD{i}") for i in range(4)]
                        f0 = fp * FH
                        for kc in range(8):
                            b = kc % 2
                            Sc.dma('sp', stg[b][:], Wl['w1'][kc * 128:(kc + 1) * 128, f0:f0 + FH], writes=[stg[b]])
                            cast(W1[:, kc, :], stg[b][:], [stg[b]], [W1])
                        for kc in range(8):
                            b = kc % 2
                            Sc.dma('sp', stg[b][:], Wl['w3'][kc * 128:(kc + 1) * 128, f0:f0 + FH], writes=[stg[b]])
                            cast(W3[:, kc, :], stg[b][:], [stg[b]], [W3])
                        for fc in range(NFC):
                            b = fc % 2
                            Sc.dma('sp', stg[b][:, 0:D], Wl['w2'][f0 + fc * 128:f0 + (fc + 1) * 128, :], writes=[stg[b]])
                            cast(W2[:, fc, :], stg[b][:, 0:D], [stg[b]], [W2])
                        if last and fp == 1:
                            Sc.dma('sp', gfb[:], gfin_in[0].partition_broadcast(128), writes=[gfb])
                        for g in range(NG):
                            hh = H2[g % 2]
                            Sc.dma('sp', hh[:], h2T[:, g * 512:(g + 1) * 512].rearrange("(c p) t -> p c t", p=128), writes=[hh])
                            for fc in range(NFC):
                                g1, g3, s_ = G1p[fc % 2], G3p[fc % 2], sl[fc % 2]
                                for kc in range(8):
                                    op('pe', lambda: T.matmul(g1[:], lhsT=W1[:, kc, fc * 128:(fc + 1) * 128], rhs=hh[:, kc, :],
                                                              start=(kc == 0), stop=(kc == 7)), reads=[W1, hh], writes=[g1])
                                for kc in range(8):
                                    op('pe', lambda: T.matmul(g3[:], lhsT=W3[:, kc, fc * 128:(fc + 1) * 128], rhs=hh[:, kc, :],
                                                              start=(kc == 0), stop=(kc == 7)), reads=[W3, hh], writes=[g3])
                                op('act', lambda: A.activation(out=s_[:], in_=g1[:], func=AF.Silu), reads=[g1], writes=[s_])
                                op('dve', lambda: V.tensor_tensor(out=AT[:, fc, :], in0=s_[:], in1=g3[:], op=ALU.mult),
                                   reads=[s_, g3], writes=[AT])
                            for r in range(4):
                                i = g * 4 + r
                                x_t = xt[i % 2]
                                Sc.dma('sp', x_t[:], xout[i * 128:(i + 1) * 128, :], writes=[x_t])
                                for nh in range(2):
                                    pd = PD[(i % 2) * 2 + nh]
                                    hs = slice(nh * 512, (nh + 1) * 512)
                                    for fc in range(NFC):
                                        op('pe', lambda: T.matmul(pd[:], lhsT=AT[:, fc, r * 128:(r + 1) * 128], rhs=W2[:, fc, hs],
                                                                  start=(fc == 0), stop=(fc == NFC - 1)), reads=[AT, W2], writes=[pd])
                                    op('dve', lambda: V.tensor_tensor(out=tmp[:, hs], in0=pd[:], in1=MOD[:, 5, hs], op=ALU.mult),
                                       reads=[pd, MOD], writes=[tmp])
                                    op('dve', lambda: V.tensor_tensor(out=x_t[:, hs], in0=x_t[:, hs], in1=tmp[:, hs], op=ALU.add),
                                       reads=[x_t, tmp], writes=[x_t])
                                Sc.dma('pool', xout[i * 128:(i + 1) * 128, :], x_t[:], reads=[x_t])
                                if last and fp == 1:
                                    o_t = ost[i % 2]
                                    op('act', lambda: A.activation(out=junk[:], in_=x_t[:], func=AF.Square, scale=1.0 / 32.0,
                                                                   accum_out=st[:, 0:1]), reads=[x_t], writes=[junk, st], multi=True)
                                    op('act', lambda: A.activation(out=st[:, 1:2], in_=st[:, 0:1], func=AF.Sqrt, bias=epsT[:, 0:1],
                                                                   scale=1.0), reads=[st, epsT], writes=[st])
                                    op('dve', lambda: V.reciprocal(out=st[:, 2:3], in_=st[:, 1:2]), reads=[st], writes=[st])
                                    op('dve', lambda: V.scalar_tensor_tensor(out=o_t[:], in0=x_t[:], scalar=st[:, 2:3], in1=gfb[:],
                                                                             op0=ALU.mult, op1=ALU.mult), reads=[x_t, st, gfb], writes=[o_t])
                                    Sc.dma('pool', out[seq, i * 128:(i + 1) * 128, :], o_t[:], reads=[o_t])
                    Sc.barrier()
        Sc.barrier()
        print("program instructions:", Sc.ninstr, Sc.cnt)
    return nc


_PROG_CACHE = {}


def _get_prog(S, nseq, nlayer, steps):
    key = (S, nseq, nlayer, tuple(steps))
    if key not in _PROG_CACHE:
        _PROG_CACHE[key] = build(S, nseq, nlayer, list(steps))
    return _PROG_CACHE[key]


def kernel(**inputs):
    x = np.asarray(inputs['x'], dtype=np.float32)
    c = np.asarray(inputs['c'], dtype=np.float32)
    B, S, _ = x.shape
    L = np.asarray(inputs['w_mod']).shape[0]
    per = B // N_CORES
    consts = host_consts(S)
    t5 = np.ascontiguousarray(np.asarray(inputs['t5_table'], dtype=np.float32))
    gfin = np.ascontiguousarray(np.asarray(inputs['g_final'], dtype=np.float32)).reshape(1, -1)
    lw = [layout_weights(inputs, l) for l in range(L)]
    steps = tuple((sl, l, l == L - 1) for l in range(L) for sl in range(per))
    nc = _get_prog(S, per, L, steps)
    in_maps = []
    for core in range(N_CORES):
        m = dict(x=np.ascontiguousarray(x[core * per:(core + 1) * per]),
                 c=np.ascontiguousarray(c[core * per:(core + 1) * per]), t5=t5, g_final=gfin)
        for l in range(L):
            m.update({f"{k}_{l}": v for k, v in lw[l].items()})
        m.update(consts)
        in_maps.append(m)
    res = run_bass_kernel_spmd(nc, in_maps, core_ids=list(range(N_CORES)))
    full = np.empty((B, S, D), np.float32)
    for core in range(N_CORES):
        full[core * per:(core + 1) * per] = res.results[core]["out"]
    return full
```
